# Optimizing a Trainium2 kernel written in Bass

```python
import jax
import jax.numpy as jnp
from jax import lax
import numpy as np

D_MODEL = 1024
BATCH = 16
SEQ = 2048
DEPTH = 2

GRID_W = 64
CTX_LEN = 256

RET_HEADS = 4
RET_DK = 128
RET_DV = 128
RET_CHUNK = 128
NA_HEADS = 8
NA_DH = 64
NA_WIN_R = 8
NA_WIN_C = 16
NA_QB = NA_WIN_C
NA_KBW = 2 * NA_WIN_C
POOL_SIZES = (2, 4, 8, 16)
POOL_GROUP = D_MODEL // len(POOL_SIZES)
MOE_GROUPS = 4
MOE_PER_GROUP = 8
MOE_EXPERTS = MOE_GROUPS * MOE_PER_GROUP
MOE_HIDDEN = D_MODEL // 2
MOE_TOPK = 2

ROPE_BASE = 10000.0
LN_EPS = 1e-5
N_MOD = 6
DEEPNORM_ALPHA = (2 * DEPTH) ** 0.25
DEEPNORM_BETA = (8 * DEPTH) ** -0.25

RET_QKW = RET_HEADS * RET_DK
RET_VW = RET_HEADS * RET_DV
NA_W = NA_HEADS * NA_DH
MIX_W = RET_VW + NA_W
OFF_RK = 0
OFF_RV = OFF_RK + RET_QKW
OFF_NK = OFF_RV + RET_VW
OFF_NV = OFF_NK + NA_W
KV_COLS = OFF_NV + NA_W
OFF_RQ = KV_COLS
OFF_RG = OFF_RQ + RET_QKW
OFF_NQ = OFF_RG + RET_VW
IN_COLS = OFF_NQ + NA_W

kernel_name = 'hybrid_retention_natten_pool_hmoe_dit'


def _layer_norm(x, g, b):
    xf = x.astype(jnp.float32)
    mu = jnp.mean(xf, -1, keepdims=True)
    var = jnp.mean(jnp.square(xf - mu), -1, keepdims=True)
    return ((xf - mu) * lax.rsqrt(var + LN_EPS)).astype(x.dtype) * g + b


def _post_norm(h, y, g, b):
    return _layer_norm(DEEPNORM_ALPHA * h + y, g, b)


def _modulation(cvec, w_mod, b_mod):
    m = jax.nn.silu(cvec) @ w_mod + b_mod
    return [p[..., None, :] for p in jnp.split(m, N_MOD, axis=-1)]


def _modulate(h, shift, scale):
    return h * (1 + scale) + shift


def _split_heads(t, n_heads):
    b, n, _ = t.shape
    return jnp.transpose(t.reshape(b, n, n_heads, -1), (0, 2, 1, 3))


def _merge_heads(t):
    b, h, n, d = t.shape
    return jnp.transpose(t, (0, 2, 1, 3)).reshape(b, n, h * d)


def _axial_rope(n_tokens, head_dim):
    t = jnp.arange(n_tokens)
    rows = (t // GRID_W).astype(jnp.float32)
    cols = (t % GRID_W).astype(jnp.float32)
    n_freq = head_dim // 4
    inv_freq = ROPE_BASE ** (-jnp.arange(n_freq, dtype=jnp.float32) / n_freq)
    ang = jnp.concatenate([rows[:, None] * inv_freq, cols[:, None] * inv_freq], axis=-1)
    return jnp.cos(ang), jnp.sin(ang)


def _apply_rope(x, cos, sin):
    half = x.shape[-1] // 2
    x1, x2 = x[..., :half], x[..., half:]
    cos = cos.astype(x.dtype)
    sin = sin.astype(x.dtype)
    return jnp.concatenate([x1 * cos - x2 * sin, x1 * sin + x2 * cos], axis=-1)


def _retention_chunked(q, k, v, log_gamma, s0):
    b, h, n, _ = q.shape
    dv = v.shape[-1]
    nc = n // RET_CHUNK
    idx = jnp.arange(RET_CHUNK, dtype=jnp.float32)
    lg = log_gamma[:, None]
    diff = idx[:, None] - idx[None, :]
    intra = jnp.where(diff >= 0, jnp.exp(lg[..., None] * jnp.maximum(diff, 0.0)), 0.0)
    q_dec = jnp.exp(lg * (idx + 1.0))[..., None]
    k_dec = jnp.exp(lg * (RET_CHUNK - 1.0 - idx))[..., None]
    chunk_dec = jnp.exp(log_gamma * RET_CHUNK)[:, None, None]

    def to_chunks(t):
        return jnp.moveaxis(t.astype(jnp.float32).reshape(b, h, nc, RET_CHUNK, t.shape[-1]), 2, 0)

    def step(state, blk):
        qi, ki, vi = blk
        att = jnp.einsum('bhid,bhjd->bhij', qi, ki) * intra
        o = (jnp.einsum('bhij,bhje->bhie', att, vi)
             + jnp.einsum('bhid,bhde->bhie', qi, state) * q_dec)
        state = state * chunk_dec + jnp.einsum('bhjd,bhje->bhde', ki * k_dec, vi)
        return state, o

    state, o = lax.scan(step, s0, (to_chunks(q), to_chunks(k), to_chunks(v)))
    return jnp.moveaxis(o, 0, 2).reshape(b, h, n, dv), state


def _bidir_retention(q, k, v, log_gamma2, s0_fwd, s0_bwd):
    o_f, s_f = _retention_chunked(q, k, v, log_gamma2[0], s0_fwd)
    flip = lambda t: jnp.flip(t, axis=2)
    o_b, s_b = _retention_chunked(flip(q), flip(k), flip(v), log_gamma2[1], s0_bwd)
    return o_f + flip(o_b), s_f, s_b


def _retention_final_states(k, v, log_gamma2):
    n = k.shape[2]
    pos = jnp.arange(n, dtype=jnp.float32)
    w_f = jnp.exp(log_gamma2[0][:, None] * (n - 1.0 - pos))
    w_b = jnp.exp(log_gamma2[1][:, None] * pos)
    kf = k.astype(jnp.float32)
    vf = v.astype(jnp.float32)
    s_f = jnp.einsum('bhld,hl,bhle->bhde', kf, w_f, vf)
    s_b = jnp.einsum('bhld,hl,bhle->bhde', kf, w_b, vf)
    return s_f, s_b


def _head_group_norm(o):
    mu = jnp.mean(o, -1, keepdims=True)
    var = jnp.mean(jnp.square(o - mu), -1, keepdims=True)
    return (o - mu) * lax.rsqrt(var + LN_EPS)


def _neighbourhood_attention(q, k, v, k_ctx, v_ctx, rpb):
    b, h, n, d = q.shape
    rows = n // GRID_W
    wr = min(NA_WIN_R, rows)
    ncb = GRID_W // NA_QB
    qcol = np.arange(ncb)[:, None] * NA_QB + np.arange(NA_QB)[None, :]
    kstart = np.clip(np.arange(ncb) * NA_QB - NA_WIN_C // 2, 0, GRID_W - NA_KBW)
    kcol = kstart[:, None] + np.arange(NA_KBW)[None, :]
    wstart = np.clip(qcol - NA_WIN_C // 2, 0, GRID_W - NA_WIN_C)[..., None]
    col_ok = (kcol[:, None, :] >= wstart) & (kcol[:, None, :] < wstart + NA_WIN_C)
    col_bias_idx = np.clip(kcol[:, None, :] - qcol[..., None] + NA_WIN_C - 1, 0, 2 * NA_WIN_C - 2)
    qg = (q * NA_DH ** -0.5).reshape(b, h, rows, GRID_W, d)
    kg = k.reshape(b, h, rows, GRID_W, d)
    vg = v.reshape(b, h, rows, GRID_W, d)
    n_win = wr * NA_KBW

    def row_block(r):
        r0 = jnp.clip(r - wr // 2, 0, rows - wr)
        k_blk = lax.dynamic_slice_in_dim(kg, r0, wr, axis=2)[:, :, :, kcol]
        v_blk = lax.dynamic_slice_in_dim(vg, r0, wr, axis=2)[:, :, :, kcol]
        q_r = lax.dynamic_index_in_dim(qg, r, axis=2, keepdims=False).reshape(b, h, ncb, NA_QB, d)
        row_bias_idx = r0 + jnp.arange(wr) - r + NA_WIN_R - 1
        bias = rpb[:, row_bias_idx[:, None, None, None], col_bias_idx[None]]
        s_win = jnp.einsum('bhcqd,bhrckd->bhcqrk', q_r, k_blk).astype(jnp.float32)
        s_win = s_win + jnp.transpose(bias, (0, 2, 3, 1, 4)).astype(jnp.float32)
        s_win = jnp.where(col_ok[:, :, None, :], s_win, -jnp.inf)
        s_ctx = jnp.einsum('bhcqd,bhld->bhcql', q_r, k_ctx).astype(jnp.float32)
        logits = jnp.concatenate([s_win.reshape(b, h, ncb, NA_QB, n_win), s_ctx], axis=-1)
        p = jax.nn.softmax(logits, axis=-1).astype(v.dtype)
        p_win = p[..., :n_win].reshape(b, h, ncb, NA_QB, wr, NA_KBW)
        o = (jnp.einsum('bhcqrk,bhrckd->bhcqd', p_win, v_blk)
             + jnp.einsum('bhcql,bhld->bhcqd', p[..., n_win:], v_ctx))
        return o.reshape(b, h, GRID_W, d)

    o = lax.map(row_block, jnp.arange(rows))
    return jnp.transpose(o, (1, 2, 0, 3, 4)).reshape(b, h, n, d)


def _context_attention(q, k, v):
    s = jnp.einsum('bhqd,bhkd->bhqk', q, k).astype(jnp.float32) * NA_DH ** -0.5
    p = jax.nn.softmax(s, axis=-1).astype(v.dtype)
    return jnp.einsum('bhqk,bhkd->bhqd', p, v)


def _mixer_retention_na(h, hc, w_in, w_out, log_decay, rpb, update_ctx):
    b, n, _ = h.shape
    dtype = h.dtype
    log_gamma2 = jnp.log1p(-jnp.exp(log_decay.astype(jnp.float32)))
    cols = lambda t, off, width: t[..., off:off + width]
    p = h @ w_in
    pc = hc @ (w_in if update_ctx else w_in[:, :KV_COLS])
    rk_c = _split_heads(cols(pc, OFF_RK, RET_QKW), RET_HEADS)
    rv_c = _split_heads(cols(pc, OFF_RV, RET_VW), RET_HEADS)
    nk_c = _split_heads(cols(pc, OFF_NK, NA_W), NA_HEADS)
    nv_c = _split_heads(cols(pc, OFF_NV, NA_W), NA_HEADS)
    cos, sin = _axial_rope(n, RET_DK)
    rq = _apply_rope(_split_heads(cols(p, OFF_RQ, RET_QKW), RET_HEADS), cos, sin) * RET_DK ** -0.5
    rk = _apply_rope(_split_heads(cols(p, OFF_RK, RET_QKW), RET_HEADS), cos, sin)
    rv = _split_heads(cols(p, OFF_RV, RET_VW), RET_HEADS)
    rg = cols(p, OFF_RG, RET_VW)
    nq = _split_heads(cols(p, OFF_NQ, NA_W), NA_HEADS)
    nk = _split_heads(cols(p, OFF_NK, NA_W), NA_HEADS)
    nv = _split_heads(cols(p, OFF_NV, NA_W), NA_HEADS)
    if update_ctx:
        rq_c = _split_heads(cols(pc, OFF_RQ, RET_QKW), RET_HEADS) * RET_DK ** -0.5
        zeros = jnp.zeros((b, RET_HEADS, RET_DK, RET_DV), jnp.float32)
        o_ret_c, s_f, s_b = _bidir_retention(rq_c, rk_c, rv_c, log_gamma2, zeros, zeros)
    else:
        s_f, s_b = _retention_final_states(rk_c, rv_c, log_gamma2)
    o_ret, _, _ = _bidir_retention(rq, rk, rv, log_gamma2, s_f, s_b)
    y_ret = jax.nn.silu(rg) * _merge_heads(_head_group_norm(o_ret)).astype(dtype)
    y_na = _merge_heads(_neighbourhood_attention(nq, nk, nv, nk_c, nv_c, rpb))
    y = jnp.concatenate([y_ret, y_na], axis=-1) @ w_out
    if not update_ctx:
        return y, None
    rg_c = cols(pc, OFF_RG, RET_VW)
    nq_c = _split_heads(cols(pc, OFF_NQ, NA_W), NA_HEADS)
    y_ret_c = jax.nn.silu(rg_c) * _merge_heads(_head_group_norm(o_ret_c)).astype(dtype)
    y_na_c = _merge_heads(_context_attention(nq_c, nk_c, nv_c))
    yc = jnp.concatenate([y_ret_c, y_na_c], axis=-1) @ w_out
    return y, yc


def _pool_mixer(h, pool_w, pool_scale):
    b, n, d = h.shape
    hf = h.astype(jnp.float32).reshape(b, n, len(POOL_SIZES), POOL_GROUP)
    csum = jnp.pad(jnp.cumsum(hf, axis=1), ((0, 0), (1, 0), (0, 0), (0, 0)))
    pos = jnp.arange(n)
    outs = []
    for g, w in enumerate(POOL_SIZES):
        lo = jnp.clip(pos - w // 2, 0, n)
        hi = jnp.clip(pos + (w - w // 2), 0, n)
        cg = csum[:, :, g]
        mean = (cg[:, hi] - cg[:, lo]) / (hi - lo).astype(jnp.float32)[None, :, None]
        outs.append(mean - hf[:, :, g])
    z = jnp.stack(outs, axis=2).astype(h.dtype)
    y = jnp.einsum('bngc,gce->bnge', z, pool_w).reshape(b, n, d)
    return y * pool_scale


def _hier_moe(h, w_r1, b_r1, w_r2, b_r2, w_gate, w_up, w_down):
    b, n, d = h.shape
    t = h.reshape(b * n, d)
    logit_g = (t @ w_r1).astype(jnp.float32) + b_r1.astype(jnp.float32)
    grp = jnp.argmax(logit_g, axis=-1)
    gate_g = jnp.take_along_axis(jax.nn.softmax(logit_g, axis=-1), grp[:, None], axis=-1)
    logit_e = jnp.einsum('td,gde->tge', t, w_r2).astype(jnp.float32) + b_r2.astype(jnp.float32)
    logit_e = jnp.take_along_axis(logit_e, grp[:, None, None], axis=1)[:, 0]
    top_v, top_i = lax.top_k(logit_e, MOE_TOPK)
    w_sel = jax.nn.softmax(top_v, axis=-1) * gate_g
    eid = grp[:, None] * MOE_PER_GROUP + top_i
    combine = jnp.einsum('tk,tke->te', w_sel, jax.nn.one_hot(eid, MOE_EXPERTS, dtype=jnp.float32))

    def expert_step(acc, xs):
        wg, wu, wd, gcol = xs
        y = (jax.nn.silu(t @ wg) * (t @ wu)) @ wd
        return acc + y.astype(jnp.float32) * gcol[:, None], None

    acc, _ = lax.scan(expert_step, jnp.zeros((b * n, d), jnp.float32),
                      (w_gate, w_up, w_down, combine.T))
    return acc.astype(h.dtype).reshape(b, n, d)


def setup_inputs(seed: int = 0) -> dict:
    key = jax.random.key(seed)
    ks = jax.random.split(key, 21)
    f32 = jnp.float32

    def nrm(k, shape, scale):
        return jax.random.normal(k, shape, f32) * scale

    n_ab = (DEPTH + 1) // 2
    n_pool = DEPTH // 2
    base_log_decay = -(5.0 + jnp.arange(RET_HEADS, dtype=f32)) * jnp.log(2.0)
    return {
        'x': nrm(ks[0], (BATCH, SEQ, D_MODEL), 1.0),
        'c': nrm(ks[1], (BATCH, D_MODEL), 1.0),
        'ctx': nrm(ks[2], (BATCH, CTX_LEN, D_MODEL), 1.0),
        'c_ctx': nrm(ks[3], (D_MODEL,), 1.0),
        'w_mod': nrm(ks[4], (DEPTH, D_MODEL, N_MOD * D_MODEL), 0.5 * D_MODEL ** -0.5),
        'b_mod': nrm(ks[5], (DEPTH, N_MOD * D_MODEL), 0.02),
        'ln_g': 1.0 + nrm(ks[6], (DEPTH, 2, D_MODEL), 0.02),
        'ln_b': nrm(ks[7], (DEPTH, 2, D_MODEL), 0.02),
        'ab_w_in': nrm(ks[8], (n_ab, D_MODEL, IN_COLS), D_MODEL ** -0.5),
        'ab_w_out': nrm(ks[9], (n_ab, MIX_W, D_MODEL), DEEPNORM_BETA * MIX_W ** -0.5),
        'ab_log_decay': base_log_decay + nrm(ks[10], (n_ab, 2, RET_HEADS), 0.05),
        'ab_rpb': nrm(ks[11], (n_ab, NA_HEADS, 2 * NA_WIN_R - 1, 2 * NA_WIN_C - 1), 0.1),
        'pool_w': nrm(ks[12], (n_pool, len(POOL_SIZES), POOL_GROUP, POOL_GROUP), DEEPNORM_BETA * POOL_GROUP ** -0.5),
        'pool_scale': 1.0 + nrm(ks[13], (n_pool, D_MODEL), 0.1),
        'moe_w_r1': nrm(ks[14], (DEPTH, D_MODEL, MOE_GROUPS), D_MODEL ** -0.5),
        'moe_b_r1': nrm(ks[15], (DEPTH, MOE_GROUPS), 0.01),
        'moe_w_r2': nrm(ks[16], (DEPTH, MOE_GROUPS, D_MODEL, MOE_PER_GROUP), D_MODEL ** -0.5),
        'moe_b_r2': nrm(ks[17], (DEPTH, MOE_GROUPS, MOE_PER_GROUP), 0.01),
        'moe_w_gate': nrm(ks[18], (DEPTH, MOE_EXPERTS, D_MODEL, MOE_HIDDEN), D_MODEL ** -0.5),
        'moe_w_up': nrm(ks[19], (DEPTH, MOE_EXPERTS, D_MODEL, MOE_HIDDEN), D_MODEL ** -0.5),
        'moe_w_down': nrm(ks[20], (DEPTH, MOE_EXPERTS, MOE_HIDDEN, D_MODEL), DEEPNORM_BETA * MOE_HIDDEN ** -0.5),
    }


def reference(x, c, ctx, c_ctx, w_mod, b_mod, ln_g, ln_b, ab_w_in, ab_w_out, ab_log_decay, ab_rpb,
              pool_w, pool_scale, moe_w_r1, moe_b_r1, moe_w_r2, moe_b_r2, moe_w_gate, moe_w_up, moe_w_down):
    h, hc = x, ctx
    for i in range(DEPTH):
        j = i // 2
        carry_ctx = any(later % 2 == 0 for later in range(i + 1, DEPTH))
        moe_p = (moe_w_r1[i], moe_b_r1[i], moe_w_r2[i], moe_b_r2[i], moe_w_gate[i], moe_w_up[i], moe_w_down[i])
        m = _modulation(c, w_mod[i], b_mod[i])
        if i % 2 == 0 or carry_ctx:
            mc = _modulation(c_ctx, w_mod[i], b_mod[i])
        if i % 2 == 0:
            y, yc = _mixer_retention_na(_modulate(h, m[0], m[1]), _modulate(hc, mc[0], mc[1]),
                                        ab_w_in[j], ab_w_out[j], ab_log_decay[j], ab_rpb[j], carry_ctx)
        else:
            y = _pool_mixer(_modulate(h, m[0], m[1]), pool_w[j], pool_scale[j])
            if carry_ctx:
                yc = _pool_mixer(_modulate(hc, mc[0], mc[1]), pool_w[j], pool_scale[j])
        h = _post_norm(h, m[2] * y, ln_g[i, 0], ln_b[i, 0])
        h = _post_norm(h, m[5] * _hier_moe(_modulate(h, m[3], m[4]), *moe_p), ln_g[i, 1], ln_b[i, 1])
        if carry_ctx:
            hc = _post_norm(hc, mc[2] * yc, ln_g[i, 0], ln_b[i, 0])
            hc = _post_norm(hc, mc[5] * _hier_moe(_modulate(hc, mc[3], mc[4]), *moe_p), ln_g[i, 1], ln_b[i, 1])
    return h
```

```python
import numpy as np
from contextlib import ExitStack
from concourse.bass_utils import run_bass_kernel_spmd
import concourse.bass as bass
import concourse.mybir as mybir

F32 = mybir.dt.float32
BF16 = mybir.dt.bfloat16
ALU = mybir.AluOpType
AF = mybir.ActivationFunctionType
AX = mybir.AxisListType

COMPUTE = ("pe", "act", "dve", "pool")
EPOCH = 30000


class R:
    __slots__ = ("name", "lw", "rd")

    def __init__(self, name=""):
        self.name = name
        self.lw = None
        self.rd = []


class Op:
    __slots__ = ("eng", "fn", "is_dma", "deps", "idx", "sig", "sem", "val", "pos")

    def __init__(self, eng, fn, is_dma):
        self.eng = eng
        self.fn = fn
        self.is_dma = is_dma
        self.deps = []
        self.sig = False
        self.sem = None
        self.val = 0


class SemState:
    def __init__(self, nc, es):
        self.nc = nc
        self.es = es
        self.sems = {}
        self.cnt = {e: 0 for e in COMPUTE}
        self.dma_rr = {q: 0 for q in ("sp", "act", "pool")}
        self.dma_cnt = {}

    def get(self, name):
        if name not in self.sems:
            self.sems[name] = self.es.enter_context(self.nc.semaphore(name))
        return self.sems[name]


class Prog:
    def __init__(self, nc, ss, n_dma_sems=None):
        self.nc = nc
        self.ss = ss
        self.ops = []
        self.n_dma_sems = n_dma_sems or {"sp": 24, "act": 8, "pool": 12}

    def _add(self, eng, fn, reads, writes, is_dma):
        op = Op(eng, fn, is_dma)
        op.idx = len(self.ops)
        raw = {}
        other = {}
        for r in reads:
            if r.lw is not None:
                raw[r.lw.idx] = r.lw
        for w in writes:
            if w.lw is not None:
                other[w.lw.idx] = w.lw
            lastrd = {}
            for rd in w.rd:
                if rd.is_dma:
                    other[rd.idx] = rd
                else:
                    lastrd[rd.eng] = rd
            for rd in lastrd.values():
                other[rd.idx] = rd
        for r in reads:
            r.rd.append(op)
        for w in writes:
            w.lw = op
            w.rd = []
        for i, d in raw.items():
            if (not d.is_dma) and (not is_dma) and d.eng == eng and eng == "pe":
                continue
            op.deps.append(d)
        for i, d in other.items():
            if i in raw:
                continue
            if (not d.is_dma) and (not is_dma) and d.eng == eng:
                continue
            op.deps.append(d)
        self.ops.append(op)
        return op

    def op(self, eng, fn, reads=(), writes=()):
        assert eng in COMPUTE
        return self._add(eng, fn, list(reads), list(writes), False)

    def dma(self, q, fn, reads=(), writes=()):
        assert q in ("sp", "act", "pool")
        return self._add(q, fn, list(reads), list(writes), True)

    def emit(self, final_wait_all=True):
        nc = self.nc
        ops = self.ops
        for op in ops:
            for d in op.deps:
                d.sig = True
        last_of = {}
        for op in ops:
            last_of[op.eng if not op.is_dma else ("dma", op.eng)] = op
        ss = self.ss
        get_sem = ss.get
        cnt = ss.cnt
        dma_rr = ss.dma_rr
        dma_cnt = ss.dma_cnt
        dma_prev = {}
        final_dma = []
        for op in ops:
            if op.is_dma:
                q = op.eng
                slot = dma_rr[q] % self.n_dma_sems[q]
                dma_rr[q] += 1
                key = (q, slot)
                op.sem = get_sem(f"d_{q}_{slot}")
                dma_cnt[key] = dma_cnt.get(key, 0) + 16
                op.val = dma_cnt[key]
                prev = dma_prev.get(key)
                if prev is not None:
                    op.deps.append(prev)
                dma_prev[key] = op
                op.sig = True
            elif op.sig:
                e = op.eng
                ep = cnt[e] // EPOCH
                op.sem = get_sem(f"c_{e}_{ep}")
                cnt[e] += 1
                op.val = cnt[e] - ep * EPOCH
        final_dma = list(dma_prev.values())

        per_eng = {e: [] for e in ("pe", "act", "dve", "pool", "sp")}
        for op in ops:
            per_eng[op.eng].append(op)

        engobj = {"pe": None, "act": None, "dve": None, "pool": None, "sp": None}
        self.stats = {e: len(v) for e, v in per_eng.items()}

        def run_engine(ename, eng):
            known = {}
            for op in per_eng[ename]:
                need = {}
                for d in op.deps:
                    s = d.sem
                    if s is None:
                        continue
                    k = s.name if hasattr(s, "name") else id(s)
                    if known.get(k, 0) >= d.val:
                        continue
                    if k not in need or need[k][1] < d.val:
                        need[k] = (s, d.val)
                for k, (s, v) in need.items():
                    eng.wait_ge(s, v)
                    known[k] = v
                ins = op.fn(eng)
                if op.sig:
                    ins.then_inc(op.sem, 16 if op.is_dma else 1)
            if ename == "sp" and final_wait_all:
                for d in final_dma:
                    k = d.sem.name
                    if known.get(k, 0) < d.val:
                        eng.wait_ge(d.sem, d.val)
                        known[k] = d.val

        with nc.Block() as block:
            @block.tensor
            def _(e):
                run_engine("pe", e)

            @block.scalar
            def _(e):
                run_engine("act", e)

            @block.vector
            def _(e):
                run_engine("dve", e)

            @block.gpsimd
            def _(e):
                run_engine("pool", e)

            @block.sync
            def _(e):
                run_engine("sp", e)


D = 1024
NT = 16
NL = 2048
NCX = 256
ALPHA = float(4 ** 0.25)
EPS = 1e-5
NEG = -30000.0
OFF_RK, OFF_RV, OFF_NK, OFF_NV, OFF_RQ, OFF_RG, OFF_NQ = 0, 512, 1024, 1536, 2048, 2560, 3072


class Phase:
    cnt = 0

    def __init__(self, kb, name):
        self.kb = kb
        self.nc = kb.nc
        self.name = name
        self.P = Prog(kb.nc, kb.ss)
        self.es = ExitStack()
        self.n = 0

    def sb(self, shape, dt=F32):
        Phase.cnt += 1
        return self.es.enter_context(self.nc.sbuf_tensor(f"{self.name}_s{Phase.cnt}", list(shape), dt))

    def ps(self, shape, dt=F32):
        Phase.cnt += 1
        return self.es.enter_context(self.nc.psum_tensor(f"{self.name}_p{Phase.cnt}", list(shape), dt))

    def banks(self, n):
        return [(self.ps([128, 512], F32), R()) for _ in range(n)]

    def finish(self):
        self.P.emit()
        self.es.close()
        self.nc.all_engine_barrier()


def emit_pipelined(units):
    n = len(units)
    S = max(len(u) for u in units) if units else 0
    for step in range(n + S - 1):
        for st in range(S):
            i = step - st
            if 0 <= i < n and st < len(units[i]):
                units[i][st]()


def ld(P, dst, src, reads=(), writes=(), q="sp", **kw):
    return P.dma(q, lambda e: e.dma_start(out=dst, in_=src, **kw), reads, writes)


def phase_M(kb):
    nc = kb.nc
    ph = Phase(kb, "M")
    P = ph.P
    T = kb.t
    ident = ph.sb([128, 128]); r_id = R()
    ld(P, ident[:], T["ident"], [], [r_id])
    c3 = ph.sb([3, D]); r_c3 = R()
    ld(P, c3[0:2, :], T["c"], [], [r_c3])
    ld(P, c3[2:3, :], T["c_ctx"].rearrange("(o d) -> o d", o=1), [], [r_c3])
    s3 = ph.sb([3, D]); r_s3 = R()
    P.op("act", lambda e: e.activation(out=s3[:], in_=c3[:], func=AF.Silu), [r_c3], [r_s3])
    sT = ph.sb([128, 8, 3]); r_sT = R()
    bk = ph.banks(4)
    pT, r_pT = bk[0]
    for k in range(8):
        P.op("pe", lambda e, k=k: e.transpose(out=pT[:, k * 3:(k + 1) * 3], in_=s3[0:3, k * 128:(k + 1) * 128], identity=ident[0:3, 0:3]),
             [r_s3, r_id], [r_pT])
    P.op("dve", lambda e: e.tensor_copy(out=sT[:].rearrange("p k c -> p (k c)"), in_=pT[:, 0:24]), [r_pT], [r_sT])
    zt = ph.sb([120, 128]); r_zt = R()
    P.op("pool", lambda e: e.memset(zt[:], 0.0), [], [r_zt])
    r_RP = R()
    ld(P, T["RP"].rearrange("h a j -> (h a) j"), zt[:], [r_zt], [r_RP])
    ld(P, T["RP"][:, :, 48:79], T["rpb"], [], [r_RP])
    bt = ph.sb([128, 8, 15, 64]); r_bt = R()
    rs_bt = [R() for _ in range(128)]
    for u in range(2):
        for qc in range(64):
            p = u * 64 + qc
            ld(P, bt[p:p + 1, :, :, :], T["RP"][:, :, 63 - qc:127 - qc].rearrange("(o h) a j -> o h a j", o=1), [r_RP], [rs_bt[p]])
    cm = ph.sb([128, 64]); r_cm = R()
    ld(P, cm[:], T["colmask"], [], [r_cm])
    for h in range(8):
        P.op("pool" if h % 2 else "dve", lambda e, h=h: e.tensor_tensor(out=bt[:, h, :, :], in0=bt[:, h, :, :], in1=cm[:].unsqueeze(1).to_broadcast([128, 15, 64]), op=ALU.add),
             rs_bt + [r_cm], [r_bt])
    ld(P, T["TOEP"].rearrange("h p a j -> p h (a j)"), bt[:].rearrange("p h a j -> p h (a j)"), [r_bt], [])
    mv = ph.sb([3, 2, 6 * D]); r_mv = R()
    bb = ph.sb([3, 2, 6 * D]); r_bb = R()
    for l in range(2):
        ld(P, bb[:, l, :], T["b_mod"][l, :].partition_broadcast(3), [], [r_bb])
    wst = [(ph.sb([128, 8, 512]), R()) for _ in range(3)]
    i = 0
    for l in range(2):
        for j in range(12):
            w, r_w = wst[i % 3]
            pb, r_pb = bk[1 + i % 2]
            i += 1
            ld(P, w[:], T["w_mod"][l, :, j * 512:(j + 1) * 512].rearrange("(k p) n -> p k n", p=128), [], [r_w])
            for k in range(8):
                P.op("pe", lambda e, k=k, w=w, pb=pb: e.matmul(pb[0:3, :], lhsT=sT[:, k, :], rhs=w[:, k, :], start=(k == 0), stop=(k == 7)),
                     [r_sT, r_w], [r_pb])
            P.op("dve", lambda e, l=l, j=j, pb=pb: e.tensor_tensor(out=mv[:, l, j * 512:(j + 1) * 512], in0=pb[0:3, :], in1=bb[:, l, j * 512:(j + 1) * 512], op=ALU.add),
                 [r_pb, r_bb], [r_mv])
    for l in range(2):
        ld(P, T["MV"][l], mv[:, l, :], [r_mv], [])
    mT = ph.sb([128, 2, 48, 3]); r_mT = R()
    pM, r_pM = bk[3]
    for l in range(2):
        for c in range(48):
            P.op("pe", lambda e, l=l, c=c: e.transpose(out=pM[:, c * 3:(c + 1) * 3], in_=mv[0:3, l, c * 128:(c + 1) * 128], identity=ident[0:3, 0:3]),
                 [r_mv, r_id], [r_pM])
        P.op("dve", lambda e, l=l: e.tensor_copy(out=mT[:, l, :, :].rearrange("p c t -> p (c t)"), in_=pM[:, 0:144]), [r_pM], [r_mT])
    ld(P, T["MT"].rearrange("l p c t -> p l (c t)"), mT[:].rearrange("p l c t -> p l (c t)"), [r_mT], [])

    ph.finish()


def load_mT(ph, l):
    T = ph.kb.t
    mT = ph.sb([128, 48, 3]); r = R()
    ld(ph.P, mT[:].rearrange("p c t -> p (c t)"), T["MT"][l].rearrange("p c t -> p (c t)"), [], [r])
    return mT, r


def make_mod_cols(ph, mT, r_mT, shift_idx, scale_idx, col):
    P = ph.P
    sc1 = ph.sb([128, 8]); sh = ph.sb([128, 8]); r = R()
    P.op("dve", lambda e: e.tensor_scalar(out=sc1[:], in0=mT[:, scale_idx * 8:(scale_idx + 1) * 8, col], scalar1=1.0, scalar2=None, op0=ALU.add), [r_mT], [r])
    P.op("dve", lambda e: e.tensor_copy(out=sh[:], in_=mT[:, shift_idx * 8:(shift_idx + 1) * 8, col]), [r_mT], [r])
    return sc1, sh, r


def build_xT(ph, src_rows, n_tiles, dstT, dst_off, sc1, sh, r_mod, ident, r_id, bk, r_dst_list, xin, extra=None, all_act=False):
    P = ph.P
    ng = (n_tiles + 3) // 4
    bi = 0
    for g in range(ng):
        nt = min(4, n_tiles - g * 4)
        xt, r_xt = xin[g % len(xin)]
        ld(P, xt[:, 0:nt, :], src_rows[g * 512:g * 512 + nt * 128, :].rearrange("(t p) d -> p t d", p=128), [], [r_xt])
        for k in range(8):
            pb, r_pb = bk[bi % len(bk)]
            bi += 1
            for t in range(nt):
                P.op("pe", lambda e, pb=pb, t=t, k=k, xt=xt: e.transpose(out=pb[:, t * 128:(t + 1) * 128], in_=xt[:, t, k * 128:(k + 1) * 128], identity=ident[:]),
                     [r_xt, r_id], [r_pb])
            o = dst_off + g * 512
            if extra is not None:
                xt32, r_x32 = extra(g, k, pb, r_pb, nt)
                P.op("act", lambda e, k=k, o=o, nt=nt, xt32=xt32: e.activation(out=dstT[:, k, o:o + nt * 128], in_=xt32[:, k, 0:nt * 128], func=AF.Copy), [r_x32], [r_dst_list[g]])
                if k == 7:
                    extra(g, 8, pb, r_pb, nt)
            elif k % 2 == 0:
                P.op("act", lambda e, pb=pb, k=k, o=o, nt=nt: e.activation(out=dstT[:, k, o:o + nt * 128], in_=pb[:, 0:nt * 128], func=AF.Identity,
                                                                             scale=sc1[:, k:k + 1], bias=sh[:, k:k + 1]), [r_pb, r_mod], [r_dst_list[g]])
            else:
                P.op("dve", lambda e, pb=pb, k=k, o=o, nt=nt: e.tensor_scalar(out=dstT[:, k, o:o + nt * 128], in0=pb[:, 0:nt * 128], scalar1=sc1[:, k:k + 1], scalar2=sh[:, k:k + 1],
                                                                                op0=ALU.mult, op1=ALU.add), [r_pb, r_mod], [r_dst_list[g]])


def load_bcast(ph, row_ap, width=D):
    t = ph.sb([128, width]); r = R()
    ld(ph.P, t[:], row_ap.partition_broadcast(128), [], [r])
    return t, r


def post_norm(ph, ysrc, y_reads, hsrc_ap, gate, r_gate, lng, r_lng, lnb, r_lnb, dst_ap, bufs, i, pre=None):
    P = ph.P
    nb_ = len(bufs["h"])
    hb, r_hb = bufs["h"][i % nb_]
    tb, r_tb = bufs["t"][i % nb_]
    ob, r_ob = bufs["o"][i % nb_]
    st, r_st = bufs["st"][i % nb_]

    def s0():
        ld(P, hb[:], hsrc_ap, [], [r_hb])
        if pre is not None:
            pre()
        off = 0
        for ap in ysrc:
            w = ap.shape[-1]
            P.op("dve", lambda e, ap=ap, off=off, w=w: e.tensor_tensor(out=tb[:, off:off + w], in0=ap, in1=gate[:, off:off + w], op=ALU.mult), list(y_reads) + [r_gate], [r_tb])
            off += w
        P.op("act", lambda e: e.mul(out=hb[:], in_=hb[:], mul=ALPHA), [r_hb], [r_hb])
        P.op("pool", lambda e: e.tensor_tensor(out=hb[:], in0=hb[:], in1=tb[:], op=ALU.add), [r_hb, r_tb], [r_hb])

    def s1():
        for j in range(2):
            P.op("dve", lambda e, j=j: e.bn_stats(out=st[:, j * 6:(j + 1) * 6], in_=hb[:, j * 512:(j + 1) * 512]), [r_hb], [r_st])
        P.op("dve", lambda e: e.bn_aggr(out=st[:, 12:14], in_=st[:, 0:12].rearrange("p (a b) -> p a b", a=2)), [r_st], [r_st])
        P.op("dve", lambda e: e.tensor_scalar(out=st[:, 14:15], in0=st[:, 13:14], scalar1=EPS, scalar2=None, op0=ALU.add), [r_st], [r_st])
        P.op("act", lambda e: e.activation(out=st[:, 14:15], in_=st[:, 14:15], func=AF.Sqrt), [r_st], [r_st])
        P.op("dve", lambda e: e.reciprocal(out=st[:, 14:15], in_=st[:, 14:15]), [r_st], [r_st])
        P.op("dve", lambda e: e.scalar_tensor_tensor(out=st[:, 15:16], in0=st[:, 12:13], scalar=-1.0, in1=st[:, 14:15], op0=ALU.mult, op1=ALU.mult), [r_st], [r_st])

    def s2():
        P.op("act", lambda e: e.activation(out=tb[:], in_=hb[:], func=AF.Identity, scale=st[:, 14:15], bias=st[:, 15:16]), [r_hb, r_st], [r_tb])
        P.op("pool", lambda e: e.tensor_tensor(out=ob[:], in0=tb[:], in1=lng[:], op=ALU.mult), [r_tb, r_lng], [r_ob])
        P.op("dve", lambda e: e.tensor_tensor(out=ob[:], in0=ob[:], in1=lnb[:], op=ALU.add), [r_ob, r_lnb], [r_ob])
        ld(P, dst_ap, ob[:], [r_ob], [], q="pool")

    return [s0, s1, s2]


def pn_bufs(ph, n=3):
    return {
        "h": [(ph.sb([128, D]), R()) for _ in range(n)],
        "t": [(ph.sb([128, D]), R()) for _ in range(n)],
        "o": [(ph.sb([128, D]), R()) for _ in range(n)],
        "st": [(ph.sb([128, 16]), R()) for _ in range(n)],
    }


class ACtx:
    pass


def phase_A(kb, b):
    nc = kb.nc
    T = kb.t
    pers = ExitStack()
    Phase.cnt += 1
    xmT = pers.enter_context(nc.sbuf_tensor(f"A_xmT{Phase.cnt}", [128, 8, NL + NCX], BF16))
    catT = pers.enter_context(nc.sbuf_tensor(f"A_catT{Phase.cnt}", [128, 8, NL], BF16))
    identf = pers.enter_context(nc.sbuf_tensor(f"A_idf{Phase.cnt}", [128, 128], F32))
    identb = pers.enter_context(nc.sbuf_tensor(f"A_idb{Phase.cnt}", [128, 128], BF16))

    ph = Phase(kb, "A1")
    P = ph.P
    r_id = R()
    ld(P, identf[:], T["ident"], [], [r_id])
    r_idb = R()
    P.op("dve", lambda e: e.tensor_copy(out=identb[:], in_=identf[:]), [r_id], [r_idb])
    mT, r_mT = load_mT(ph, 0)
    sc1, sh, r_mod = make_mod_cols(ph, mT, r_mT, 0, 1, b)
    sc1c, shc, r_modc = make_mod_cols(ph, mT, r_mT, 0, 1, 2)
    bk = ph.banks(4)
    xin = [(ph.sb([128, 4, D]), R()) for _ in range(2)]
    r_x = [R() for _ in range(5)]
    build_xT(ph, T["x"][b], 16, xmT, 0, sc1, sh, r_mod, identf, r_id, bk, r_x[0:4], xin)
    build_xT(ph, T["ctx"][b], 2, xmT, NL, sc1c, shc, r_modc, identf, r_id, bk, r_x[4:5], xin)
    ph.finish()

    for h in range(4):
        phase_A_ret(kb, b, h, xmT, catT, identb)
    phase_A_na_all(kb, b, xmT, catT, identb)
    ph = Phase(kb, "A4")
    P = ph.P
    wst = ph.sb([128, 8, D]); r_wst = R()
    wo = ph.sb([128, 8, D], BF16); r_wo = R()
    ld(P, wst[:], T["w_out"].rearrange("(k p) n -> p k n", p=128), [], [r_wst])
    for k in range(8):
        P.op("pool" if k % 2 else "dve", lambda e, k=k: e.tensor_copy(out=wo[:, k, :], in_=wst[:, k, :]), [r_wst], [r_wo])
    gate, r_gate = load_bcast(ph, T["MV"][0, b, 2 * D:3 * D])
    lng, r_lng = load_bcast(ph, T["ln_g"][0, 0])
    lnb, r_lnb = load_bcast(ph, T["ln_b"][0, 0])
    bufs = pn_bufs(ph)
    bk = ph.banks(4)
    r_cat = R()
    units = []
    for t in range(NT):
        (p0, r0), (p1, r1) = bk[(t % 2) * 2], bk[(t % 2) * 2 + 1]

        def pre(t=t, p0=p0, r0=r0, p1=p1, r1=r1):
            for half, (pb, r_pb) in enumerate(((p0, r0), (p1, r1))):
                for k in range(8):
                    P.op("pe", lambda e, k=k, pb=pb, half=half: e.matmul(pb[:], lhsT=catT[:, k, t * 128:(t + 1) * 128], rhs=wo[:, k, half * 512:(half + 1) * 512],
                                                                      start=(k == 0), stop=(k == 7)), [r_cat, r_wo], [r_pb])
        units.append(post_norm(ph, [p0[:], p1[:]], [r0, r1], T["x"][b, t * 128:(t + 1) * 128, :], gate, r_gate, lng, r_lng, lnb, r_lnb,
                               T["H1"][b, t * 128:(t + 1) * 128, :], bufs, t, pre=pre))
    emit_pipelined(units)
    ph.finish()
    pers.close()


def phase_A_ret(kb, b, h, xmT, catT, identb):
    nc = kb.nc
    T = kb.t
    ph = Phase(kb, f"AR{h}")
    P = ph.P
    r_xm = R(); r_idb = R()
    ldt = ph.sb([128, 8]); r_ld = R()
    ld(P, ldt[:], T["log_decay"].rearrange("a h -> (a h)").partition_broadcast(128), [], [r_ld])
    lg = ph.sb([128, 8]); r_lg = R()
    P.op("act", lambda e: e.activation(out=lg[:], in_=ldt[:], func=AF.Exp), [r_ld], [r_lg])
    P.op("act", lambda e: e.activation(out=lg[:], in_=lg[:], func=AF.Ln, scale=-1.0, bias=1.0), [r_lg], [r_lg])
    rtab = ph.sb([128, 4, 128]); r_rt = R()
    ld(P, rtab[:], T["rtab"], [], [r_rt])
    ktab = ph.sb([128, 4]); r_kt = R()
    ld(P, ktab[:], T["ktab"], [], [r_kt])
    lgf = lg[:, h:h + 1]
    lgb = lg[:, 4 + h:5 + h]
    dm = ph.sb([128, 128]); dm2 = ph.sb([128, 128]); r_dm = R(); r_dm2 = R()
    P.op("act", lambda e: e.activation(out=dm[:], in_=rtab[:, 0, :], func=AF.Exp, scale=lgf), [r_rt, r_lg], [r_dm])
    P.op("act", lambda e: e.activation(out=dm2[:], in_=rtab[:, 1, :], func=AF.Exp, scale=lgb), [r_rt, r_lg], [r_dm2])
    P.op("dve", lambda e: e.tensor_tensor(out=dm[:], in0=dm[:], in1=dm2[:], op=ALU.add), [r_dm, r_dm2], [r_dm])
    qd = ph.sb([128, 2, 128]); r_qd = R()
    P.op("act", lambda e: e.activation(out=qd[:, 0, :], in_=rtab[:, 2, :], func=AF.Exp, scale=lgf), [r_rt, r_lg], [r_qd])
    P.op("act", lambda e: e.activation(out=qd[:, 1, :], in_=rtab[:, 3, :], func=AF.Exp, scale=lgb), [r_rt, r_lg], [r_qd])
    kdc = ph.sb([128, 4]); r_kd = R()
    for j, s in enumerate((lgf, lgb, lgf, lgb)):
        P.op("act", lambda e, j=j, s=s: e.activation(out=kdc[:, j:j + 1], in_=ktab[:, j:j + 1], func=AF.Exp, scale=s), [r_kt, r_lg], [r_kd])
    cs = ph.sb([128, NT, 2, 64]); r_cs = R()
    ld(P, cs[:], T["rope"], [], [r_cs])
    wst = ph.sb([128, 8, 4, 128]); r_wst = R()
    for j, off in enumerate((OFF_RK, OFF_RQ, OFF_RV, OFF_RG)):
        ld(P, wst[:, :, j, :], T["w_in"][:, off + h * 128:off + (h + 1) * 128].rearrange("(k p) n -> p k n", p=128), [], [r_wst])
    wb = ph.sb([128, 8, 512], BF16); r_wb = R()
    for k in range(8):
        P.op("pool" if k % 2 else "dve", lambda e, k=k: e.tensor_copy(out=wb[:, k, :], in_=wst[:, k, :, :].rearrange("p a n -> p (a n)")), [r_wst], [r_wb])
    NTA = NT + 2
    kq = ph.sb([128, NTA, 2, 128], BF16); r_kq = [R() for _ in range(NTA)]
    vt = ph.sb([128, NTA, 128], BF16); r_vt = [R() for _ in range(NTA)]
    gs = ph.sb([128, NT, 128]); r_gs = [R() for _ in range(NT)]
    kd = ph.sb([128, NTA, 2, 128], BF16); r_kdd = [R() for _ in range(NTA)]
    kT = ph.sb([128, NTA * 128], BF16); r_kT = [R() for _ in range(NTA)]
    qT = ph.sb([128, NL], BF16); r_qT = [R() for _ in range(NT)]
    bk = ph.banks(3)
    kqf = [(ph.sb([128, 2, 128]), R()) for _ in range(3)]
    tmp = [(ph.sb([128, 4, 2, 64]), [R() for _ in range(4)]) for _ in range(3)]
    ptb = [(ph.ps([128, 1024], BF16), R()) for _ in range(2)]
    order = [16, 17] + list(range(NT))

    def tok_unit(n, t):
        pb, r_pb = bk[n % 3]
        tok = NL + (t - 16) * 128 if t >= 16 else t * 128
        kf, r_kf = kqf[n % 3]
        tm, r_tm = tmp[n % 3]
        pt, r_pt = ptb[n % 2]
        eng = "dve" if n % 2 == 0 else "pool"

        def sA():
            for k in range(8):
                P.op("pe", lambda e, k=k: e.matmul(pb[:], lhsT=xmT[:, k, tok:tok + 128], rhs=wb[:, k, :], start=(k == 0), stop=(k == 7)), [r_xm, r_wb], [r_pb])
            P.op("act", lambda e: e.activation(out=vt[:, t, :], in_=pb[:, 256:384], func=AF.Copy), [r_pb], [r_vt[t]])
            if t >= 16:
                P.op("act", lambda e: e.activation(out=kq[:, t, 0, :], in_=pb[:, 0:128], func=AF.Copy), [r_pb], [r_kq[t]])
            else:
                P.op("act", lambda e: e.activation(out=kf[:, 0, :], in_=pb[:, 0:128], func=AF.Copy), [r_pb], [r_kf])
                P.op("act", lambda e: e.activation(out=kf[:, 1, :], in_=pb[:, 128:256], func=AF.Identity, scale=float(128 ** -0.5)), [r_pb], [r_kf])
                P.op("act", lambda e: e.activation(out=gs[:, t, :], in_=pb[:, 384:512], func=AF.Silu), [r_pb], [r_gs[t]])

        def sB():
            if t < 16:
                cosb = cs[:, t, 0, :].unsqueeze(1).to_broadcast([128, 2, 64])
                sinb = cs[:, t, 1, :].unsqueeze(1).to_broadcast([128, 2, 64])
                x1 = kf[:, :, 0:64]
                x2 = kf[:, :, 64:128]
                P.op(eng, lambda e: e.tensor_tensor(out=tm[:, 0, :, :], in0=x1, in1=cosb, op=ALU.mult), [r_kf, r_cs], [r_tm[0]])
                P.op(eng, lambda e: e.tensor_tensor(out=tm[:, 1, :, :], in0=x2, in1=sinb, op=ALU.mult), [r_kf, r_cs], [r_tm[1]])
                P.op(eng, lambda e: e.tensor_tensor(out=tm[:, 2, :, :], in0=x1, in1=sinb, op=ALU.mult), [r_kf, r_cs], [r_tm[2]])
                P.op(eng, lambda e: e.tensor_tensor(out=tm[:, 3, :, :], in0=x2, in1=cosb, op=ALU.mult), [r_kf, r_cs], [r_tm[3]])
                P.op(eng, lambda e: e.tensor_tensor(out=kq[:, t, :, 0:64], in0=tm[:, 0, :, :], in1=tm[:, 1, :, :], op=ALU.subtract), [r_tm[0], r_tm[1]], [r_kq[t]])
                P.op(eng, lambda e: e.tensor_tensor(out=kq[:, t, :, 64:128], in0=tm[:, 2, :, :], in1=tm[:, 3, :, :], op=ALU.add), [r_tm[2], r_tm[3]], [r_kq[t]])
            P.op(eng, lambda e: e.tensor_scalar(out=kd[:, t, 0, :], in0=kq[:, t, 0, :], scalar1=kdc[:, 0:1], scalar2=None, op0=ALU.mult), [r_kq[t], r_kd], [r_kdd[t]])
            P.op(eng, lambda e: e.tensor_scalar(out=kd[:, t, 1, :], in0=kq[:, t, 0, :], scalar1=kdc[:, 1:2], scalar2=None, op0=ALU.mult), [r_kq[t], r_kd], [r_kdd[t]])

        def sC():
            P.op("pe", lambda e: e.transpose(out=pt[:, 0:128], in_=kq[:, t, 0, :], identity=identb[:]), [r_kq[t], r_idb], [r_pt])
            if t < 16:
                P.op("pe", lambda e: e.transpose(out=pt[:, 128:256], in_=kq[:, t, 1, :], identity=identb[:]), [r_kq[t], r_idb], [r_pt])
                P.op("act", lambda e: e.activation(out=qT[:, t * 128:(t + 1) * 128], in_=pt[:, 128:256], func=AF.Copy), [r_pt], [r_qT[t]])
            P.op("act", lambda e: e.activation(out=kT[:, t * 128:(t + 1) * 128], in_=pt[:, 0:128], func=AF.Copy), [r_pt], [r_kT[t]])

        return [sA, sB, sC]

    emit_pipelined([tok_unit(n, t) for n, t in enumerate(order)])
    ub = ph.banks(2)
    Sf = [(ph.sb([128, 128]), R()) for _ in range(2)]
    Sb = [(ph.sb([128, 128]), R()) for _ in range(2)]
    sbf = ph.sb([128, NT, 2, 128], BF16); r_sbf = [R() for _ in range(NT)]
    chains = (
        (0, [16, 17] + list(range(NT)), Sf),
        (1, [17, 16] + list(range(NT - 1, -1, -1)), Sb),
    )
    ui = 0
    for d, seq, SS in chains:
        cur = None
        for n, c in enumerate(seq):
            if n >= 2:
                P.op("act", lambda e, c=c, d=d, cur=cur: e.activation(out=sbf[:, c, d, :], in_=cur[0][:], func=AF.Copy), [cur[1]], [r_sbf[c]])
            if n == len(seq) - 1:
                break
            pb, r_pb = ub[ui % 2]
            sl = (ui // 2) % 4
            ui += 1
            P.op("pe", lambda e, pb=pb, sl=sl, c=c, d=d: e.matmul(pb[:, sl * 128:(sl + 1) * 128], lhsT=kd[:, c, d, :], rhs=vt[:, c, :], start=True, stop=True),
                 [r_kdd[c], r_vt[c]], [r_pb])
            nxt = SS[n % 2]
            if cur is None:
                P.op("dve", lambda e, pb=pb, sl=sl, nxt=nxt: e.tensor_copy(out=nxt[0][:], in_=pb[:, sl * 128:(sl + 1) * 128]), [r_pb], [nxt[1]])
            else:
                P.op("dve", lambda e, pb=pb, sl=sl, nxt=nxt, cur=cur, d=d: e.scalar_tensor_tensor(out=nxt[0][:], in0=cur[0][:], scalar=kdc[:, 2 + d:3 + d], in1=pb[:, sl * 128:(sl + 1) * 128],
                                                                                              op0=ALU.mult, op1=ALU.add), [r_pb, cur[1], r_kd], [nxt[1]])
            cur = nxt
    attm = [(ph.sb([128, 128], BF16), R()) for _ in range(3)]
    qfb = [(ph.sb([128, 2, 128], BF16), R()) for _ in range(3)]
    on = [(ph.sb([128, 128]), R()) for _ in range(3)]
    yr = ph.sb([128, NT, 128], BF16); r_yr = [R() for _ in range(NT)]
    st = [(ph.sb([128, 16]), R()) for _ in range(3)]
    r_cat = R()

    def chunk_unit(c):
        am, r_am = attm[c % 3]
        qf, r_qf = qfb[c % 3]
        pb, r_pb = bk[c % 3]
        o_sl = pb[:, 0:128]
        a_sl = pb[:, 128:256]
        r_a = r_pb
        s_, r_s = st[c % 3]
        o_, r_o = on[c % 3]
        pt, r_pt = ptb[(c // 4) % 2]

        def sa():
            P.op("pe", lambda e: e.matmul(a_sl, lhsT=kT[:, c * 128:(c + 1) * 128], rhs=qT[:, c * 128:(c + 1) * 128], start=True, stop=True), [r_kT[c], r_qT[c]], [r_a])
            P.op("dve", lambda e: e.tensor_tensor(out=am[:], in0=a_sl, in1=dm[:], op=ALU.mult), [r_a, r_dm], [r_am])
            P.op("pool", lambda e: e.tensor_tensor(out=qf[:], in0=qT[:, c * 128:(c + 1) * 128].unsqueeze(1).to_broadcast([128, 2, 128]), in1=qd[:], op=ALU.mult), [r_qT[c], r_qd], [r_qf])

        def sb_():
            P.op("pe", lambda e: e.matmul(o_sl, lhsT=am[:], rhs=vt[:, c, :], start=True, stop=False), [r_am, r_vt[c]], [r_pb])
            P.op("pe", lambda e: e.matmul(o_sl, lhsT=qf[:, 0, :], rhs=sbf[:, c, 0, :], start=False, stop=False), [r_qf, r_sbf[c]], [r_pb])
            P.op("pe", lambda e: e.matmul(o_sl, lhsT=qf[:, 1, :], rhs=sbf[:, c, 1, :], start=False, stop=True), [r_qf, r_sbf[c]], [r_pb])
            P.op("dve", lambda e: e.bn_stats(out=s_[:, 0:6], in_=o_sl), [r_pb], [r_s])
            P.op("dve", lambda e: e.bn_aggr(out=s_[:, 6:8], in_=s_[:, 0:6]), [r_s], [r_s])
            P.op("dve", lambda e: e.tensor_scalar(out=s_[:, 8:9], in0=s_[:, 7:8], scalar1=EPS, scalar2=None, op0=ALU.add), [r_s], [r_s])
            P.op("act", lambda e: e.activation(out=s_[:, 8:9], in_=s_[:, 8:9], func=AF.Sqrt), [r_s], [r_s])
            P.op("dve", lambda e: e.reciprocal(out=s_[:, 8:9], in_=s_[:, 8:9]), [r_s], [r_s])
            P.op("dve", lambda e: e.scalar_tensor_tensor(out=s_[:, 9:10], in0=s_[:, 6:7], scalar=-1.0, in1=s_[:, 8:9], op0=ALU.mult, op1=ALU.mult), [r_s], [r_s])
            P.op("act", lambda e: e.activation(out=o_[:], in_=o_sl, func=AF.Identity, scale=s_[:, 8:9], bias=s_[:, 9:10]), [r_pb, r_s], [r_o])
            P.op("pool", lambda e: e.tensor_tensor(out=yr[:, c, :], in0=o_[:], in1=gs[:, c, :], op=ALU.mult), [r_o, r_gs[c]], [r_yr[c]])

        def sc():
            P.op("pe", lambda e: e.transpose(out=pt[:, (c % 4) * 128:(c % 4 + 1) * 128], in_=yr[:, c, :], identity=identb[:]), [r_yr[c], r_idb], [r_pt])
            if c % 4 == 3:
                c0 = c - 3
                P.op("act", lambda e: e.activation(out=catT[:, h, c0 * 128:(c0 + 4) * 128], in_=pt[:, 0:512], func=AF.Copy), [r_pt], [r_cat])

        return [sa, sb_, sc]

    emit_pipelined([chunk_unit(c) for c in range(NT)])
    ph.finish()


def na_variants():
    out = {}
    for m in range(16):
        ts = min(max(m - 2, 0), 11)
        key = []
        for u in range(2):
            r = 2 * m + u
            r0 = min(max(r - 4, 0), 24)
            key.append((r0 - 2 * ts, r0 - r + 7))
        out.setdefault(tuple(key), []).append(m)
    return out


def na_unit(P, it, m, u, pu, bias_m, bk, L, Pe, PT, st, ptb, ob, qT, kT, nv, yn, allq, allk, allv, r_yn, identb, r_idb):
    ts = min(max(m - 2, 0), 11)
    bt, r_b = bias_m
    btf = bt[:].rearrange("p a j -> p (a j)")
    (pA, r_pA), (pB, r_pB) = bk[(it % 2)], bk[2 + (it % 2)]
    lq = qT[pu, m * 128:(m + 1) * 128]
    Lt, r_L = L[it % 3]
    s_, r_s = st[it % 3]
    Pt, r_P = Pe[it % 3]
    pt, r_pt = ptb[it % 2]
    PTt, r_PT = PT[it % 3]
    po, r_po = ob[it % 2]
    osl = po[:, 0:64]

    def s1():
        P.op("pe", lambda e: e.matmul(pA[:, 0:512], lhsT=lq, rhs=kT[pu, ts * 128:ts * 128 + 512], start=True, stop=True), allq + allk, [r_pA])
        P.op("pe", lambda e: e.matmul(pB[:, 0:128], lhsT=lq, rhs=kT[pu, ts * 128 + 512:ts * 128 + 640], start=True, stop=True), allq + allk, [r_pB])
        P.op("pe", lambda e: e.matmul(pB[:, 128:384], lhsT=lq, rhs=kT[pu, NL:NL + NCX], start=True, stop=True), allq + allk, [r_pB])
        P.op("dve", lambda e: e.tensor_tensor(out=Lt[:, 0:512], in0=pA[:, 0:512], in1=btf[:, 0:512], op=ALU.add), [r_pA, r_b], [r_L])
        P.op("dve", lambda e: e.tensor_tensor(out=Lt[:, 512:640], in0=pB[:, 0:128], in1=btf[:, 512:640], op=ALU.add), [r_pB, r_b], [r_L])
        P.op("dve", lambda e: e.tensor_copy(out=Lt[:, 640:896], in_=pB[:, 128:384]), [r_pB], [r_L])
        P.op("dve", lambda e: e.reduce_max(out=s_[:, 0:1], in_=Lt[:], axis=AX.X), [r_L], [r_s])
        P.op("dve", lambda e: e.tensor_scalar(out=s_[:, 1:2], in0=s_[:, 0:1], scalar1=-1.0, scalar2=None, op0=ALU.mult), [r_s], [r_s])
        P.op("act", lambda e: e.activation(out=Pt[:], in_=Lt[:], func=AF.Exp, bias=s_[:, 1:2], accum_out=s_[:, 2:3]), [r_L, r_s], [r_P, r_s])

    def s2():
        for c in range(7):
            P.op("pe", lambda e, c=c: e.transpose(out=pt[:, c * 128:(c + 1) * 128], in_=Pt[:, c * 128:(c + 1) * 128], identity=identb[:]), [r_P, r_idb], [r_pt])
        P.op("act", lambda e: e.activation(out=PTt[:].rearrange("p c n -> p (c n)"), in_=pt[:, 0:896], func=AF.Copy), [r_pt], [r_PT])
        P.op("dve", lambda e: e.reciprocal(out=s_[:, 3:4], in_=s_[:, 2:3]), [r_s], [r_s])

    def s3():
        for c in range(7):
            tile_i = ts + c if c < 5 else 16 + (c - 5)
            P.op("pe", lambda e, c=c, tile_i=tile_i: e.matmul(osl, lhsT=PTt[:, c, :], rhs=nv[:, tile_i, pu], start=(c == 0), stop=(c == 6)), [r_PT] + allv, [r_po])
        P.op("act", lambda e: e.activation(out=yn[:, m, pu], in_=osl, func=AF.Identity, scale=s_[:, 3:4]), [r_po, r_s], [r_yn[m]])

    return [s1, s2, s3]


def phase_A_na_all(kb, b, xmT, catT, identb):
    nc = kb.nc
    T = kb.t
    ph = Phase(kb, "AN")
    P = ph.P
    r_xm = R(); r_idb = R(); r_cat = R()
    NTA = NT + 2
    wst = ph.sb([128, 8, 3, 128]); r_wst = R()
    wbs = [(ph.sb([128, 8, 384], BF16), R()) for _ in range(2)]
    qTs = [(ph.sb([128, NL], BF16), [R() for _ in range(4)]) for _ in range(2)]
    kTs = [(ph.sb([128, NL + NCX], BF16), [R() for _ in range(5)]) for _ in range(2)]
    nvs = [(ph.sb([128, NTA, 128], BF16), [R() for _ in range(5)]) for _ in range(2)]
    yns = [(ph.sb([128, NT, 128], BF16), [R() for _ in range(NT)]) for _ in range(2)]
    bk = ph.banks(4)
    ptb = [(ph.ps([128, 1024], BF16), R()) for _ in range(2)]
    ob = [(ph.ps([128, 512], F32), R()) for _ in range(2)]
    cb = (bk[0][0][:].bitcast(BF16), bk[0][1])
    L = [(ph.sb([128, 896]), R()) for _ in range(3)]
    Pe = [(ph.sb([128, 896], BF16), R()) for _ in range(3)]
    PT = [(ph.sb([128, 7, 128], BF16), R()) for _ in range(3)]
    st = [(ph.sb([128, 4]), R()) for _ in range(3)]
    variants = na_variants()
    tbs = [(ph.sb([128, 15, 64]), R()) for _ in range(2)]
    bsets = [[(ph.sb([128, 10, 64]), R()) for _ in range(len(variants))] for _ in range(2)]
    bi_box = [0]

    def proj_parts(hp):
        s_ = hp % 2
        wb, r_wb = wbs[s_]
        qT, r_qT = qTs[s_]
        kT, r_kT = kTs[s_]
        nv, r_nv = nvs[s_]
        parts = []

        def wload():
            for j, off in enumerate((OFF_NQ, OFF_NK, OFF_NV)):
                ld(P, wst[:, :, j, :], T["w_in"][:, off + hp * 128:off + (hp + 1) * 128].rearrange("(k p) n -> p k n", p=128), [], [r_wst])
            for k in range(8):
                P.op("pool", lambda e, k=k: e.tensor_copy(out=wb[:, k, :], in_=wst[:, k, :, :].rearrange("p a n -> p (a n)")), [r_wst], [r_wb])
        parts.append(wload)
        for g in range(5):
            def part(g=g):
                n = 512 if g < 4 else 256
                o = g * 512
                if g < 4:
                    pb, r_pb = bk[bi_box[0] % 4]; bi_box[0] += 1
                    for k in range(8):
                        P.op("pe", lambda e, k=k: e.matmul(pb[:, 0:n], lhsT=wb[:, k, 0:128], rhs=xmT[:, k, o:o + n], start=(k == 0), stop=(k == 7)), [r_wb, r_xm], [r_pb])
                    P.op("act", lambda e: e.activation(out=qT[:, o:o + n], in_=pb[:, 0:n], func=AF.Identity, scale=0.125), [r_pb], [r_qT[g]])
                pb2, r_pb2 = bk[bi_box[0] % 4]; bi_box[0] += 1
                for k in range(8):
                    P.op("pe", lambda e, k=k: e.matmul(pb2[:, 0:n], lhsT=wb[:, k, 128:256], rhs=xmT[:, k, o:o + n], start=(k == 0), stop=(k == 7)), [r_wb, r_xm], [r_pb2])
                P.op("act", lambda e: e.activation(out=kT[:, o:o + n], in_=pb2[:, 0:n], func=AF.Copy), [r_pb2], [r_kT[g]])
                pb3, r_pb3 = bk[bi_box[0] % 4]; bi_box[0] += 1
                ntl = n // 128
                for tt in range(ntl):
                    for k in range(8):
                        P.op("pe", lambda e, k=k, tt=tt: e.matmul(pb3[:, tt * 128:(tt + 1) * 128], lhsT=xmT[:, k, o + tt * 128:o + (tt + 1) * 128], rhs=wb[:, k, 256:384],
                                                                 start=(k == 0), stop=(k == 7)), [r_wb, r_xm], [r_pb3])
                P.op("act", lambda e: e.activation(out=nv[:, g * 4:g * 4 + ntl, :].rearrange("p t n -> p (t n)"), in_=pb3[:, 0:ntl * 128], func=AF.Copy), [r_pb3], [r_nv[g]])
            parts.append(part)
        return parts

    def bias_build(h):
        tb, r_tb = tbs[h % 2]
        ld(P, tb[:], T["TOEP"][h], [], [r_tb])
        bias = {}
        for vi, (key, ms) in enumerate(variants.items()):
            bt, r_b = bsets[h % 2][vi]
            P.op("pool", lambda e, bt=bt: e.memset(bt[:], NEG), [], [r_b])
            for uu in range(2):
                i_lo, a_lo = key[uu]
                pp = slice(64 * uu, 64 * uu + 64)
                P.op("pool", lambda e, bt=bt, tb=tb, pp=pp, i_lo=i_lo, a_lo=a_lo: e.tensor_copy(out=bt[pp, i_lo:i_lo + 8, :], in_=tb[pp, a_lo:a_lo + 8, :]), [r_tb, r_b], [r_b])
            for m in ms:
                bias[m] = (bt, r_b)
        return bias

    for part in proj_parts(0):
        part()
    it = 0
    for hp in range(4):
        s_ = hp % 2
        qT, r_qT = qTs[s_]
        kT, r_kT = kTs[s_]
        nv, r_nv = nvs[s_]
        yn, r_yn = yns[s_]
        units = []
        for u in range(2):
            h = 2 * hp + u
            pu = slice(64 * u, 64 * u + 64)
            bias = bias_build(h)
            for m in range(16):
                units.append(na_unit(P, it, m, u, pu, bias[m], bk, L, Pe, PT, st, ptb, ob, qT, kT, nv, yn, r_qT, r_kT, r_nv, r_yn, identb, r_idb))
                it += 1
        nxt = proj_parts(hp + 1) if hp < 3 else []
        inject = {6 + 4 * j: p for j, p in enumerate(nxt)}
        n = len(units)
        S = 3
        for step in range(n + S - 1):
            for stg in range(S):
                i = step - stg
                if 0 <= i < n:
                    units[i][stg]()
            if step in inject:
                inject[step]()
        pc, r_pc = cb
        for m in range(16):
            P.op("pe", lambda e, m=m, yn=yn: e.transpose(out=pc[:, (m % 8) * 128:(m % 8 + 1) * 128], in_=yn[:, m, :], identity=identb[:]), [r_yn[m], r_idb], [r_pc])
            if m % 8 == 7:
                m0 = m - 7
                P.op("act", lambda e, m0=m0, hp=hp: e.activation(out=catT[:, 4 + hp, m0 * 128:(m0 + 8) * 128], in_=pc[:, 0:1024], func=AF.Copy), [r_pc], [r_cat])
    ph.finish()


def phase_E(kb, l, b, src, dst):
    nc = kb.nc
    T = kb.t
    pers = ExitStack()
    Phase.cnt += 1
    acc = pers.enter_context(nc.sbuf_tensor(f"E_acc{Phase.cnt}", [128, NT, D], F32))
    x2T = pers.enter_context(nc.sbuf_tensor(f"E_x2T{Phase.cnt}", [128, 8, NL], BF16))
    comb = pers.enter_context(nc.sbuf_tensor(f"E_comb{Phase.cnt}", [128, NT, 32], F32))

    ph = Phase(kb, "Ea")
    P = ph.P
    identf = ph.sb([128, 128]); r_id = R()
    ld(P, identf[:], T["ident"], [], [r_id])
    mT, r_mT = load_mT(ph, l)
    sc1, sh, r_mod = make_mod_cols(ph, mT, r_mT, 3, 4, b)
    wr = ph.sb([128, 8, 36]); r_wr = R()
    ld(P, wr[:, :, 0:4], T["w_r1"][l].rearrange("(k p) n -> p k n", p=128), [], [r_wr])
    for g in range(4):
        ld(P, wr[:, :, 4 + g * 8:12 + g * 8], T["w_r2"][l, g].rearrange("(k p) n -> p k n", p=128), [], [r_wr])
    brow = ph.sb([128, 36]); r_br = R()
    ld(P, brow[:, 0:4], T["b_r1"][l].partition_broadcast(128), [], [r_br])
    ld(P, brow[:, 4:36], T["b_r2"][l].rearrange("g e -> (g e)").partition_broadcast(128), [], [r_br])
    bk = ph.banks(6)
    rb = ph.banks(2)
    xin = [(ph.sb([128, 4, D]), R()) for _ in range(2)]
    xf = [(ph.sb([128, 8, 512]), [R() for _ in range(8)]) for _ in range(2)]
    logit = ph.sb([128, NT, 36]); r_lg = R()
    r_x = [R() for _ in range(4)]

    def extra(g, k, pb, r_pb, nt):
        xt, r_xf = xf[g % 2]
        if k < 8:
            P.op("dve", lambda e: e.tensor_scalar(out=xt[:, k, :], in0=pb[:, 0:512], scalar1=sc1[:, k:k + 1], scalar2=sh[:, k:k + 1], op0=ALU.mult, op1=ALU.add), [r_pb, r_mod], [r_xf[k]])
            return xt, r_xf[k]
        pr, r_pr = rb[g % 2]
        for tt in range(4):
            for kk in range(8):
                P.op("pe", lambda e, tt=tt, kk=kk: e.matmul(pr[:, tt * 36:(tt + 1) * 36], lhsT=xt[:, kk, tt * 128:(tt + 1) * 128], rhs=wr[:, kk, :], start=(kk == 0), stop=(kk == 7)),
                     r_xf + [r_wr], [r_pr])
        P.op("dve", lambda e: e.tensor_tensor(out=logit[:, g * 4:(g + 1) * 4, :], in0=pr[:, 0:144].rearrange("p (t n) -> p t n", t=4),
                                              in1=brow[:].unsqueeze(1).to_broadcast([128, 4, 36]), op=ALU.add), [r_pr, r_br], [r_lg])

    build_xT(ph, src[b], 16, x2T, 0, sc1, sh, r_mod, identf, r_id, bk, r_x, xin, extra=extra, all_act=True)
    def dv(fn, reads, writes):
        P.op("dve", fn, reads, writes)
    s4 = ph.sb([128, NT, 4]); mg = ph.sb([128, NT, 4]); r_a = R()
    s1 = ph.sb([128, NT, 8]); r_s1 = R()
    lg4 = logit[:, :, 0:4]
    le = logit[:, :, 4:36].rearrange("p t (g e) -> p t g e", g=4)
    dv(lambda e: e.tensor_reduce(out=s1[:, :, 0], in_=lg4, axis=AX.X, op=ALU.max), [r_lg], [r_s1])
    dv(lambda e: e.tensor_tensor(out=mg[:], in0=lg4, in1=s1[:, :, 0:1].to_broadcast([128, NT, 4]), op=ALU.is_equal), [r_lg, r_s1], [r_a])
    dv(lambda e: e.tensor_tensor(out=s4[:], in0=lg4, in1=s1[:, :, 0:1].to_broadcast([128, NT, 4]), op=ALU.subtract), [r_lg, r_s1], [r_a])
    P.op("act", lambda e: e.activation(out=s4[:], in_=s4[:], func=AF.Exp), [r_a], [r_a])
    dv(lambda e: e.tensor_reduce(out=s1[:, :, 1], in_=s4[:], axis=AX.X, op=ALU.add), [r_a], [r_s1])
    dv(lambda e: e.reciprocal(out=s1[:, :, 2], in_=s1[:, :, 1]), [r_s1], [r_s1])
    t48 = ph.sb([128, NT, 4, 8]); r_t48 = R()
    dv(lambda e: e.tensor_tensor(out=t48[:], in0=le, in1=mg[:].unsqueeze(3).to_broadcast([128, NT, 4, 8]), op=ALU.mult), [r_lg, r_a], [r_t48])
    ls = ph.sb([128, NT, 8]); l2 = ph.sb([128, NT, 8]); k1 = ph.sb([128, NT, 8]); k2 = ph.sb([128, NT, 8]); r_ls = R()
    dv(lambda e: e.tensor_reduce(out=ls[:], in_=t48[:].rearrange("p t g e -> p t e g"), axis=AX.X, op=ALU.add), [r_t48], [r_ls])
    dv(lambda e: e.tensor_reduce(out=s1[:, :, 3], in_=ls[:], axis=AX.X, op=ALU.max), [r_ls], [r_s1])
    dv(lambda e: e.tensor_tensor(out=k1[:], in0=ls[:], in1=s1[:, :, 3:4].to_broadcast([128, NT, 8]), op=ALU.is_equal), [r_ls, r_s1], [r_ls])
    dv(lambda e: e.scalar_tensor_tensor(out=l2[:], in0=k1[:], scalar=-1e30, in1=ls[:], op0=ALU.mult, op1=ALU.add), [r_ls], [r_ls])
    dv(lambda e: e.tensor_reduce(out=s1[:, :, 4], in_=l2[:], axis=AX.X, op=ALU.max), [r_ls], [r_s1])
    dv(lambda e: e.tensor_tensor(out=k2[:], in0=l2[:], in1=s1[:, :, 4:5].to_broadcast([128, NT, 8]), op=ALU.is_equal), [r_ls, r_s1], [r_ls])
    dv(lambda e: e.tensor_tensor(out=s1[:, :, 5], in0=s1[:, :, 4], in1=s1[:, :, 3], op=ALU.subtract), [r_s1], [r_s1])
    P.op("act", lambda e: e.activation(out=s1[:, :, 5], in_=s1[:, :, 5], func=AF.Exp), [r_s1], [r_s1])
    dv(lambda e: e.tensor_scalar(out=s1[:, :, 6], in0=s1[:, :, 5], scalar1=1.0, scalar2=None, op0=ALU.add), [r_s1], [r_s1])
    dv(lambda e: e.reciprocal(out=s1[:, :, 6], in_=s1[:, :, 6]), [r_s1], [r_s1])
    dv(lambda e: e.tensor_tensor(out=s1[:, :, 6], in0=s1[:, :, 6], in1=s1[:, :, 2], op=ALU.mult), [r_s1], [r_s1])
    dv(lambda e: e.tensor_tensor(out=s1[:, :, 7], in0=s1[:, :, 6], in1=s1[:, :, 5], op=ALU.mult), [r_s1], [r_s1])
    dv(lambda e: e.tensor_tensor(out=k1[:], in0=k1[:], in1=s1[:, :, 6:7].to_broadcast([128, NT, 8]), op=ALU.mult), [r_ls, r_s1], [r_ls])
    dv(lambda e: e.tensor_tensor(out=k2[:], in0=k2[:], in1=s1[:, :, 7:8].to_broadcast([128, NT, 8]), op=ALU.mult), [r_ls, r_s1], [r_ls])
    dv(lambda e: e.tensor_tensor(out=k1[:], in0=k1[:], in1=k2[:], op=ALU.add), [r_ls], [r_ls])
    r_comb = R()
    dv(lambda e: e.tensor_tensor(out=comb[:].rearrange("p t (g e) -> p t g e", g=4), in0=mg[:].unsqueeze(3).to_broadcast([128, NT, 4, 8]),
                                 in1=k1[:].unsqueeze(2).to_broadcast([128, NT, 4, 8]), op=ALU.mult), [r_a, r_ls], [r_comb])
    if kb.dbg:
        ld(P, T["dbg_comb"][b], comb[:], [r_comb], [])
    ph.finish()

    if getattr(kb, "stop", None) == "Ea":
        pers.close()
        return
    ph = Phase(kb, "Eb")
    P = ph.P
    r_x2 = R(); r_comb = R()
    stg = [(ph.sb([128, 2048]), R()) for _ in range(4)]
    wbf = [dict(g=(ph.sb([128, 8, 512], BF16), R()), u=(ph.sb([128, 8, 512], BF16), R()), d=(ph.sb([128, 4, D], BF16), R())) for _ in range(2)]
    hT = [(ph.sb([128, 4, 512], BF16), R()) for _ in range(2)]
    sg = [(ph.sb([128, 512], BF16), R()) for _ in range(2)]
    gb = ph.banks(2); ub = ph.banks(2); yb = ph.banks(4)
    r_acc = [R() for _ in range(NT)]
    si = 0
    gi_box = [0]
    yi_box = [0]
    pending_down = [None]
    for ex in range(32):
        w = wbf[ex % 2]
        for name, srcw in (("g", T["w_gate"][l, ex]), ("u", T["w_up"][l, ex]), ("d", T["w_down"][l, ex])):
            wt, r_wt = w[name]
            for piece in range(2):
                st_, r_st = stg[si % 4]; si += 1
                if name == "d":
                    ld(P, st_[:].rearrange("p (k n) -> p k n", k=2), srcw[piece * 256:(piece + 1) * 256, :].rearrange("(k p) n -> p k n", p=128), [], [r_st])
                    P.op("pool", lambda e, wt=wt, st_=st_, piece=piece: e.tensor_copy(out=wt[:, piece * 2:(piece + 1) * 2, :].rearrange("p k n -> p (k n)"), in_=st_[:]), [r_st], [r_wt])
                else:
                    ld(P, st_[:].rearrange("p (k n) -> p k n", k=4), srcw[piece * 512:(piece + 1) * 512, :].rearrange("(k p) n -> p k n", p=128), [], [r_st])
                    P.op("pool", lambda e, wt=wt, st_=st_, piece=piece: e.tensor_copy(out=wt[:, piece * 4:(piece + 1) * 4, :].rearrange("p k n -> p (k n)"), in_=st_[:]), [r_st], [r_wt])
        (wg, r_wg), (wu, r_wu), (wd, r_wd) = w["g"], w["u"], w["d"]
        for tg in range(4):
            h_, r_h = hT[(ex * 4 + tg) % 2]

            def up(ex=ex, tg=tg, h_=h_, r_h=r_h, wg=wg, r_wg=r_wg, wu=wu, r_wu=r_wu):
                for hc in range(4):
                    gi = gi_box[0]
                    gi_box[0] += 1
                    (pg, r_pg), (pu_, r_pu) = gb[gi % 2], ub[gi % 2]
                    s_, r_s = sg[gi % 2]
                    for k in range(8):
                        P.op("pe", lambda e, k=k, pg=pg, hc=hc: e.matmul(pg[:], lhsT=wg[:, k, hc * 128:(hc + 1) * 128], rhs=x2T[:, k, tg * 512:(tg + 1) * 512], start=(k == 0), stop=(k == 7)),
                             [r_wg, r_x2], [r_pg])
                    for k in range(8):
                        P.op("pe", lambda e, k=k, pu_=pu_, hc=hc: e.matmul(pu_[:], lhsT=wu[:, k, hc * 128:(hc + 1) * 128], rhs=x2T[:, k, tg * 512:(tg + 1) * 512], start=(k == 0), stop=(k == 7)),
                             [r_wu, r_x2], [r_pu])
                    P.op("act", lambda e, s_=s_, pg=pg: e.activation(out=s_[:], in_=pg[:], func=AF.Silu), [r_pg], [r_s])
                    P.op("dve", lambda e, s_=s_, pu_=pu_, hc=hc: e.tensor_tensor(out=h_[:, hc, :], in0=pu_[:], in1=s_[:], op=ALU.mult), [r_pu, r_s], [r_h])

            def down(ex=ex, tg=tg, h_=h_, r_h=r_h, wd=wd, r_wd=r_wd):
                for tt in range(4):
                    t = tg * 4 + tt
                    for half in range(2):
                        yi = yi_box[0]
                        yi_box[0] += 1
                        py, r_py = yb[yi % 4]
                        for k in range(4):
                            P.op("pe", lambda e, k=k, py=py, tt=tt, half=half: e.matmul(py[:], lhsT=h_[:, k, tt * 128:(tt + 1) * 128], rhs=wd[:, k, half * 512:(half + 1) * 512],
                                                                                 start=(k == 0), stop=(k == 3)), [r_h, r_wd], [r_py])
                        a_sl = acc[:, t, half * 512:(half + 1) * 512]
                        cw = comb[:, t, ex:ex + 1]
                        if ex == 0:
                            P.op("dve", lambda e, a_sl=a_sl, py=py, cw=cw: e.tensor_scalar(out=a_sl, in0=py[:], scalar1=cw, scalar2=None, op0=ALU.mult), [r_py, r_comb], [r_acc[t]])
                        else:
                            P.op("dve", lambda e, a_sl=a_sl, py=py, cw=cw: e.scalar_tensor_tensor(out=a_sl, in0=py[:], scalar=cw, in1=a_sl, op0=ALU.mult, op1=ALU.add), [r_py, r_comb, r_acc[t]], [r_acc[t]])

            up()
            if pending_down[0] is not None:
                pending_down[0]()
            pending_down[0] = down
    pending_down[0]()
    ph.finish()

    if getattr(kb, "stop", None) == "Eb":
        pers.close()
        return
    ph = Phase(kb, "Ec")
    P = ph.P
    gate, r_gate = load_bcast(ph, T["MV"][l, b, 5 * D:6 * D])
    lng, r_lng = load_bcast(ph, T["ln_g"][l, 1])
    lnb, r_lnb = load_bcast(ph, T["ln_b"][l, 1])
    bufs = pn_bufs(ph)
    r_acc2 = R()
    emit_pipelined([post_norm(ph, [acc[:, t, :]], [r_acc2], src[b, t * 128:(t + 1) * 128, :], gate, r_gate, lng, r_lng, lnb, r_lnb,
                              dst[b, t * 128:(t + 1) * 128, :], bufs, t) for t in range(NT)])
    ph.finish()
    pers.close()


def phase_P(kb, b, src, dst):
    nc = kb.nc
    T = kb.t
    l = 1
    NP_ = NL + 16
    pers = ExitStack()
    Phase.cnt += 1
    zT = pers.enter_context(nc.sbuf_tensor(f"P_zT{Phase.cnt}", [128, 8, NL], BF16))
    ph = Phase(kb, "Pa")
    P = ph.P
    identf = ph.sb([128, 128]); r_id = R()
    ld(P, identf[:], T["ident"], [], [r_id])
    mT, r_mT = load_mT(ph, l)
    sc1, sh, r_mod = make_mod_cols(ph, mT, r_mT, 0, 1, b)
    hm = ph.sb([128, 8, NP_]); r_hm = [R() for _ in range(4)]
    r_pad = R()
    P.op("pool", lambda e: e.memset(hm[:, :, 0:8], 0.0), [], [r_pad])
    P.op("pool", lambda e: e.memset(hm[:, :, NL + 8:NL + 16], 0.0), [], [r_pad])
    bk = ph.banks(6)
    xin = [(ph.sb([128, 4, D]), R()) for _ in range(2)]
    build_xT(ph, src[b], 16, hm, 8, sc1, sh, r_mod, identf, r_id, bk, r_hm, xin)
    invc = ph.sb([128, 4, 16]); r_ic = R()
    ld(P, invc[:], T["invc"], [], [r_ic])
    A = [(ph.sb([128, NP_]), R()) for _ in range(2)]
    Bf = [(ph.sb([128, NP_]), R()) for _ in range(2)]
    zb = [(ph.sb([128, 16]), R()) for _ in range(2)]
    r_z = R()
    allhm = r_hm + [r_pad]
    for c in range(8):
        g = c // 2
        w = (2, 4, 8, 16)[g]
        x = hm[:, c, :]
        eng = "dve" if c % 2 == 0 else "pool"
        (a, r_a), (bb, r_b) = A[c % 2], Bf[c % 2]
        P.op(eng, lambda e, a=a, x=x: e.tensor_tensor(out=a[:, 1:NL + 15], in0=x[:, 0:NL + 14], in1=x[:, 1:NL + 15], op=ALU.add), allhm, [r_a])
        cur, r_cur = a, r_a
        if g >= 1:
            P.op(eng, lambda e, a=a, bb=bb: e.tensor_tensor(out=bb[:, 2:NL + 14], in0=a[:, 1:NL + 13], in1=a[:, 3:NL + 15], op=ALU.add), [r_a], [r_b])
            cur, r_cur = bb, r_b
        if g >= 2:
            P.op(eng, lambda e, a=a, bb=bb: e.tensor_tensor(out=a[:, 4:NL + 12], in0=bb[:, 2:NL + 10], in1=bb[:, 6:NL + 14], op=ALU.add), [r_b], [r_a])
            cur, r_cur = a, r_a
        if g >= 3:
            P.op(eng, lambda e, a=a, bb=bb: e.tensor_tensor(out=bb[:, 8:NL + 8], in0=a[:, 4:NL + 4], in1=a[:, 12:NL + 12], op=ALU.add), [r_a], [r_b])
            cur, r_cur = bb, r_b
        P.op("dve", lambda e, cur=cur, x=x, c=c, w=w: e.scalar_tensor_tensor(out=zT[:, c, :], in0=cur[:, 8:NL + 8], scalar=1.0 / w, in1=x[:, 8:NL + 8], op0=ALU.mult, op1=ALU.subtract),
             [r_cur] + allhm, [r_z])
        z_, r_zb = zb[c % 2]
        for side, (o_src, o_dst) in enumerate(((8, 0), (NL, NL - 8))):
            P.op(eng, lambda e, cur=cur, z_=z_, g=g, side=side, o_src=o_src: e.tensor_tensor(out=z_[:, side * 8:(side + 1) * 8], in0=cur[:, o_src:o_src + 8], in1=invc[:, g, side * 8:(side + 1) * 8], op=ALU.mult),
                 [r_cur, r_ic], [r_zb])
            P.op(eng, lambda e, z_=z_, x=x, c=c, side=side, o_src=o_src, o_dst=o_dst: e.tensor_tensor(out=zT[:, c, o_dst:o_dst + 8], in0=z_[:, side * 8:(side + 1) * 8], in1=x[:, o_src:o_src + 8], op=ALU.subtract),
                 [r_zb] + allhm, [r_z])
    ph.finish()
    ph = Phase(kb, "Pb")
    P = ph.P
    pws = ph.sb([128, 4, 2, 256]); r_pws = R()
    for g in range(4):
        ld(P, pws[:, g, :, :], T["pool_w"][g].rearrange("(k p) e -> p k e", p=128), [], [r_pws])
    pw = ph.sb([128, 4, 2, 256], BF16); r_pw = R()
    P.op("pool", lambda e: e.tensor_copy(out=pw[:], in_=pws[:]), [r_pws], [r_pw])
    gate, r_gate = load_bcast(ph, T["MV"][l, b, 2 * D:3 * D])
    psc, r_psc = load_bcast(ph, T["pool_scale"])
    P.op("dve", lambda e: e.tensor_tensor(out=gate[:], in0=gate[:], in1=psc[:], op=ALU.mult), [r_gate, r_psc], [r_gate])
    lng, r_lng = load_bcast(ph, T["ln_g"][l, 0])
    lnb, r_lnb = load_bcast(ph, T["ln_b"][l, 0])
    bufs = pn_bufs(ph)
    bk = ph.banks(4)
    r_z = R()
    units = []
    for t in range(NT):
        (p0, r0), (p1, r1) = bk[(t % 2) * 2], bk[(t % 2) * 2 + 1]

        def pre(t=t, p0=p0, r0=r0, p1=p1, r1=r1):
            for g in range(4):
                pb, r_pb = (p0, r0) if g < 2 else (p1, r1)
                for kk in range(2):
                    P.op("pe", lambda e, pb=pb, g=g, kk=kk: e.matmul(pb[:, (g % 2) * 256:(g % 2 + 1) * 256], lhsT=zT[:, 2 * g + kk, t * 128:(t + 1) * 128], rhs=pw[:, g, kk, :],
                                                                  start=(kk == 0), stop=(kk == 1)), [r_z, r_pw], [r_pb])
        units.append(post_norm(ph, [p0[:], p1[:]], [r0, r1], src[b, t * 128:(t + 1) * 128, :], gate, r_gate, lng, r_lng, lnb, r_lnb,
                               dst[b, t * 128:(t + 1) * 128, :], bufs, t, pre=pre))
    emit_pipelined(units)
    ph.finish()
    pers.close()


IN_SHAPES = {
    "x": [2, NL, D], "ctx": [2, NCX, D], "c": [2, D], "c_ctx": [D],
    "w_mod": [2, D, 6 * D], "b_mod": [2, 6 * D], "ln_g": [2, 2, D], "ln_b": [2, 2, D],
    "w_in": [D, 3584], "w_out": [D, D], "log_decay": [2, 4], "rpb": [8, 15, 31],
    "pool_w": [4, 256, 256], "pool_scale": [D],
    "w_r1": [2, D, 4], "b_r1": [2, 4], "w_r2": [2, 4, D, 8], "b_r2": [2, 4, 8],
    "w_gate": [2, 32, D, 512], "w_up": [2, 32, D, 512], "w_down": [2, 32, 512, D],
    "ident": [128, 128], "rope": [128, NT, 2, 64], "rtab": [128, 4, 128], "ktab": [128, 4],
    "colmask": [128, 64], "invc": [128, 4, 16],
}
SCRATCH = {
    "MV": [2, 3, 6 * D], "MT": [2, 128, 48, 3], "RP": [8, 15, 128], "TOEP": [8, 128, 15, 64],
    "H1": [2, NL, D], "H2": [2, NL, D], "H3": [2, NL, D],
}


class KB:
    pass


def build(phases=("M", "A", "E0", "P", "E1"), ext_in=(), ext_out=(), dbg=False, nb=2, stop=None):
    nc = bass.Bass("TRN2", target_bir_lowering=False)
    kb = KB()
    kb.stop = stop
    kb.nc = nc
    kb.dbg = dbg
    kb.t = {}
    for k, shp in IN_SHAPES.items():
        kb.t[k] = nc.dram_tensor(k, list(shp), F32, kind="ExternalInput").ap()
    for k, shp in SCRATCH.items():
        kind = "ExternalInput" if k in ext_in else ("ExternalOutput" if k in ext_out else "Internal")
        kb.t[k] = nc.dram_tensor(k, list(shp), F32, kind=kind).ap()
    kb.t["out"] = nc.dram_tensor("out", [2, NL, D], F32, kind="ExternalOutput").ap()
    if dbg:
        kb.t["dbg_comb"] = nc.dram_tensor("dbg_comb", [2, 128, NT, 32], F32, kind="ExternalOutput").ap()
    es = ExitStack()
    kb.ss = SemState(nc, es)
    T = kb.t
    with es:
        if "M" in phases:
            phase_M(kb)
        for b in range(nb):
            if "A" in phases:
                phase_A(kb, b)
            if "E0" in phases:
                phase_E(kb, 0, b, T["H1"], T["H2"])
            if "P" in phases:
                phase_P(kb, b, T["H2"], T["H3"])
            if "E1" in phases:
                phase_E(kb, 1, b, T["H3"], T["out"])
    return nc


def host_consts():
    f32 = np.float32
    c = {}
    c["ident"] = np.eye(128, dtype=f32)
    pos = (np.arange(NT)[None, :] * 128 + np.arange(128)[:, None]).astype(np.int64)
    rows = (pos // 64).astype(f32)
    cols = (pos % 64).astype(f32)
    n_freq = 32
    inv_freq = (np.float32(10000.0) ** (-np.arange(n_freq, dtype=f32) / np.float32(n_freq))).astype(f32)
    ang = np.concatenate([rows[..., None] * inv_freq, cols[..., None] * inv_freq], axis=-1).astype(f32)
    c["rope"] = np.stack([np.cos(ang), np.sin(ang)], axis=2).astype(f32)
    j = np.arange(128, dtype=f32)[:, None]
    i = np.arange(128, dtype=f32)[None, :]
    BIGT = np.float32(1e6)
    tf = np.where(i >= j, i - j, BIGT)
    tb = np.where(j >= i, j - i, BIGT)
    qif = np.broadcast_to(i + 1.0, (128, 128))
    qib = np.broadcast_to(128.0 - i, (128, 128))
    c["rtab"] = np.stack([tf, tb, qif, qib], axis=1).astype(f32)
    p = np.arange(128, dtype=f32)
    c["ktab"] = np.stack([127.0 - p, p, np.full(128, 128.0, f32), np.full(128, 128.0, f32)], axis=1).astype(f32)
    qc = np.arange(64)[:, None]
    kc = np.arange(64)[None, :]
    ws = np.clip(qc - 8, 0, 48)
    ok = (kc >= ws) & (kc < ws + 16)
    cm = np.where(ok, 0.0, NEG).astype(f32)
    c["colmask"] = np.concatenate([cm, cm], axis=0)
    invc = np.zeros((4, 16), f32)
    for g, w in enumerate((2, 4, 8, 16)):
        for s, t0 in enumerate((0, NL - 8)):
            for k in range(8):
                t = t0 + k
                lo = min(max(t - w // 2, 0), NL)
                hi = min(max(t + (w - w // 2), 0), NL)
                invc[g, s * 8 + k] = 1.0 / (hi - lo)
    c["invc"] = np.broadcast_to(invc[None], (128, 4, 16)).astype(f32).copy()
    return c


def make_in_maps(inputs, n_cores=8, nb=2):
    consts = host_consts()
    f = lambda a: np.ascontiguousarray(np.asarray(a, dtype=np.float32))
    shared = {
        "c_ctx": f(inputs["c_ctx"]), "w_mod": f(inputs["w_mod"]), "b_mod": f(inputs["b_mod"]),
        "ln_g": f(inputs["ln_g"]), "ln_b": f(inputs["ln_b"]),
        "w_in": f(inputs["ab_w_in"][0]), "w_out": f(inputs["ab_w_out"][0]),
        "log_decay": f(inputs["ab_log_decay"][0]), "rpb": f(inputs["ab_rpb"][0]),
        "pool_w": f(inputs["pool_w"][0]), "pool_scale": f(inputs["pool_scale"][0]),
        "w_r1": f(inputs["moe_w_r1"]), "b_r1": f(inputs["moe_b_r1"]), "w_r2": f(inputs["moe_w_r2"]), "b_r2": f(inputs["moe_b_r2"]),
        "w_gate": f(inputs["moe_w_gate"]), "w_up": f(inputs["moe_w_up"]), "w_down": f(inputs["moe_w_down"]),
    }
    shared.update(consts)
    maps = []
    for i in range(n_cores):
        m = dict(shared)
        m["x"] = f(inputs["x"][i * nb:(i + 1) * nb])
        m["ctx"] = f(inputs["ctx"][i * nb:(i + 1) * nb])
        m["c"] = f(inputs["c"][i * nb:(i + 1) * nb])
        maps.append(m)
    return maps


def kernel(**inputs):
    nc = build()
    maps = make_in_maps(inputs, n_cores=8, nb=2)
    res = run_bass_kernel_spmd(nc, maps, core_ids=list(range(8)))
    out = np.concatenate([np.asarray(r["out"]) for r in res.results], axis=0)
    return out.astype(np.float32)
```

```python
import numpy as np
from contextlib import ExitStack
from concourse.bass_utils import run_bass_kernel_spmd
import concourse.bass as bass
import concourse.mybir as mybir

F32 = mybir.dt.float32
BF16 = mybir.dt.bfloat16
ALU = mybir.AluOpType
AF = mybir.ActivationFunctionType
AX = mybir.AxisListType

COMPUTE = ("pe", "act", "dve", "pool")
EPOCH = 30000


class R:
    __slots__ = ("name", "lw", "rd")

    def __init__(self, name=""):
        self.name = name
        self.lw = None
        self.rd = []


class Op:
    __slots__ = ("eng", "fn", "is_dma", "deps", "idx", "sig", "sem", "val", "pos")

    def __init__(self, eng, fn, is_dma):
        self.eng = eng
        self.fn = fn
        self.is_dma = is_dma
        self.deps = []
        self.sig = False
        self.sem = None
        self.val = 0


class SemState:
    def __init__(self, nc, es):
        self.nc = nc
        self.es = es
        self.sems = {}
        self.cnt = {e: 0 for e in COMPUTE}
        self.dma_rr = {q: 0 for q in ("sp", "act", "pool")}
        self.dma_cnt = {}

    def get(self, name):
        if name not in self.sems:
            self.sems[name] = self.es.enter_context(self.nc.semaphore(name))
        return self.sems[name]


class Prog:
    def __init__(self, nc, ss, n_dma_sems=None):
        self.nc = nc
        self.ss = ss
        self.ops = []
        self.n_dma_sems = n_dma_sems or {"sp": 24, "act": 8, "pool": 12}

    def _add(self, eng, fn, reads, writes, is_dma):
        op = Op(eng, fn, is_dma)
        op.idx = len(self.ops)
        raw = {}
        other = {}
        for r in reads:
            if r.lw is not None:
                raw[r.lw.idx] = r.lw
        for w in writes:
            if w.lw is not None:
                other[w.lw.idx] = w.lw
            lastrd = {}
            for rd in w.rd:
                if rd.is_dma:
                    other[rd.idx] = rd
                else:
                    lastrd[rd.eng] = rd
            for rd in lastrd.values():
                other[rd.idx] = rd
        for r in reads:
            r.rd.append(op)
        for w in writes:
            w.lw = op
            w.rd = []
        for i, d in raw.items():
            if (not d.is_dma) and (not is_dma) and d.eng == eng and eng == "pe":
                continue
            op.deps.append(d)
        for i, d in other.items():
            if i in raw:
                continue
            if (not d.is_dma) and (not is_dma) and d.eng == eng:
                continue
            op.deps.append(d)
        self.ops.append(op)
        return op

    def op(self, eng, fn, reads=(), writes=()):
        assert eng in COMPUTE
        return self._add(eng, fn, list(reads), list(writes), False)

    def dma(self, q, fn, reads=(), writes=()):
        assert q in ("sp", "act", "pool")
        return self._add(q, fn, list(reads), list(writes), True)

    def emit(self, final_wait_all=True):
        nc = self.nc
        ops = self.ops
        for op in ops:
            for d in op.deps:
                d.sig = True
        last_of = {}
        for op in ops:
            last_of[op.eng if not op.is_dma else ("dma", op.eng)] = op
        ss = self.ss
        get_sem = ss.get
        cnt = ss.cnt
        dma_rr = ss.dma_rr
        dma_cnt = ss.dma_cnt
        dma_prev = {}
        final_dma = []
        for op in ops:
            if op.is_dma:
                q = op.eng
                slot = dma_rr[q] % self.n_dma_sems[q]
                dma_rr[q] += 1
                key = (q, slot)
                op.sem = get_sem(f"d_{q}_{slot}")
                dma_cnt[key] = dma_cnt.get(key, 0) + 16
                op.val = dma_cnt[key]
                prev = dma_prev.get(key)
                if prev is not None:
                    op.deps.append(prev)
                dma_prev[key] = op
                op.sig = True
            elif op.sig:
                e = op.eng
                ep = cnt[e] // EPOCH
                op.sem = get_sem(f"c_{e}_{ep}")
                cnt[e] += 1
                op.val = cnt[e] - ep * EPOCH
        final_dma = list(dma_prev.values())

        per_eng = {e: [] for e in ("pe", "act", "dve", "pool", "sp")}
        for op in ops:
            per_eng[op.eng].append(op)

        engobj = {"pe": None, "act": None, "dve": None, "pool": None, "sp": None}
        self.stats = {e: len(v) for e, v in per_eng.items()}

        def run_engine(ename, eng):
            known = {}
            for op in per_eng[ename]:
                need = {}
                for d in op.deps:
                    s = d.sem
                    if s is None:
                        continue
                    k = s.name if hasattr(s, "name") else id(s)
                    if known.get(k, 0) >= d.val:
                        continue
                    if k not in need or need[k][1] < d.val:
                        need[k] = (s, d.val)
                for k, (s, v) in need.items():
                    eng.wait_ge(s, v)
                    known[k] = v
                ins = op.fn(eng)
                if op.sig:
                    ins.then_inc(op.sem, 16 if op.is_dma else 1)
            if ename == "sp" and final_wait_all:
                for d in final_dma:
                    k = d.sem.name
                    if known.get(k, 0) < d.val:
                        eng.wait_ge(d.sem, d.val)
                        known[k] = d.val

        with nc.Block() as block:
            @block.tensor
            def _(e):
                run_engine("pe", e)

            @block.scalar
            def _(e):
                run_engine("act", e)

            @block.vector
            def _(e):
                run_engine("dve", e)

            @block.gpsimd
            def _(e):
                run_engine("pool", e)

            @block.sync
            def _(e):
                run_engine("sp", e)


D = 1024
NT = 16
NL = 2048
NCX = 256
ALPHA = float(4 ** 0.25)
EPS = 1e-5
NEG = -30000.0
OFF_RK, OFF_RV, OFF_NK, OFF_NV, OFF_RQ, OFF_RG, OFF_NQ = 0, 512, 1024, 1536, 2048, 2560, 3072


class Phase:
    cnt = 0

    def __init__(self, kb, name):
        self.kb = kb
        self.nc = kb.nc
        self.name = name
        self.P = Prog(kb.nc, kb.ss)
        self.es = ExitStack()
        self.n = 0

    def sb(self, shape, dt=F32):
        Phase.cnt += 1
        return self.es.enter_context(self.nc.sbuf_tensor(f"{self.name}_s{Phase.cnt}", list(shape), dt))

    def ps(self, shape, dt=F32):
        Phase.cnt += 1
        return self.es.enter_context(self.nc.psum_tensor(f"{self.name}_p{Phase.cnt}", list(shape), dt))

    def banks(self, n):
        return [(self.ps([128, 512], F32), R()) for _ in range(n)]

    def finish(self):
        self.P.emit()
        self.es.close()
        self.nc.all_engine_barrier()


def emit_pipelined(units):
    n = len(units)
    S = max(len(u) for u in units) if units else 0
    for step in range(n + S - 1):
        for st in range(S):
            i = step - st
            if 0 <= i < n and st < len(units[i]):
                units[i][st]()


def ld(P, dst, src, reads=(), writes=(), q="sp", **kw):
    return P.dma(q, lambda e: e.dma_start(out=dst, in_=src, **kw), reads, writes)


def phase_M(kb):
    nc = kb.nc
    ph = Phase(kb, "M")
    P = ph.P
    T = kb.t
    ident = ph.sb([128, 128]); r_id = R()
    ld(P, ident[:], T["ident"], [], [r_id])
    c3 = ph.sb([3, D]); r_c3 = R()
    ld(P, c3[0:2, :], T["c"], [], [r_c3])
    ld(P, c3[2:3, :], T["c_ctx"].rearrange("(o d) -> o d", o=1), [], [r_c3])
    s3 = ph.sb([3, D]); r_s3 = R()
    P.op("act", lambda e: e.activation(out=s3[:], in_=c3[:], func=AF.Silu), [r_c3], [r_s3])
    sT = ph.sb([128, 8, 3]); r_sT = R()
    bk = ph.banks(4)
    pT, r_pT = bk[0]
    for k in range(8):
        P.op("pe", lambda e, k=k: e.transpose(out=pT[:, k * 3:(k + 1) * 3], in_=s3[0:3, k * 128:(k + 1) * 128], identity=ident[0:3, 0:3]),
             [r_s3, r_id], [r_pT])
    P.op("dve", lambda e: e.tensor_copy(out=sT[:].rearrange("p k c -> p (k c)"), in_=pT[:, 0:24]), [r_pT], [r_sT])
    mv = ph.sb([3, 2, 6 * D]); r_mv = R()
    bb = ph.sb([3, 2, 6 * D]); r_bb = R()
    for l in range(2):
        ld(P, bb[:, l, :], T["b_mod"][l, :].partition_broadcast(3), [], [r_bb])
    wst = [(ph.sb([128, 8, 512]), R()) for _ in range(2)]
    i = 0
    for l in range(2):
        for j in range(12):
            w, r_w = wst[i % 2]
            pb, r_pb = bk[1 + i % 2]
            i += 1
            ld(P, w[:], T["w_mod"][l, :, j * 512:(j + 1) * 512].rearrange("(k p) n -> p k n", p=128), [], [r_w])
            for k in range(8):
                P.op("pe", lambda e, k=k, w=w, pb=pb: e.matmul(pb[0:3, :], lhsT=sT[:, k, :], rhs=w[:, k, :], start=(k == 0), stop=(k == 7)),
                     [r_sT, r_w], [r_pb])
            P.op("dve", lambda e, l=l, j=j, pb=pb: e.tensor_tensor(out=mv[:, l, j * 512:(j + 1) * 512], in0=pb[0:3, :], in1=bb[:, l, j * 512:(j + 1) * 512], op=ALU.add),
                 [r_pb, r_bb], [r_mv])
    for l in range(2):
        ld(P, T["MV"][l], mv[:, l, :], [r_mv], [])
    mT = ph.sb([128, 2, 48, 3]); r_mT = R()
    pM, r_pM = bk[3]
    for l in range(2):
        for c in range(48):
            P.op("pe", lambda e, l=l, c=c: e.transpose(out=pM[:, c * 3:(c + 1) * 3], in_=mv[0:3, l, c * 128:(c + 1) * 128], identity=ident[0:3, 0:3]),
                 [r_mv, r_id], [r_pM])
        P.op("dve", lambda e, l=l: e.tensor_copy(out=mT[:, l, :, :].rearrange("p c t -> p (c t)"), in_=pM[:, 0:144]), [r_pM], [r_mT])
    ld(P, T["MT"].rearrange("l p c t -> p l (c t)"), mT[:].rearrange("p l c t -> p l (c t)"), [r_mT], [])

    zt = ph.sb([120, 128]); r_zt = R()
    P.op("pool", lambda e: e.memset(zt[:], 0.0), [], [r_zt])
    r_RP = R()
    ld(P, T["RP"].rearrange("h a j -> (h a) j"), zt[:], [r_zt], [r_RP])
    ld(P, T["RP"][:, :, 48:79], T["rpb"], [], [r_RP])
    bt = ph.sb([128, 8, 15, 64]); r_bt = R()
    rs_bt = [R() for _ in range(128)]
    for u in range(2):
        for qc in range(64):
            p = u * 64 + qc
            ld(P, bt[p:p + 1, :, :, :], T["RP"][:, :, 63 - qc:127 - qc].rearrange("(o h) a j -> o h a j", o=1), [r_RP], [rs_bt[p]])
    cm = ph.sb([128, 64]); r_cm = R()
    ld(P, cm[:], T["colmask"], [], [r_cm])
    for h in range(8):
        P.op("pool" if h % 2 else "dve", lambda e, h=h: e.tensor_tensor(out=bt[:, h, :, :], in0=bt[:, h, :, :], in1=cm[:].unsqueeze(1).to_broadcast([128, 15, 64]), op=ALU.add),
             rs_bt + [r_cm], [r_bt])
    ld(P, T["TOEP"].rearrange("h p a j -> p h (a j)"), bt[:].rearrange("p h a j -> p h (a j)"), [r_bt], [])
    ph.finish()


def load_mT(ph, l):
    T = ph.kb.t
    mT = ph.sb([128, 48, 3]); r = R()
    ld(ph.P, mT[:].rearrange("p c t -> p (c t)"), T["MT"][l].rearrange("p c t -> p (c t)"), [], [r])
    return mT, r


def make_mod_cols(ph, mT, r_mT, shift_idx, scale_idx, col):
    P = ph.P
    sc1 = ph.sb([128, 8]); sh = ph.sb([128, 8]); r = R()
    P.op("dve", lambda e: e.tensor_scalar(out=sc1[:], in0=mT[:, scale_idx * 8:(scale_idx + 1) * 8, col], scalar1=1.0, scalar2=None, op0=ALU.add), [r_mT], [r])
    P.op("dve", lambda e: e.tensor_copy(out=sh[:], in_=mT[:, shift_idx * 8:(shift_idx + 1) * 8, col]), [r_mT], [r])
    return sc1, sh, r


def build_xT(ph, src_rows, n_tiles, dstT, dst_off, sc1, sh, r_mod, ident, r_id, bk, r_dst_list, xin, extra=None, all_act=False):
    P = ph.P
    ng = (n_tiles + 3) // 4
    bi = 0
    for g in range(ng):
        nt = min(4, n_tiles - g * 4)
        xt, r_xt = xin[g % len(xin)]
        ld(P, xt[:, 0:nt, :], src_rows[g * 512:g * 512 + nt * 128, :].rearrange("(t p) d -> p t d", p=128), [], [r_xt])
        for k in range(8):
            pb, r_pb = bk[bi % len(bk)]
            bi += 1
            for t in range(nt):
                P.op("pe", lambda e, pb=pb, t=t, k=k, xt=xt: e.transpose(out=pb[:, t * 128:(t + 1) * 128], in_=xt[:, t, k * 128:(k + 1) * 128], identity=ident[:]),
                     [r_xt, r_id], [r_pb])
            o = dst_off + g * 512
            if extra is not None:
                xt32, r_x32 = extra(g, k, pb, r_pb, nt)
                P.op("act", lambda e, k=k, o=o, nt=nt, xt32=xt32: e.activation(out=dstT[:, k, o:o + nt * 128], in_=xt32[:, k, 0:nt * 128], func=AF.Copy), [r_x32], [r_dst_list[g]])
                if k == 7:
                    extra(g, 8, pb, r_pb, nt)
            elif k % 2 == 0:
                P.op("act", lambda e, pb=pb, k=k, o=o, nt=nt: e.activation(out=dstT[:, k, o:o + nt * 128], in_=pb[:, 0:nt * 128], func=AF.Identity,
                                                                             scale=sc1[:, k:k + 1], bias=sh[:, k:k + 1]), [r_pb, r_mod], [r_dst_list[g]])
            else:
                P.op("dve", lambda e, pb=pb, k=k, o=o, nt=nt: e.tensor_scalar(out=dstT[:, k, o:o + nt * 128], in0=pb[:, 0:nt * 128], scalar1=sc1[:, k:k + 1], scalar2=sh[:, k:k + 1],
                                                                                op0=ALU.mult, op1=ALU.add), [r_pb, r_mod], [r_dst_list[g]])


def load_bcast(ph, row_ap, width=D):
    t = ph.sb([128, width]); r = R()
    ld(ph.P, t[:], row_ap.partition_broadcast(128), [], [r])
    return t, r


def post_norm(ph, ysrc, y_reads, hsrc_ap, gate, r_gate, lng, r_lng, lnb, r_lnb, dst_ap, bufs, i, pre=None):
    P = ph.P
    nb_ = len(bufs["h"])
    hb, r_hb = bufs["h"][i % nb_]
    tb, r_tb = bufs["t"][i % nb_]
    ob, r_ob = bufs["o"][i % nb_]
    st, r_st = bufs["st"][i % nb_]

    def s0():
        ld(P, hb[:], hsrc_ap, [], [r_hb])
        if pre is not None:
            pre()
        off = 0
        for ap in ysrc:
            w = ap.shape[-1]
            P.op("dve", lambda e, ap=ap, off=off, w=w: e.tensor_tensor(out=tb[:, off:off + w], in0=ap, in1=gate[:, off:off + w], op=ALU.mult), list(y_reads) + [r_gate], [r_tb])
            off += w
        P.op("act", lambda e: e.mul(out=hb[:], in_=hb[:], mul=ALPHA), [r_hb], [r_hb])
        P.op("pool", lambda e: e.tensor_tensor(out=hb[:], in0=hb[:], in1=tb[:], op=ALU.add), [r_hb, r_tb], [r_hb])

    def s1():
        for j in range(2):
            P.op("dve", lambda e, j=j: e.bn_stats(out=st[:, j * 6:(j + 1) * 6], in_=hb[:, j * 512:(j + 1) * 512]), [r_hb], [r_st])
        P.op("dve", lambda e: e.bn_aggr(out=st[:, 12:14], in_=st[:, 0:12].rearrange("p (a b) -> p a b", a=2)), [r_st], [r_st])
        P.op("dve", lambda e: e.tensor_scalar(out=st[:, 14:15], in0=st[:, 13:14], scalar1=EPS, scalar2=None, op0=ALU.add), [r_st], [r_st])
        P.op("act", lambda e: e.activation(out=st[:, 14:15], in_=st[:, 14:15], func=AF.Sqrt), [r_st], [r_st])
        P.op("dve", lambda e: e.reciprocal(out=st[:, 14:15], in_=st[:, 14:15]), [r_st], [r_st])
        P.op("dve", lambda e: e.scalar_tensor_tensor(out=st[:, 15:16], in0=st[:, 12:13], scalar=-1.0, in1=st[:, 14:15], op0=ALU.mult, op1=ALU.mult), [r_st], [r_st])

    def s2():
        P.op("act", lambda e: e.activation(out=tb[:], in_=hb[:], func=AF.Identity, scale=st[:, 14:15], bias=st[:, 15:16]), [r_hb, r_st], [r_tb])
        P.op("pool", lambda e: e.tensor_tensor(out=ob[:], in0=tb[:], in1=lng[:], op=ALU.mult), [r_tb, r_lng], [r_ob])
        P.op("dve", lambda e: e.tensor_tensor(out=ob[:], in0=ob[:], in1=lnb[:], op=ALU.add), [r_ob, r_lnb], [r_ob])
        ld(P, dst_ap, ob[:], [r_ob], [], q="pool")

    return [s0, s1, s2]


def pn_bufs(ph, n=3):
    return {
        "h": [(ph.sb([128, D]), R()) for _ in range(n)],
        "t": [(ph.sb([128, D]), R()) for _ in range(n)],
        "o": [(ph.sb([128, D]), R()) for _ in range(n)],
        "st": [(ph.sb([128, 16]), R()) for _ in range(n)],
    }


class ACtx:
    pass


def phase_A(kb, b):
    nc = kb.nc
    T = kb.t
    pers = ExitStack()
    Phase.cnt += 1
    xmT = pers.enter_context(nc.sbuf_tensor(f"A_xmT{Phase.cnt}", [128, 8, NL + NCX], BF16))
    catT = pers.enter_context(nc.sbuf_tensor(f"A_catT{Phase.cnt}", [128, 8, NL], BF16))
    identf = pers.enter_context(nc.sbuf_tensor(f"A_idf{Phase.cnt}", [128, 128], F32))
    identb = pers.enter_context(nc.sbuf_tensor(f"A_idb{Phase.cnt}", [128, 128], BF16))

    ph = Phase(kb, "A1")
    P = ph.P
    r_id = R()
    ld(P, identf[:], T["ident"], [], [r_id])
    r_idb = R()
    P.op("dve", lambda e: e.tensor_copy(out=identb[:], in_=identf[:]), [r_id], [r_idb])
    mT, r_mT = load_mT(ph, 0)
    sc1, sh, r_mod = make_mod_cols(ph, mT, r_mT, 0, 1, b)
    sc1c, shc, r_modc = make_mod_cols(ph, mT, r_mT, 0, 1, 2)
    bk = ph.banks(4)
    xin = [(ph.sb([128, 4, D]), R()) for _ in range(2)]
    r_x = [R() for _ in range(5)]
    build_xT(ph, T["x"][b], 16, xmT, 0, sc1, sh, r_mod, identf, r_id, bk, r_x[0:4], xin)
    build_xT(ph, T["ctx"][b], 2, xmT, NL, sc1c, shc, r_modc, identf, r_id, bk, r_x[4:5], xin)
    ph.finish()

    for h in range(4):
        phase_A_ret(kb, b, h, xmT, catT, identb)
    for hp in range(4):
        phase_A_na(kb, b, hp, xmT, catT, identb)
    ph = Phase(kb, "A4")
    P = ph.P
    wst = ph.sb([128, 8, D]); r_wst = R()
    wo = ph.sb([128, 8, D], BF16); r_wo = R()
    ld(P, wst[:], T["w_out"].rearrange("(k p) n -> p k n", p=128), [], [r_wst])
    for k in range(8):
        P.op("pool" if k % 2 else "dve", lambda e, k=k: e.tensor_copy(out=wo[:, k, :], in_=wst[:, k, :]), [r_wst], [r_wo])
    gate, r_gate = load_bcast(ph, T["MV"][0, b, 2 * D:3 * D])
    lng, r_lng = load_bcast(ph, T["ln_g"][0, 0])
    lnb, r_lnb = load_bcast(ph, T["ln_b"][0, 0])
    bufs = pn_bufs(ph)
    bk = ph.banks(4)
    r_cat = R()
    units = []
    for t in range(NT):
        (p0, r0), (p1, r1) = bk[(t % 2) * 2], bk[(t % 2) * 2 + 1]

        def pre(t=t, p0=p0, r0=r0, p1=p1, r1=r1):
            for half, (pb, r_pb) in enumerate(((p0, r0), (p1, r1))):
                for k in range(8):
                    P.op("pe", lambda e, k=k, pb=pb, half=half: e.matmul(pb[:], lhsT=catT[:, k, t * 128:(t + 1) * 128], rhs=wo[:, k, half * 512:(half + 1) * 512],
                                                                      start=(k == 0), stop=(k == 7)), [r_cat, r_wo], [r_pb])
        units.append(post_norm(ph, [p0[:], p1[:]], [r0, r1], T["x"][b, t * 128:(t + 1) * 128, :], gate, r_gate, lng, r_lng, lnb, r_lnb,
                               T["H1"][b, t * 128:(t + 1) * 128, :], bufs, t, pre=pre))
    emit_pipelined(units)
    ph.finish()
    pers.close()


def phase_A_ret(kb, b, h, xmT, catT, identb):
    nc = kb.nc
    T = kb.t
    ph = Phase(kb, f"AR{h}")
    P = ph.P
    r_xm = R(); r_idb = R()
    ldt = ph.sb([128, 8]); r_ld = R()
    ld(P, ldt[:], T["log_decay"].rearrange("a h -> (a h)").partition_broadcast(128), [], [r_ld])
    lg = ph.sb([128, 8]); r_lg = R()
    P.op("act", lambda e: e.activation(out=lg[:], in_=ldt[:], func=AF.Exp), [r_ld], [r_lg])
    P.op("act", lambda e: e.activation(out=lg[:], in_=lg[:], func=AF.Ln, scale=-1.0, bias=1.0), [r_lg], [r_lg])
    rtab = ph.sb([128, 4, 128]); r_rt = R()
    ld(P, rtab[:], T["rtab"], [], [r_rt])
    ktab = ph.sb([128, 4]); r_kt = R()
    ld(P, ktab[:], T["ktab"], [], [r_kt])
    lgf = lg[:, h:h + 1]
    lgb = lg[:, 4 + h:5 + h]
    dm = ph.sb([128, 128]); dm2 = ph.sb([128, 128]); r_dm = R(); r_dm2 = R()
    P.op("act", lambda e: e.activation(out=dm[:], in_=rtab[:, 0, :], func=AF.Exp, scale=lgf), [r_rt, r_lg], [r_dm])
    P.op("act", lambda e: e.activation(out=dm2[:], in_=rtab[:, 1, :], func=AF.Exp, scale=lgb), [r_rt, r_lg], [r_dm2])
    P.op("dve", lambda e: e.tensor_tensor(out=dm[:], in0=dm[:], in1=dm2[:], op=ALU.add), [r_dm, r_dm2], [r_dm])
    qd = ph.sb([128, 2, 128]); r_qd = R()
    P.op("act", lambda e: e.activation(out=qd[:, 0, :], in_=rtab[:, 2, :], func=AF.Exp, scale=lgf), [r_rt, r_lg], [r_qd])
    P.op("act", lambda e: e.activation(out=qd[:, 1, :], in_=rtab[:, 3, :], func=AF.Exp, scale=lgb), [r_rt, r_lg], [r_qd])
    kdc = ph.sb([128, 4]); r_kd = R()
    for j, s in enumerate((lgf, lgb, lgf, lgb)):
        P.op("act", lambda e, j=j, s=s: e.activation(out=kdc[:, j:j + 1], in_=ktab[:, j:j + 1], func=AF.Exp, scale=s), [r_kt, r_lg], [r_kd])
    cs = ph.sb([128, NT, 2, 64]); r_cs = R()
    ld(P, cs[:], T["rope"], [], [r_cs])
    wst = ph.sb([128, 8, 4, 128]); r_wst = R()
    for j, off in enumerate((OFF_RK, OFF_RQ, OFF_RV, OFF_RG)):
        ld(P, wst[:, :, j, :], T["w_in"][:, off + h * 128:off + (h + 1) * 128].rearrange("(k p) n -> p k n", p=128), [], [r_wst])
    wb = ph.sb([128, 8, 512], BF16); r_wb = R()
    for k in range(8):
        P.op("pool" if k % 2 else "dve", lambda e, k=k: e.tensor_copy(out=wb[:, k, :], in_=wst[:, k, :, :].rearrange("p a n -> p (a n)")), [r_wst], [r_wb])
    NTA = NT + 2
    kq = ph.sb([128, NTA, 2, 128], BF16); r_kq = [R() for _ in range(NTA)]
    vt = ph.sb([128, NTA, 128], BF16); r_vt = [R() for _ in range(NTA)]
    gs = ph.sb([128, NT, 128]); r_gs = [R() for _ in range(NT)]
    kd = ph.sb([128, NTA, 2, 128], BF16); r_kdd = [R() for _ in range(NTA)]
    kT = ph.sb([128, NTA * 128], BF16); r_kT = [R() for _ in range(NTA)]
    qT = ph.sb([128, NL], BF16); r_qT = [R() for _ in range(NT)]
    bk = ph.banks(3)
    kqf = [(ph.sb([128, 2, 128]), R()) for _ in range(3)]
    tmp = [(ph.sb([128, 4, 2, 64]), [R() for _ in range(4)]) for _ in range(3)]
    ptb = [(ph.ps([128, 1024], BF16), R()) for _ in range(2)]
    order = [16, 17] + list(range(NT))

    def tok_unit(n, t):
        pb, r_pb = bk[n % 3]
        tok = NL + (t - 16) * 128 if t >= 16 else t * 128
        kf, r_kf = kqf[n % 3]
        tm, r_tm = tmp[n % 3]
        pt, r_pt = ptb[n % 2]
        eng = "dve" if n % 2 == 0 else "pool"

        def sA():
            for k in range(8):
                P.op("pe", lambda e, k=k: e.matmul(pb[:], lhsT=xmT[:, k, tok:tok + 128], rhs=wb[:, k, :], start=(k == 0), stop=(k == 7)), [r_xm, r_wb], [r_pb])
            P.op("act", lambda e: e.activation(out=vt[:, t, :], in_=pb[:, 256:384], func=AF.Copy), [r_pb], [r_vt[t]])
            if t >= 16:
                P.op("act", lambda e: e.activation(out=kq[:, t, 0, :], in_=pb[:, 0:128], func=AF.Copy), [r_pb], [r_kq[t]])
            else:
                P.op("act", lambda e: e.activation(out=kf[:, 0, :], in_=pb[:, 0:128], func=AF.Copy), [r_pb], [r_kf])
                P.op("act", lambda e: e.activation(out=kf[:, 1, :], in_=pb[:, 128:256], func=AF.Identity, scale=float(128 ** -0.5)), [r_pb], [r_kf])
                P.op("act", lambda e: e.activation(out=gs[:, t, :], in_=pb[:, 384:512], func=AF.Silu), [r_pb], [r_gs[t]])

        def sB():
            if t < 16:
                cosb = cs[:, t, 0, :].unsqueeze(1).to_broadcast([128, 2, 64])
                sinb = cs[:, t, 1, :].unsqueeze(1).to_broadcast([128, 2, 64])
                x1 = kf[:, :, 0:64]
                x2 = kf[:, :, 64:128]
                P.op(eng, lambda e: e.tensor_tensor(out=tm[:, 0, :, :], in0=x1, in1=cosb, op=ALU.mult), [r_kf, r_cs], [r_tm[0]])
                P.op(eng, lambda e: e.tensor_tensor(out=tm[:, 1, :, :], in0=x2, in1=sinb, op=ALU.mult), [r_kf, r_cs], [r_tm[1]])
                P.op(eng, lambda e: e.tensor_tensor(out=tm[:, 2, :, :], in0=x1, in1=sinb, op=ALU.mult), [r_kf, r_cs], [r_tm[2]])
                P.op(eng, lambda e: e.tensor_tensor(out=tm[:, 3, :, :], in0=x2, in1=cosb, op=ALU.mult), [r_kf, r_cs], [r_tm[3]])
                P.op(eng, lambda e: e.tensor_tensor(out=kq[:, t, :, 0:64], in0=tm[:, 0, :, :], in1=tm[:, 1, :, :], op=ALU.subtract), [r_tm[0], r_tm[1]], [r_kq[t]])
                P.op(eng, lambda e: e.tensor_tensor(out=kq[:, t, :, 64:128], in0=tm[:, 2, :, :], in1=tm[:, 3, :, :], op=ALU.add), [r_tm[2], r_tm[3]], [r_kq[t]])
            P.op(eng, lambda e: e.tensor_scalar(out=kd[:, t, 0, :], in0=kq[:, t, 0, :], scalar1=kdc[:, 0:1], scalar2=None, op0=ALU.mult), [r_kq[t], r_kd], [r_kdd[t]])
            P.op(eng, lambda e: e.tensor_scalar(out=kd[:, t, 1, :], in0=kq[:, t, 0, :], scalar1=kdc[:, 1:2], scalar2=None, op0=ALU.mult), [r_kq[t], r_kd], [r_kdd[t]])

        def sC():
            P.op("pe", lambda e: e.transpose(out=pt[:, 0:128], in_=kq[:, t, 0, :], identity=identb[:]), [r_kq[t], r_idb], [r_pt])
            if t < 16:
                P.op("pe", lambda e: e.transpose(out=pt[:, 128:256], in_=kq[:, t, 1, :], identity=identb[:]), [r_kq[t], r_idb], [r_pt])
                P.op("act", lambda e: e.activation(out=qT[:, t * 128:(t + 1) * 128], in_=pt[:, 128:256], func=AF.Copy), [r_pt], [r_qT[t]])
            P.op("act", lambda e: e.activation(out=kT[:, t * 128:(t + 1) * 128], in_=pt[:, 0:128], func=AF.Copy), [r_pt], [r_kT[t]])

        return [sA, sB, sC]

    emit_pipelined([tok_unit(n, t) for n, t in enumerate(order)])
    ub = ph.banks(2)
    Sf = [(ph.sb([128, 128]), R()) for _ in range(2)]
    Sb = [(ph.sb([128, 128]), R()) for _ in range(2)]
    sbf = ph.sb([128, NT, 2, 128], BF16); r_sbf = [R() for _ in range(NT)]
    chains = (
        (0, [16, 17] + list(range(NT)), Sf),
        (1, [17, 16] + list(range(NT - 1, -1, -1)), Sb),
    )
    ui = 0
    for d, seq, SS in chains:
        cur = None
        for n, c in enumerate(seq):
            if n >= 2:
                P.op("act", lambda e, c=c, d=d, cur=cur: e.activation(out=sbf[:, c, d, :], in_=cur[0][:], func=AF.Copy), [cur[1]], [r_sbf[c]])
            if n == len(seq) - 1:
                break
            pb, r_pb = ub[ui % 2]
            sl = (ui // 2) % 4
            ui += 1
            P.op("pe", lambda e, pb=pb, sl=sl, c=c, d=d: e.matmul(pb[:, sl * 128:(sl + 1) * 128], lhsT=kd[:, c, d, :], rhs=vt[:, c, :], start=True, stop=True),
                 [r_kdd[c], r_vt[c]], [r_pb])
            nxt = SS[n % 2]
            if cur is None:
                P.op("dve", lambda e, pb=pb, sl=sl, nxt=nxt: e.tensor_copy(out=nxt[0][:], in_=pb[:, sl * 128:(sl + 1) * 128]), [r_pb], [nxt[1]])
            else:
                P.op("dve", lambda e, pb=pb, sl=sl, nxt=nxt, cur=cur, d=d: e.scalar_tensor_tensor(out=nxt[0][:], in0=cur[0][:], scalar=kdc[:, 2 + d:3 + d], in1=pb[:, sl * 128:(sl + 1) * 128],
                                                                                              op0=ALU.mult, op1=ALU.add), [r_pb, cur[1], r_kd], [nxt[1]])
            cur = nxt
    attm = [(ph.sb([128, 128], BF16), R()) for _ in range(3)]
    qfb = [(ph.sb([128, 2, 128], BF16), R()) for _ in range(3)]
    on = [(ph.sb([128, 128]), R()) for _ in range(3)]
    yr = ph.sb([128, NT, 128], BF16); r_yr = [R() for _ in range(NT)]
    st = [(ph.sb([128, 16]), R()) for _ in range(3)]
    r_cat = R()

    def chunk_unit(c):
        am, r_am = attm[c % 3]
        qf, r_qf = qfb[c % 3]
        pb, r_pb = bk[c % 3]
        o_sl = pb[:, 0:128]
        a_sl = pb[:, 128:256]
        r_a = r_pb
        s_, r_s = st[c % 3]
        o_, r_o = on[c % 3]
        pt, r_pt = ptb[(c // 4) % 2]

        def sa():
            P.op("pe", lambda e: e.matmul(a_sl, lhsT=kT[:, c * 128:(c + 1) * 128], rhs=qT[:, c * 128:(c + 1) * 128], start=True, stop=True), [r_kT[c], r_qT[c]], [r_a])
            P.op("dve", lambda e: e.tensor_tensor(out=am[:], in0=a_sl, in1=dm[:], op=ALU.mult), [r_a, r_dm], [r_am])
            P.op("pool", lambda e: e.tensor_tensor(out=qf[:], in0=qT[:, c * 128:(c + 1) * 128].unsqueeze(1).to_broadcast([128, 2, 128]), in1=qd[:], op=ALU.mult), [r_qT[c], r_qd], [r_qf])

        def sb_():
            P.op("pe", lambda e: e.matmul(o_sl, lhsT=am[:], rhs=vt[:, c, :], start=True, stop=False), [r_am, r_vt[c]], [r_pb])
            P.op("pe", lambda e: e.matmul(o_sl, lhsT=qf[:, 0, :], rhs=sbf[:, c, 0, :], start=False, stop=False), [r_qf, r_sbf[c]], [r_pb])
            P.op("pe", lambda e: e.matmul(o_sl, lhsT=qf[:, 1, :], rhs=sbf[:, c, 1, :], start=False, stop=True), [r_qf, r_sbf[c]], [r_pb])
            P.op("dve", lambda e: e.bn_stats(out=s_[:, 0:6], in_=o_sl), [r_pb], [r_s])
            P.op("dve", lambda e: e.bn_aggr(out=s_[:, 6:8], in_=s_[:, 0:6]), [r_s], [r_s])
            P.op("dve", lambda e: e.tensor_scalar(out=s_[:, 8:9], in0=s_[:, 7:8], scalar1=EPS, scalar2=None, op0=ALU.add), [r_s], [r_s])
            P.op("act", lambda e: e.activation(out=s_[:, 8:9], in_=s_[:, 8:9], func=AF.Sqrt), [r_s], [r_s])
            P.op("dve", lambda e: e.reciprocal(out=s_[:, 8:9], in_=s_[:, 8:9]), [r_s], [r_s])
            P.op("dve", lambda e: e.scalar_tensor_tensor(out=s_[:, 9:10], in0=s_[:, 6:7], scalar=-1.0, in1=s_[:, 8:9], op0=ALU.mult, op1=ALU.mult), [r_s], [r_s])
            P.op("act", lambda e: e.activation(out=o_[:], in_=o_sl, func=AF.Identity, scale=s_[:, 8:9], bias=s_[:, 9:10]), [r_pb, r_s], [r_o])
            P.op("pool", lambda e: e.tensor_tensor(out=yr[:, c, :], in0=o_[:], in1=gs[:, c, :], op=ALU.mult), [r_o, r_gs[c]], [r_yr[c]])

        def sc():
            P.op("pe", lambda e: e.transpose(out=pt[:, (c % 4) * 128:(c % 4 + 1) * 128], in_=yr[:, c, :], identity=identb[:]), [r_yr[c], r_idb], [r_pt])
            if c % 4 == 3:
                c0 = c - 3
                P.op("act", lambda e: e.activation(out=catT[:, h, c0 * 128:(c0 + 4) * 128], in_=pt[:, 0:512], func=AF.Copy), [r_pt], [r_cat])

        return [sa, sb_, sc]

    emit_pipelined([chunk_unit(c) for c in range(NT)])
    ph.finish()


def na_variants():
    out = {}
    for m in range(16):
        ts = min(max(m - 2, 0), 11)
        key = []
        for u in range(2):
            r = 2 * m + u
            r0 = min(max(r - 4, 0), 24)
            key.append((r0 - 2 * ts, r0 - r + 7))
        out.setdefault(tuple(key), []).append(m)
    return out


def na_unit(P, it, m, u, pu, bias_m, bk, L, Pe, PT, st, ptb, ob, qT, kT, nv, yn, allq, allk, allv, r_yn, identb, r_idb):
    ts = min(max(m - 2, 0), 11)
    bt, r_b = bias_m
    btf = bt[:].rearrange("p a j -> p (a j)")
    (pA, r_pA), (pB, r_pB) = bk[(it % 2)], bk[2 + (it % 2)]
    lq = qT[pu, m * 128:(m + 1) * 128]
    Lt, r_L = L[it % 3]
    s_, r_s = st[it % 3]
    Pt, r_P = Pe[it % 3]
    pt, r_pt = ptb[it % 2]
    PTt, r_PT = PT[it % 3]
    po, r_po = ob[it % 2]
    osl = po[:, 0:64]

    def s1():
        P.op("pe", lambda e: e.matmul(pA[:, 0:512], lhsT=lq, rhs=kT[pu, ts * 128:ts * 128 + 512], start=True, stop=True), allq + allk, [r_pA])
        P.op("pe", lambda e: e.matmul(pB[:, 0:128], lhsT=lq, rhs=kT[pu, ts * 128 + 512:ts * 128 + 640], start=True, stop=True), allq + allk, [r_pB])
        P.op("pe", lambda e: e.matmul(pB[:, 128:384], lhsT=lq, rhs=kT[pu, NL:NL + NCX], start=True, stop=True), allq + allk, [r_pB])
        P.op("dve", lambda e: e.tensor_tensor(out=Lt[:, 0:512], in0=pA[:, 0:512], in1=btf[:, 0:512], op=ALU.add), [r_pA, r_b], [r_L])
        P.op("dve", lambda e: e.tensor_tensor(out=Lt[:, 512:640], in0=pB[:, 0:128], in1=btf[:, 512:640], op=ALU.add), [r_pB, r_b], [r_L])
        P.op("dve", lambda e: e.tensor_copy(out=Lt[:, 640:896], in_=pB[:, 128:384]), [r_pB], [r_L])
        P.op("dve", lambda e: e.reduce_max(out=s_[:, 0:1], in_=Lt[:], axis=AX.X), [r_L], [r_s])
        P.op("dve", lambda e: e.tensor_scalar(out=s_[:, 1:2], in0=s_[:, 0:1], scalar1=-1.0, scalar2=None, op0=ALU.mult), [r_s], [r_s])
        P.op("act", lambda e: e.activation(out=Pt[:], in_=Lt[:], func=AF.Exp, bias=s_[:, 1:2], accum_out=s_[:, 2:3]), [r_L, r_s], [r_P, r_s])

    def s2():
        for c in range(7):
            P.op("pe", lambda e, c=c: e.transpose(out=pt[:, c * 128:(c + 1) * 128], in_=Pt[:, c * 128:(c + 1) * 128], identity=identb[:]), [r_P, r_idb], [r_pt])
        P.op("act", lambda e: e.activation(out=PTt[:].rearrange("p c n -> p (c n)"), in_=pt[:, 0:896], func=AF.Copy), [r_pt], [r_PT])
        P.op("dve", lambda e: e.reciprocal(out=s_[:, 3:4], in_=s_[:, 2:3]), [r_s], [r_s])

    def s3():
        for c in range(7):
            tile_i = ts + c if c < 5 else 16 + (c - 5)
            P.op("pe", lambda e, c=c, tile_i=tile_i: e.matmul(osl, lhsT=PTt[:, c, :], rhs=nv[:, tile_i, pu], start=(c == 0), stop=(c == 6)), [r_PT] + allv, [r_po])
        P.op("act", lambda e: e.activation(out=yn[:, m, pu], in_=osl, func=AF.Identity, scale=s_[:, 3:4]), [r_po, r_s], [r_yn[m]])

    return [s1, s2, s3]


def phase_A_na(kb, b, hp, xmT, catT, identb):
    nc = kb.nc
    T = kb.t
    ph = Phase(kb, f"AN{hp}")
    P = ph.P
    r_xm = R(); r_idb = R(); r_cat = R()
    NTA = NT + 2
    wst = ph.sb([128, 8, 3, 128]); r_wst = R()
    for j, off in enumerate((OFF_NQ, OFF_NK, OFF_NV)):
        ld(P, wst[:, :, j, :], T["w_in"][:, off + hp * 128:off + (hp + 1) * 128].rearrange("(k p) n -> p k n", p=128), [], [r_wst])
    wb = ph.sb([128, 8, 384], BF16); r_wb = R()
    for k in range(8):
        P.op("pool" if k % 2 else "dve", lambda e, k=k: e.tensor_copy(out=wb[:, k, :], in_=wst[:, k, :, :].rearrange("p a n -> p (a n)")), [r_wst], [r_wb])
    qT = ph.sb([128, NL], BF16); r_qT = [R() for _ in range(4)]
    kT = ph.sb([128, NL + NCX], BF16); r_kT = [R() for _ in range(5)]
    nv = ph.sb([128, NTA, 128], BF16); r_nv = [R() for _ in range(5)]
    bk = ph.banks(4)
    bi = 0
    for g in range(5):
        n = 512 if g < 4 else 256
        o = g * 512
        if g < 4:
            pb, r_pb = bk[bi % 4]; bi += 1
            for k in range(8):
                P.op("pe", lambda e, k=k, pb=pb, o=o, n=n: e.matmul(pb[:, 0:n], lhsT=wb[:, k, 0:128], rhs=xmT[:, k, o:o + n], start=(k == 0), stop=(k == 7)), [r_wb, r_xm], [r_pb])
            P.op("act", lambda e, pb=pb, o=o, n=n: e.activation(out=qT[:, o:o + n], in_=pb[:, 0:n], func=AF.Identity, scale=0.125), [r_pb], [r_qT[g]])
        pb, r_pb = bk[bi % 4]; bi += 1
        for k in range(8):
            P.op("pe", lambda e, k=k, pb=pb, o=o, n=n: e.matmul(pb[:, 0:n], lhsT=wb[:, k, 128:256], rhs=xmT[:, k, o:o + n], start=(k == 0), stop=(k == 7)), [r_wb, r_xm], [r_pb])
        P.op("dve", lambda e, pb=pb, o=o, n=n: e.tensor_copy(out=kT[:, o:o + n], in_=pb[:, 0:n]), [r_pb], [r_kT[g]])
        pb, r_pb = bk[bi % 4]; bi += 1
        ntl = n // 128
        for tt in range(ntl):
            for k in range(8):
                P.op("pe", lambda e, k=k, pb=pb, o=o, tt=tt: e.matmul(pb[:, tt * 128:(tt + 1) * 128], lhsT=xmT[:, k, o + tt * 128:o + (tt + 1) * 128], rhs=wb[:, k, 256:384],
                                                                   start=(k == 0), stop=(k == 7)), [r_wb, r_xm], [r_pb])
        P.op("act", lambda e, pb=pb, g=g, ntl=ntl: e.activation(out=nv[:, g * 4:g * 4 + ntl, :].rearrange("p t n -> p (t n)"), in_=pb[:, 0:ntl * 128], func=AF.Copy), [r_pb], [r_nv[g]])
    allq = r_qT; allk = r_kT; allv = r_nv
    variants = na_variants()
    ptb = [(ph.ps([128, 1024], BF16), R()) for _ in range(2)]
    ob = [(ph.ps([128, 512], F32), R()) for _ in range(2)]
    cb = (bk[0][0][:].bitcast(BF16), bk[0][1])
    yn = ph.sb([128, NT, 128], BF16); r_yn = [R() for _ in range(NT)]
    L = [(ph.sb([128, 896]), R()) for _ in range(3)]
    Pe = [(ph.sb([128, 896], BF16), R()) for _ in range(3)]
    PT = [(ph.sb([128, 7, 128], BF16), R()) for _ in range(3)]
    st = [(ph.sb([128, 4]), R()) for _ in range(3)]
    units = []
    it = 0
    for u in range(2):
        h = 2 * hp + u
        pu = slice(64 * u, 64 * u + 64)
        tb = ph.sb([128, 15, 64]); r_tb = R()
        ld(P, tb[:], T["TOEP"][h], [], [r_tb])
        bias = {}
        for vi, (key, ms) in enumerate(variants.items()):
            bt = ph.sb([128, 10, 64]); r_b = R()
            P.op("pool", lambda e, bt=bt: e.memset(bt[:], NEG), [], [r_b])
            for uu in range(2):
                i_lo, a_lo = key[uu]
                pp = slice(64 * uu, 64 * uu + 64)
                P.op("pool", lambda e, bt=bt, tb=tb, pp=pp, i_lo=i_lo, a_lo=a_lo: e.tensor_copy(out=bt[pp, i_lo:i_lo + 8, :], in_=tb[pp, a_lo:a_lo + 8, :]), [r_tb, r_b], [r_b])
            for m in ms:
                bias[m] = (bt, r_b)
        for m in range(16):
            units.append(na_unit(P, it, m, u, pu, bias[m], bk, L, Pe, PT, st, ptb, ob, qT, kT, nv, yn, allq, allk, allv, r_yn, identb, r_idb))
            it += 1
    emit_pipelined(units)
    pc, r_pc = cb
    for m in range(16):
        P.op("pe", lambda e, m=m: e.transpose(out=pc[:, (m % 8) * 128:(m % 8 + 1) * 128], in_=yn[:, m, :], identity=identb[:]), [r_yn[m], r_idb], [r_pc])
        if m % 8 == 7:
            m0 = m - 7
            P.op("act", lambda e, m0=m0: e.activation(out=catT[:, 4 + hp, m0 * 128:(m0 + 8) * 128], in_=pc[:, 0:1024], func=AF.Copy), [r_pc], [r_cat])
    ph.finish()


def phase_E(kb, l, b, src, dst):
    nc = kb.nc
    T = kb.t
    pers = ExitStack()
    Phase.cnt += 1
    acc = pers.enter_context(nc.sbuf_tensor(f"E_acc{Phase.cnt}", [128, NT, D], F32))
    x2T = pers.enter_context(nc.sbuf_tensor(f"E_x2T{Phase.cnt}", [128, 8, NL], BF16))
    comb = pers.enter_context(nc.sbuf_tensor(f"E_comb{Phase.cnt}", [128, NT, 32], F32))

    ph = Phase(kb, "Ea")
    P = ph.P
    identf = ph.sb([128, 128]); r_id = R()
    ld(P, identf[:], T["ident"], [], [r_id])
    mT, r_mT = load_mT(ph, l)
    sc1, sh, r_mod = make_mod_cols(ph, mT, r_mT, 3, 4, b)
    wr = ph.sb([128, 8, 36]); r_wr = R()
    ld(P, wr[:, :, 0:4], T["w_r1"][l].rearrange("(k p) n -> p k n", p=128), [], [r_wr])
    for g in range(4):
        ld(P, wr[:, :, 4 + g * 8:12 + g * 8], T["w_r2"][l, g].rearrange("(k p) n -> p k n", p=128), [], [r_wr])
    brow = ph.sb([128, 36]); r_br = R()
    ld(P, brow[:, 0:4], T["b_r1"][l].partition_broadcast(128), [], [r_br])
    ld(P, brow[:, 4:36], T["b_r2"][l].rearrange("g e -> (g e)").partition_broadcast(128), [], [r_br])
    bk = ph.banks(6)
    rb = ph.banks(2)
    xin = [(ph.sb([128, 4, D]), R()) for _ in range(2)]
    xf = [(ph.sb([128, 8, 512]), [R() for _ in range(8)]) for _ in range(2)]
    logit = ph.sb([128, NT, 36]); r_lg = R()
    r_x = [R() for _ in range(4)]

    def extra(g, k, pb, r_pb, nt):
        xt, r_xf = xf[g % 2]
        if k < 8:
            P.op("dve", lambda e: e.tensor_scalar(out=xt[:, k, :], in0=pb[:, 0:512], scalar1=sc1[:, k:k + 1], scalar2=sh[:, k:k + 1], op0=ALU.mult, op1=ALU.add), [r_pb, r_mod], [r_xf[k]])
            return xt, r_xf[k]
        pr, r_pr = rb[g % 2]
        for tt in range(4):
            for kk in range(8):
                P.op("pe", lambda e, tt=tt, kk=kk: e.matmul(pr[:, tt * 36:(tt + 1) * 36], lhsT=xt[:, kk, tt * 128:(tt + 1) * 128], rhs=wr[:, kk, :], start=(kk == 0), stop=(kk == 7)),
                     r_xf + [r_wr], [r_pr])
        P.op("dve", lambda e: e.tensor_tensor(out=logit[:, g * 4:(g + 1) * 4, :], in0=pr[:, 0:144].rearrange("p (t n) -> p t n", t=4),
                                              in1=brow[:].unsqueeze(1).to_broadcast([128, 4, 36]), op=ALU.add), [r_pr, r_br], [r_lg])

    build_xT(ph, src[b], 16, x2T, 0, sc1, sh, r_mod, identf, r_id, bk, r_x, xin, extra=extra, all_act=True)
    def dv(fn, reads, writes):
        P.op("dve", fn, reads, writes)
    s4 = ph.sb([128, NT, 4]); mg = ph.sb([128, NT, 4]); r_a = R()
    s1 = ph.sb([128, NT, 8]); r_s1 = R()
    lg4 = logit[:, :, 0:4]
    le = logit[:, :, 4:36].rearrange("p t (g e) -> p t g e", g=4)
    dv(lambda e: e.tensor_reduce(out=s1[:, :, 0], in_=lg4, axis=AX.X, op=ALU.max), [r_lg], [r_s1])
    dv(lambda e: e.tensor_tensor(out=mg[:], in0=lg4, in1=s1[:, :, 0:1].to_broadcast([128, NT, 4]), op=ALU.is_equal), [r_lg, r_s1], [r_a])
    dv(lambda e: e.tensor_tensor(out=s4[:], in0=lg4, in1=s1[:, :, 0:1].to_broadcast([128, NT, 4]), op=ALU.subtract), [r_lg, r_s1], [r_a])
    P.op("act", lambda e: e.activation(out=s4[:], in_=s4[:], func=AF.Exp), [r_a], [r_a])
    dv(lambda e: e.tensor_reduce(out=s1[:, :, 1], in_=s4[:], axis=AX.X, op=ALU.add), [r_a], [r_s1])
    dv(lambda e: e.reciprocal(out=s1[:, :, 2], in_=s1[:, :, 1]), [r_s1], [r_s1])
    t48 = ph.sb([128, NT, 4, 8]); r_t48 = R()
    dv(lambda e: e.tensor_tensor(out=t48[:], in0=le, in1=mg[:].unsqueeze(3).to_broadcast([128, NT, 4, 8]), op=ALU.mult), [r_lg, r_a], [r_t48])
    ls = ph.sb([128, NT, 8]); l2 = ph.sb([128, NT, 8]); k1 = ph.sb([128, NT, 8]); k2 = ph.sb([128, NT, 8]); r_ls = R()
    dv(lambda e: e.tensor_reduce(out=ls[:], in_=t48[:].rearrange("p t g e -> p t e g"), axis=AX.X, op=ALU.add), [r_t48], [r_ls])
    dv(lambda e: e.tensor_reduce(out=s1[:, :, 3], in_=ls[:], axis=AX.X, op=ALU.max), [r_ls], [r_s1])
    dv(lambda e: e.tensor_tensor(out=k1[:], in0=ls[:], in1=s1[:, :, 3:4].to_broadcast([128, NT, 8]), op=ALU.is_equal), [r_ls, r_s1], [r_ls])
    dv(lambda e: e.scalar_tensor_tensor(out=l2[:], in0=k1[:], scalar=-1e30, in1=ls[:], op0=ALU.mult, op1=ALU.add), [r_ls], [r_ls])
    dv(lambda e: e.tensor_reduce(out=s1[:, :, 4], in_=l2[:], axis=AX.X, op=ALU.max), [r_ls], [r_s1])
    dv(lambda e: e.tensor_tensor(out=k2[:], in0=l2[:], in1=s1[:, :, 4:5].to_broadcast([128, NT, 8]), op=ALU.is_equal), [r_ls, r_s1], [r_ls])
    dv(lambda e: e.tensor_tensor(out=s1[:, :, 5], in0=s1[:, :, 4], in1=s1[:, :, 3], op=ALU.subtract), [r_s1], [r_s1])
    P.op("act", lambda e: e.activation(out=s1[:, :, 5], in_=s1[:, :, 5], func=AF.Exp), [r_s1], [r_s1])
    dv(lambda e: e.tensor_scalar(out=s1[:, :, 6], in0=s1[:, :, 5], scalar1=1.0, scalar2=None, op0=ALU.add), [r_s1], [r_s1])
    dv(lambda e: e.reciprocal(out=s1[:, :, 6], in_=s1[:, :, 6]), [r_s1], [r_s1])
    dv(lambda e: e.tensor_tensor(out=s1[:, :, 6], in0=s1[:, :, 6], in1=s1[:, :, 2], op=ALU.mult), [r_s1], [r_s1])
    dv(lambda e: e.tensor_tensor(out=s1[:, :, 7], in0=s1[:, :, 6], in1=s1[:, :, 5], op=ALU.mult), [r_s1], [r_s1])
    dv(lambda e: e.tensor_tensor(out=k1[:], in0=k1[:], in1=s1[:, :, 6:7].to_broadcast([128, NT, 8]), op=ALU.mult), [r_ls, r_s1], [r_ls])
    dv(lambda e: e.tensor_tensor(out=k2[:], in0=k2[:], in1=s1[:, :, 7:8].to_broadcast([128, NT, 8]), op=ALU.mult), [r_ls, r_s1], [r_ls])
    dv(lambda e: e.tensor_tensor(out=k1[:], in0=k1[:], in1=k2[:], op=ALU.add), [r_ls], [r_ls])
    r_comb = R()
    dv(lambda e: e.tensor_tensor(out=comb[:].rearrange("p t (g e) -> p t g e", g=4), in0=mg[:].unsqueeze(3).to_broadcast([128, NT, 4, 8]),
                                 in1=k1[:].unsqueeze(2).to_broadcast([128, NT, 4, 8]), op=ALU.mult), [r_a, r_ls], [r_comb])
    if kb.dbg:
        ld(P, T["dbg_comb"][b], comb[:], [r_comb], [])
    ph.finish()

    if getattr(kb, "stop", None) == "Ea":
        pers.close()
        return
    ph = Phase(kb, "Eb")
    P = ph.P
    r_x2 = R(); r_comb = R()
    stg = [(ph.sb([128, 2048]), R()) for _ in range(4)]
    wbf = [dict(g=(ph.sb([128, 8, 512], BF16), R()), u=(ph.sb([128, 8, 512], BF16), R()), d=(ph.sb([128, 4, D], BF16), R())) for _ in range(2)]
    hT = [(ph.sb([128, 4, 512], BF16), R()) for _ in range(2)]
    sg = [(ph.sb([128, 512], BF16), R()) for _ in range(2)]
    gb = ph.banks(2); ub = ph.banks(2); yb = ph.banks(4)
    r_acc = [R() for _ in range(NT)]
    si = 0
    gi_box = [0]
    yi_box = [0]
    pending_down = [None]
    for ex in range(32):
        w = wbf[ex % 2]
        for name, srcw in (("g", T["w_gate"][l, ex]), ("u", T["w_up"][l, ex]), ("d", T["w_down"][l, ex])):
            wt, r_wt = w[name]
            for piece in range(2):
                st_, r_st = stg[si % 4]; si += 1
                if name == "d":
                    ld(P, st_[:].rearrange("p (k n) -> p k n", k=2), srcw[piece * 256:(piece + 1) * 256, :].rearrange("(k p) n -> p k n", p=128), [], [r_st])
                    P.op("pool", lambda e, wt=wt, st_=st_, piece=piece: e.tensor_copy(out=wt[:, piece * 2:(piece + 1) * 2, :].rearrange("p k n -> p (k n)"), in_=st_[:]), [r_st], [r_wt])
                else:
                    ld(P, st_[:].rearrange("p (k n) -> p k n", k=4), srcw[piece * 512:(piece + 1) * 512, :].rearrange("(k p) n -> p k n", p=128), [], [r_st])
                    P.op("pool", lambda e, wt=wt, st_=st_, piece=piece: e.tensor_copy(out=wt[:, piece * 4:(piece + 1) * 4, :].rearrange("p k n -> p (k n)"), in_=st_[:]), [r_st], [r_wt])
        (wg, r_wg), (wu, r_wu), (wd, r_wd) = w["g"], w["u"], w["d"]
        for tg in range(4):
            h_, r_h = hT[(ex * 4 + tg) % 2]

            def up(ex=ex, tg=tg, h_=h_, r_h=r_h, wg=wg, r_wg=r_wg, wu=wu, r_wu=r_wu):
                for hc in range(4):
                    gi = gi_box[0]
                    gi_box[0] += 1
                    (pg, r_pg), (pu_, r_pu) = gb[gi % 2], ub[gi % 2]
                    s_, r_s = sg[gi % 2]
                    for k in range(8):
                        P.op("pe", lambda e, k=k, pg=pg, hc=hc: e.matmul(pg[:], lhsT=wg[:, k, hc * 128:(hc + 1) * 128], rhs=x2T[:, k, tg * 512:(tg + 1) * 512], start=(k == 0), stop=(k == 7)),
                             [r_wg, r_x2], [r_pg])
                    for k in range(8):
                        P.op("pe", lambda e, k=k, pu_=pu_, hc=hc: e.matmul(pu_[:], lhsT=wu[:, k, hc * 128:(hc + 1) * 128], rhs=x2T[:, k, tg * 512:(tg + 1) * 512], start=(k == 0), stop=(k == 7)),
                             [r_wu, r_x2], [r_pu])
                    P.op("act", lambda e, s_=s_, pg=pg: e.activation(out=s_[:], in_=pg[:], func=AF.Silu), [r_pg], [r_s])
                    P.op("dve", lambda e, s_=s_, pu_=pu_, hc=hc: e.tensor_tensor(out=h_[:, hc, :], in0=pu_[:], in1=s_[:], op=ALU.mult), [r_pu, r_s], [r_h])

            def down(ex=ex, tg=tg, h_=h_, r_h=r_h, wd=wd, r_wd=r_wd):
                for tt in range(4):
                    t = tg * 4 + tt
                    for half in range(2):
                        yi = yi_box[0]
                        yi_box[0] += 1
                        py, r_py = yb[yi % 4]
                        for k in range(4):
                            P.op("pe", lambda e, k=k, py=py, tt=tt, half=half: e.matmul(py[:], lhsT=h_[:, k, tt * 128:(tt + 1) * 128], rhs=wd[:, k, half * 512:(half + 1) * 512],
                                                                                 start=(k == 0), stop=(k == 3)), [r_h, r_wd], [r_py])
                        a_sl = acc[:, t, half * 512:(half + 1) * 512]
                        cw = comb[:, t, ex:ex + 1]
                        if ex == 0:
                            P.op("dve", lambda e, a_sl=a_sl, py=py, cw=cw: e.tensor_scalar(out=a_sl, in0=py[:], scalar1=cw, scalar2=None, op0=ALU.mult), [r_py, r_comb], [r_acc[t]])
                        else:
                            P.op("dve", lambda e, a_sl=a_sl, py=py, cw=cw: e.scalar_tensor_tensor(out=a_sl, in0=py[:], scalar=cw, in1=a_sl, op0=ALU.mult, op1=ALU.add), [r_py, r_comb, r_acc[t]], [r_acc[t]])

            up()
            if pending_down[0] is not None:
                pending_down[0]()
            pending_down[0] = down
    pending_down[0]()
    ph.finish()

    if getattr(kb, "stop", None) == "Eb":
        pers.close()
        return
    ph = Phase(kb, "Ec")
    P = ph.P
    gate, r_gate = load_bcast(ph, T["MV"][l, b, 5 * D:6 * D])
    lng, r_lng = load_bcast(ph, T["ln_g"][l, 1])
    lnb, r_lnb = load_bcast(ph, T["ln_b"][l, 1])
    bufs = pn_bufs(ph)
    r_acc2 = R()
    emit_pipelined([post_norm(ph, [acc[:, t, :]], [r_acc2], src[b, t * 128:(t + 1) * 128, :], gate, r_gate, lng, r_lng, lnb, r_lnb,
                              dst[b, t * 128:(t + 1) * 128, :], bufs, t) for t in range(NT)])
    ph.finish()
    pers.close()


def phase_P(kb, b, src, dst):
    nc = kb.nc
    T = kb.t
    l = 1
    NP_ = NL + 16
    pers = ExitStack()
    Phase.cnt += 1
    zT = pers.enter_context(nc.sbuf_tensor(f"P_zT{Phase.cnt}", [128, 8, NL], BF16))
    ph = Phase(kb, "Pa")
    P = ph.P
    identf = ph.sb([128, 128]); r_id = R()
    ld(P, identf[:], T["ident"], [], [r_id])
    mT, r_mT = load_mT(ph, l)
    sc1, sh, r_mod = make_mod_cols(ph, mT, r_mT, 0, 1, b)
    hm = ph.sb([128, 8, NP_]); r_hm = [R() for _ in range(4)]
    r_pad = R()
    P.op("pool", lambda e: e.memset(hm[:, :, 0:8], 0.0), [], [r_pad])
    P.op("pool", lambda e: e.memset(hm[:, :, NL + 8:NL + 16], 0.0), [], [r_pad])
    bk = ph.banks(6)
    xin = [(ph.sb([128, 4, D]), R()) for _ in range(2)]
    build_xT(ph, src[b], 16, hm, 8, sc1, sh, r_mod, identf, r_id, bk, r_hm, xin)
    invc = ph.sb([128, 4, 16]); r_ic = R()
    ld(P, invc[:], T["invc"], [], [r_ic])
    A = [(ph.sb([128, NP_]), R()) for _ in range(2)]
    Bf = [(ph.sb([128, NP_]), R()) for _ in range(2)]
    zb = [(ph.sb([128, 16]), R()) for _ in range(2)]
    r_z = R()
    allhm = r_hm + [r_pad]
    for c in range(8):
        g = c // 2
        w = (2, 4, 8, 16)[g]
        x = hm[:, c, :]
        eng = "dve" if c % 2 == 0 else "pool"
        (a, r_a), (bb, r_b) = A[c % 2], Bf[c % 2]
        P.op(eng, lambda e, a=a, x=x: e.tensor_tensor(out=a[:, 1:NL + 15], in0=x[:, 0:NL + 14], in1=x[:, 1:NL + 15], op=ALU.add), allhm, [r_a])
        cur, r_cur = a, r_a
        if g >= 1:
            P.op(eng, lambda e, a=a, bb=bb: e.tensor_tensor(out=bb[:, 2:NL + 14], in0=a[:, 1:NL + 13], in1=a[:, 3:NL + 15], op=ALU.add), [r_a], [r_b])
            cur, r_cur = bb, r_b
        if g >= 2:
            P.op(eng, lambda e, a=a, bb=bb: e.tensor_tensor(out=a[:, 4:NL + 12], in0=bb[:, 2:NL + 10], in1=bb[:, 6:NL + 14], op=ALU.add), [r_b], [r_a])
            cur, r_cur = a, r_a
        if g >= 3:
            P.op(eng, lambda e, a=a, bb=bb: e.tensor_tensor(out=bb[:, 8:NL + 8], in0=a[:, 4:NL + 4], in1=a[:, 12:NL + 12], op=ALU.add), [r_a], [r_b])
            cur, r_cur = bb, r_b
        P.op("dve", lambda e, cur=cur, x=x, c=c, w=w: e.scalar_tensor_tensor(out=zT[:, c, :], in0=cur[:, 8:NL + 8], scalar=1.0 / w, in1=x[:, 8:NL + 8], op0=ALU.mult, op1=ALU.subtract),
             [r_cur] + allhm, [r_z])
        z_, r_zb = zb[c % 2]
        for side, (o_src, o_dst) in enumerate(((8, 0), (NL, NL - 8))):
            P.op(eng, lambda e, cur=cur, z_=z_, g=g, side=side, o_src=o_src: e.tensor_tensor(out=z_[:, side * 8:(side + 1) * 8], in0=cur[:, o_src:o_src + 8], in1=invc[:, g, side * 8:(side + 1) * 8], op=ALU.mult),
                 [r_cur, r_ic], [r_zb])
            P.op(eng, lambda e, z_=z_, x=x, c=c, side=side, o_src=o_src, o_dst=o_dst: e.tensor_tensor(out=zT[:, c, o_dst:o_dst + 8], in0=z_[:, side * 8:(side + 1) * 8], in1=x[:, o_src:o_src + 8], op=ALU.subtract),
                 [r_zb] + allhm, [r_z])
    ph.finish()
    ph = Phase(kb, "Pb")
    P = ph.P
    pws = ph.sb([128, 4, 2, 256]); r_pws = R()
    for g in range(4):
        ld(P, pws[:, g, :, :], T["pool_w"][g].rearrange("(k p) e -> p k e", p=128), [], [r_pws])
    pw = ph.sb([128, 4, 2, 256], BF16); r_pw = R()
    P.op("pool", lambda e: e.tensor_copy(out=pw[:], in_=pws[:]), [r_pws], [r_pw])
    gate, r_gate = load_bcast(ph, T["MV"][l, b, 2 * D:3 * D])
    psc, r_psc = load_bcast(ph, T["pool_scale"])
    P.op("dve", lambda e: e.tensor_tensor(out=gate[:], in0=gate[:], in1=psc[:], op=ALU.mult), [r_gate, r_psc], [r_gate])
    lng, r_lng = load_bcast(ph, T["ln_g"][l, 0])
    lnb, r_lnb = load_bcast(ph, T["ln_b"][l, 0])
    bufs = pn_bufs(ph)
    bk = ph.banks(4)
    r_z = R()
    units = []
    for t in range(NT):
        (p0, r0), (p1, r1) = bk[(t % 2) * 2], bk[(t % 2) * 2 + 1]

        def pre(t=t, p0=p0, r0=r0, p1=p1, r1=r1):
            for g in range(4):
                pb, r_pb = (p0, r0) if g < 2 else (p1, r1)
                for kk in range(2):
                    P.op("pe", lambda e, pb=pb, g=g, kk=kk: e.matmul(pb[:, (g % 2) * 256:(g % 2 + 1) * 256], lhsT=zT[:, 2 * g + kk, t * 128:(t + 1) * 128], rhs=pw[:, g, kk, :],
                                                                  start=(kk == 0), stop=(kk == 1)), [r_z, r_pw], [r_pb])
        units.append(post_norm(ph, [p0[:], p1[:]], [r0, r1], src[b, t * 128:(t + 1) * 128, :], gate, r_gate, lng, r_lng, lnb, r_lnb,
                               dst[b, t * 128:(t + 1) * 128, :], bufs, t, pre=pre))
    emit_pipelined(units)
    ph.finish()
    pers.close()


IN_SHAPES = {
    "x": [2, NL, D], "ctx": [2, NCX, D], "c": [2, D], "c_ctx": [D],
    "w_mod": [2, D, 6 * D], "b_mod": [2, 6 * D], "ln_g": [2, 2, D], "ln_b": [2, 2, D],
    "w_in": [D, 3584], "w_out": [D, D], "log_decay": [2, 4], "rpb": [8, 15, 31],
    "pool_w": [4, 256, 256], "pool_scale": [D],
    "w_r1": [2, D, 4], "b_r1": [2, 4], "w_r2": [2, 4, D, 8], "b_r2": [2, 4, 8],
    "w_gate": [2, 32, D, 512], "w_up": [2, 32, D, 512], "w_down": [2, 32, 512, D],
    "ident": [128, 128], "rope": [128, NT, 2, 64], "rtab": [128, 4, 128], "ktab": [128, 4],
    "colmask": [128, 64], "invc": [128, 4, 16],
}
SCRATCH = {
    "MV": [2, 3, 6 * D], "MT": [2, 128, 48, 3], "RP": [8, 15, 128], "TOEP": [8, 128, 15, 64],
    "H1": [2, NL, D], "H2": [2, NL, D], "H3": [2, NL, D],
}


class KB:
    pass


def build(phases=("M", "A", "E0", "P", "E1"), ext_in=(), ext_out=(), dbg=False, nb=2, stop=None):
    nc = bass.Bass("TRN2", target_bir_lowering=False)
    kb = KB()
    kb.stop = stop
    kb.nc = nc
    kb.dbg = dbg
    kb.t = {}
    for k, shp in IN_SHAPES.items():
        kb.t[k] = nc.dram_tensor(k, list(shp), F32, kind="ExternalInput").ap()
    for k, shp in SCRATCH.items():
        kind = "ExternalInput" if k in ext_in else ("ExternalOutput" if k in ext_out else "Internal")
        kb.t[k] = nc.dram_tensor(k, list(shp), F32, kind=kind).ap()
    kb.t["out"] = nc.dram_tensor("out", [2, NL, D], F32, kind="ExternalOutput").ap()
    if dbg:
        kb.t["dbg_comb"] = nc.dram_tensor("dbg_comb", [2, 128, NT, 32], F32, kind="ExternalOutput").ap()
    es = ExitStack()
    kb.ss = SemState(nc, es)
    T = kb.t
    with es:
        if "M" in phases:
            phase_M(kb)
        for b in range(nb):
            if "A" in phases:
                phase_A(kb, b)
            if "E0" in phases:
                phase_E(kb, 0, b, T["H1"], T["H2"])
            if "P" in phases:
                phase_P(kb, b, T["H2"], T["H3"])
            if "E1" in phases:
                phase_E(kb, 1, b, T["H3"], T["out"])
    return nc


def host_consts():
    f32 = np.float32
    c = {}
    c["ident"] = np.eye(128, dtype=f32)
    pos = (np.arange(NT)[None, :] * 128 + np.arange(128)[:, None]).astype(np.int64)
    rows = (pos // 64).astype(f32)
    cols = (pos % 64).astype(f32)
    n_freq = 32
    inv_freq = (np.float32(10000.0) ** (-np.arange(n_freq, dtype=f32) / np.float32(n_freq))).astype(f32)
    ang = np.concatenate([rows[..., None] * inv_freq, cols[..., None] * inv_freq], axis=-1).astype(f32)
    c["rope"] = np.stack([np.cos(ang), np.sin(ang)], axis=2).astype(f32)
    j = np.arange(128, dtype=f32)[:, None]
    i = np.arange(128, dtype=f32)[None, :]
    BIGT = np.float32(1e6)
    tf = np.where(i >= j, i - j, BIGT)
    tb = np.where(j >= i, j - i, BIGT)
    qif = np.broadcast_to(i + 1.0, (128, 128))
    qib = np.broadcast_to(128.0 - i, (128, 128))
    c["rtab"] = np.stack([tf, tb, qif, qib], axis=1).astype(f32)
    p = np.arange(128, dtype=f32)
    c["ktab"] = np.stack([127.0 - p, p, np.full(128, 128.0, f32), np.full(128, 128.0, f32)], axis=1).astype(f32)
    qc = np.arange(64)[:, None]
    kc = np.arange(64)[None, :]
    ws = np.clip(qc - 8, 0, 48)
    ok = (kc >= ws) & (kc < ws + 16)
    cm = np.where(ok, 0.0, NEG).astype(f32)
    c["colmask"] = np.concatenate([cm, cm], axis=0)
    invc = np.zeros((4, 16), f32)
    for g, w in enumerate((2, 4, 8, 16)):
        for s, t0 in enumerate((0, NL - 8)):
            for k in range(8):
                t = t0 + k
                lo = min(max(t - w // 2, 0), NL)
                hi = min(max(t + (w - w // 2), 0), NL)
                invc[g, s * 8 + k] = 1.0 / (hi - lo)
    c["invc"] = np.broadcast_to(invc[None], (128, 4, 16)).astype(f32).copy()
    return c


def make_in_maps(inputs, n_cores=8, nb=2):
    consts = host_consts()
    f = lambda a: np.ascontiguousarray(np.asarray(a, dtype=np.float32))
    shared = {
        "c_ctx": f(inputs["c_ctx"]), "w_mod": f(inputs["w_mod"]), "b_mod": f(inputs["b_mod"]),
        "ln_g": f(inputs["ln_g"]), "ln_b": f(inputs["ln_b"]),
        "w_in": f(inputs["ab_w_in"][0]), "w_out": f(inputs["ab_w_out"][0]),
        "log_decay": f(inputs["ab_log_decay"][0]), "rpb": f(inputs["ab_rpb"][0]),
        "pool_w": f(inputs["pool_w"][0]), "pool_scale": f(inputs["pool_scale"][0]),
        "w_r1": f(inputs["moe_w_r1"]), "b_r1": f(inputs["moe_b_r1"]), "w_r2": f(inputs["moe_w_r2"]), "b_r2": f(inputs["moe_b_r2"]),
        "w_gate": f(inputs["moe_w_gate"]), "w_up": f(inputs["moe_w_up"]), "w_down": f(inputs["moe_w_down"]),
    }
    shared.update(consts)
    maps = []
    for i in range(n_cores):
        m = dict(shared)
        m["x"] = f(inputs["x"][i * nb:(i + 1) * nb])
        m["ctx"] = f(inputs["ctx"][i * nb:(i + 1) * nb])
        m["c"] = f(inputs["c"][i * nb:(i + 1) * nb])
        maps.append(m)
    return maps


def kernel(**inputs):
    nc = build()
    maps = make_in_maps(inputs, n_cores=8, nb=2)
    res = run_bass_kernel_spmd(nc, maps, core_ids=list(range(8)))
    out = np.concatenate([np.asarray(r["out"]) for r in res.results], axis=0)
    return out.astype(np.float32)
```

```python
import numpy as np
from contextlib import ExitStack
from concourse.bass_utils import run_bass_kernel_spmd
import concourse.bass as bass
import concourse.mybir as mybir

F32 = mybir.dt.float32
BF16 = mybir.dt.bfloat16
ALU = mybir.AluOpType
AF = mybir.ActivationFunctionType
AX = mybir.AxisListType

COMPUTE = ("pe", "act", "dve", "pool")
EPOCH = 30000


class R:
    __slots__ = ("name", "lw", "rd")

    def __init__(self, name=""):
        self.name = name
        self.lw = None
        self.rd = []


class Op:
    __slots__ = ("eng", "fn", "is_dma", "deps", "idx", "sig", "sem", "val", "pos")

    def __init__(self, eng, fn, is_dma):
        self.eng = eng
        self.fn = fn
        self.is_dma = is_dma
        self.deps = []
        self.sig = False
        self.sem = None
        self.val = 0


class SemState:
    def __init__(self, nc, es):
        self.nc = nc
        self.es = es
        self.sems = {}
        self.cnt = {e: 0 for e in COMPUTE}
        self.dma_rr = {q: 0 for q in ("sp", "act", "pool")}
        self.dma_cnt = {}

    def get(self, name):
        if name not in self.sems:
            self.sems[name] = self.es.enter_context(self.nc.semaphore(name))
        return self.sems[name]


class Prog:
    def __init__(self, nc, ss, n_dma_sems=None):
        self.nc = nc
        self.ss = ss
        self.ops = []
        self.n_dma_sems = n_dma_sems or {"sp": 24, "act": 8, "pool": 12}

    def _add(self, eng, fn, reads, writes, is_dma):
        op = Op(eng, fn, is_dma)
        op.idx = len(self.ops)
        raw = {}
        other = {}
        for r in reads:
            if r.lw is not None:
                raw[r.lw.idx] = r.lw
        for w in writes:
            if w.lw is not None:
                other[w.lw.idx] = w.lw
            lastrd = {}
            for rd in w.rd:
                if rd.is_dma:
                    other[rd.idx] = rd
                else:
                    lastrd[rd.eng] = rd
            for rd in lastrd.values():
                other[rd.idx] = rd
        for r in reads:
            r.rd.append(op)
        for w in writes:
            w.lw = op
            w.rd = []
        for i, d in raw.items():
            if (not d.is_dma) and (not is_dma) and d.eng == eng and eng == "pe":
                continue
            op.deps.append(d)
        for i, d in other.items():
            if i in raw:
                continue
            if (not d.is_dma) and (not is_dma) and d.eng == eng:
                continue
            op.deps.append(d)
        self.ops.append(op)
        return op

    def op(self, eng, fn, reads=(), writes=()):
        assert eng in COMPUTE
        return self._add(eng, fn, list(reads), list(writes), False)

    def dma(self, q, fn, reads=(), writes=()):
        assert q in ("sp", "act", "pool")
        return self._add(q, fn, list(reads), list(writes), True)

    def emit(self, final_wait_all=True):
        nc = self.nc
        ops = self.ops
        for op in ops:
            for d in op.deps:
                d.sig = True
        last_of = {}
        for op in ops:
            last_of[op.eng if not op.is_dma else ("dma", op.eng)] = op
        ss = self.ss
        get_sem = ss.get
        cnt = ss.cnt
        dma_rr = ss.dma_rr
        dma_cnt = ss.dma_cnt
        dma_prev = {}
        final_dma = []
        for op in ops:
            if op.is_dma:
                q = op.eng
                slot = dma_rr[q] % self.n_dma_sems[q]
                dma_rr[q] += 1
                key = (q, slot)
                op.sem = get_sem(f"d_{q}_{slot}")
                dma_cnt[key] = dma_cnt.get(key, 0) + 16
                op.val = dma_cnt[key]
                prev = dma_prev.get(key)
                if prev is not None:
                    op.deps.append(prev)
                dma_prev[key] = op
                op.sig = True
            elif op.sig:
                e = op.eng
                ep = cnt[e] // EPOCH
                op.sem = get_sem(f"c_{e}_{ep}")
                cnt[e] += 1
                op.val = cnt[e] - ep * EPOCH
        final_dma = list(dma_prev.values())

        per_eng = {e: [] for e in ("pe", "act", "dve", "pool", "sp")}
        for op in ops:
            per_eng[op.eng].append(op)

        engobj = {"pe": None, "act": None, "dve": None, "pool": None, "sp": None}
        self.stats = {e: len(v) for e, v in per_eng.items()}

        def run_engine(ename, eng):
            known = {}
            for op in per_eng[ename]:
                need = {}
                for d in op.deps:
                    s = d.sem
                    if s is None:
                        continue
                    k = s.name if hasattr(s, "name") else id(s)
                    if known.get(k, 0) >= d.val:
                        continue
                    if k not in need or need[k][1] < d.val:
                        need[k] = (s, d.val)
                for k, (s, v) in need.items():
                    eng.wait_ge(s, v)
                    known[k] = v
                ins = op.fn(eng)
                if op.sig:
                    ins.then_inc(op.sem, 16 if op.is_dma else 1)
            if ename == "sp" and final_wait_all:
                for d in final_dma:
                    k = d.sem.name
                    if known.get(k, 0) < d.val:
                        eng.wait_ge(d.sem, d.val)
                        known[k] = d.val

        with nc.Block() as block:
            @block.tensor
            def _(e):
                run_engine("pe", e)

            @block.scalar
            def _(e):
                run_engine("act", e)

            @block.vector
            def _(e):
                run_engine("dve", e)

            @block.gpsimd
            def _(e):
                run_engine("pool", e)

            @block.sync
            def _(e):
                run_engine("sp", e)


D = 1024
NT = 16
NL = 2048
NCX = 256
ALPHA = float(4 ** 0.25)
EPS = 1e-5
NEG = -30000.0
OFF_RK, OFF_RV, OFF_NK, OFF_NV, OFF_RQ, OFF_RG, OFF_NQ = 0, 512, 1024, 1536, 2048, 2560, 3072


class Phase:
    cnt = 0

    def __init__(self, kb, name):
        self.kb = kb
        self.nc = kb.nc
        self.name = name
        self.P = Prog(kb.nc, kb.ss)
        self.es = ExitStack()
        self.n = 0

    def sb(self, shape, dt=F32):
        Phase.cnt += 1
        return self.es.enter_context(self.nc.sbuf_tensor(f"{self.name}_s{Phase.cnt}", list(shape), dt))

    def ps(self, shape, dt=F32):
        Phase.cnt += 1
        return self.es.enter_context(self.nc.psum_tensor(f"{self.name}_p{Phase.cnt}", list(shape), dt))

    def banks(self, n):
        return [(self.ps([128, 512], F32), R()) for _ in range(n)]

    def finish(self):
        self.P.emit()
        self.es.close()
        self.nc.all_engine_barrier()


def emit_pipelined(units):
    n = len(units)
    S = max(len(u) for u in units) if units else 0
    for step in range(n + S - 1):
        for st in range(S):
            i = step - st
            if 0 <= i < n and st < len(units[i]):
                units[i][st]()


def ld(P, dst, src, reads=(), writes=(), q="sp", **kw):
    return P.dma(q, lambda e: e.dma_start(out=dst, in_=src, **kw), reads, writes)


def phase_M(kb):
    nc = kb.nc
    ph = Phase(kb, "M")
    P = ph.P
    T = kb.t
    ident = ph.sb([128, 128]); r_id = R()
    ld(P, ident[:], T["ident"], [], [r_id])
    c3 = ph.sb([3, D]); r_c3 = R()
    ld(P, c3[0:2, :], T["c"], [], [r_c3])
    ld(P, c3[2:3, :], T["c_ctx"].rearrange("(o d) -> o d", o=1), [], [r_c3])
    s3 = ph.sb([3, D]); r_s3 = R()
    P.op("act", lambda e: e.activation(out=s3[:], in_=c3[:], func=AF.Silu), [r_c3], [r_s3])
    sT = ph.sb([128, 8, 3]); r_sT = R()
    bk = ph.banks(4)
    pT, r_pT = bk[0]
    for k in range(8):
        P.op("pe", lambda e, k=k: e.transpose(out=pT[:, k * 3:(k + 1) * 3], in_=s3[0:3, k * 128:(k + 1) * 128], identity=ident[0:3, 0:3]),
             [r_s3, r_id], [r_pT])
    P.op("dve", lambda e: e.tensor_copy(out=sT[:].rearrange("p k c -> p (k c)"), in_=pT[:, 0:24]), [r_pT], [r_sT])
    mv = ph.sb([3, 2, 6 * D]); r_mv = R()
    bb = ph.sb([3, 2, 6 * D]); r_bb = R()
    for l in range(2):
        ld(P, bb[:, l, :], T["b_mod"][l, :].partition_broadcast(3), [], [r_bb])
    wst = [(ph.sb([128, 8, 512]), R()) for _ in range(2)]
    i = 0
    for l in range(2):
        for j in range(12):
            w, r_w = wst[i % 2]
            pb, r_pb = bk[1 + i % 2]
            i += 1
            ld(P, w[:], T["w_mod"][l, :, j * 512:(j + 1) * 512].rearrange("(k p) n -> p k n", p=128), [], [r_w])
            for k in range(8):
                P.op("pe", lambda e, k=k, w=w, pb=pb: e.matmul(pb[0:3, :], lhsT=sT[:, k, :], rhs=w[:, k, :], start=(k == 0), stop=(k == 7)),
                     [r_sT, r_w], [r_pb])
            P.op("dve", lambda e, l=l, j=j, pb=pb: e.tensor_tensor(out=mv[:, l, j * 512:(j + 1) * 512], in0=pb[0:3, :], in1=bb[:, l, j * 512:(j + 1) * 512], op=ALU.add),
                 [r_pb, r_bb], [r_mv])
    for l in range(2):
        ld(P, T["MV"][l], mv[:, l, :], [r_mv], [])
    mT = ph.sb([128, 2, 48, 3]); r_mT = R()
    pM, r_pM = bk[3]
    for l in range(2):
        for c in range(48):
            P.op("pe", lambda e, l=l, c=c: e.transpose(out=pM[:, c * 3:(c + 1) * 3], in_=mv[0:3, l, c * 128:(c + 1) * 128], identity=ident[0:3, 0:3]),
                 [r_mv, r_id], [r_pM])
        P.op("dve", lambda e, l=l: e.tensor_copy(out=mT[:, l, :, :].rearrange("p c t -> p (c t)"), in_=pM[:, 0:144]), [r_pM], [r_mT])
    ld(P, T["MT"].rearrange("l p c t -> p l (c t)"), mT[:].rearrange("p l c t -> p l (c t)"), [r_mT], [])

    zt = ph.sb([120, 128]); r_zt = R()
    P.op("pool", lambda e: e.memset(zt[:], 0.0), [], [r_zt])
    r_RP = R()
    ld(P, T["RP"].rearrange("h a j -> (h a) j"), zt[:], [r_zt], [r_RP])
    ld(P, T["RP"][:, :, 48:79], T["rpb"], [], [r_RP])
    bt = ph.sb([128, 8, 15, 64]); r_bt = R()
    rs_bt = [R() for _ in range(128)]
    for u in range(2):
        for qc in range(64):
            p = u * 64 + qc
            ld(P, bt[p:p + 1, :, :, :], T["RP"][:, :, 63 - qc:127 - qc].rearrange("(o h) a j -> o h a j", o=1), [r_RP], [rs_bt[p]])
    cm = ph.sb([128, 64]); r_cm = R()
    ld(P, cm[:], T["colmask"], [], [r_cm])
    for h in range(8):
        P.op("pool" if h % 2 else "dve", lambda e, h=h: e.tensor_tensor(out=bt[:, h, :, :], in0=bt[:, h, :, :], in1=cm[:].unsqueeze(1).to_broadcast([128, 15, 64]), op=ALU.add),
             rs_bt + [r_cm], [r_bt])
    ld(P, T["TOEP"].rearrange("h p a j -> p h (a j)"), bt[:].rearrange("p h a j -> p h (a j)"), [r_bt], [])
    ph.finish()


def load_mT(ph, l):
    T = ph.kb.t
    mT = ph.sb([128, 48, 3]); r = R()
    ld(ph.P, mT[:].rearrange("p c t -> p (c t)"), T["MT"][l].rearrange("p c t -> p (c t)"), [], [r])
    return mT, r


def make_mod_cols(ph, mT, r_mT, shift_idx, scale_idx, col):
    P = ph.P
    sc1 = ph.sb([128, 8]); sh = ph.sb([128, 8]); r = R()
    P.op("dve", lambda e: e.tensor_scalar(out=sc1[:], in0=mT[:, scale_idx * 8:(scale_idx + 1) * 8, col], scalar1=1.0, scalar2=None, op0=ALU.add), [r_mT], [r])
    P.op("dve", lambda e: e.tensor_copy(out=sh[:], in_=mT[:, shift_idx * 8:(shift_idx + 1) * 8, col]), [r_mT], [r])
    return sc1, sh, r


def build_xT(ph, src_rows, n_tiles, dstT, dst_off, sc1, sh, r_mod, ident, r_id, bk, r_dst_list, xin, extra=None, all_act=False):
    P = ph.P
    ng = (n_tiles + 3) // 4
    bi = 0
    for g in range(ng):
        nt = min(4, n_tiles - g * 4)
        xt, r_xt = xin[g % len(xin)]
        ld(P, xt[:, 0:nt, :], src_rows[g * 512:g * 512 + nt * 128, :].rearrange("(t p) d -> p t d", p=128), [], [r_xt])
        for k in range(8):
            pb, r_pb = bk[bi % len(bk)]
            bi += 1
            for t in range(nt):
                P.op("pe", lambda e, pb=pb, t=t, k=k, xt=xt: e.transpose(out=pb[:, t * 128:(t + 1) * 128], in_=xt[:, t, k * 128:(k + 1) * 128], identity=ident[:]),
                     [r_xt, r_id], [r_pb])
            o = dst_off + g * 512
            if extra is not None:
                xt32, r_x32 = extra(g, k, pb, r_pb, nt)
                P.op("act", lambda e, k=k, o=o, nt=nt, xt32=xt32: e.activation(out=dstT[:, k, o:o + nt * 128], in_=xt32[:, k, 0:nt * 128], func=AF.Copy), [r_x32], [r_dst_list[g]])
                if k == 7:
                    extra(g, 8, pb, r_pb, nt)
            elif k % 2 == 0:
                P.op("act", lambda e, pb=pb, k=k, o=o, nt=nt: e.activation(out=dstT[:, k, o:o + nt * 128], in_=pb[:, 0:nt * 128], func=AF.Identity,
                                                                             scale=sc1[:, k:k + 1], bias=sh[:, k:k + 1]), [r_pb, r_mod], [r_dst_list[g]])
            else:
                P.op("dve", lambda e, pb=pb, k=k, o=o, nt=nt: e.tensor_scalar(out=dstT[:, k, o:o + nt * 128], in0=pb[:, 0:nt * 128], scalar1=sc1[:, k:k + 1], scalar2=sh[:, k:k + 1],
                                                                                op0=ALU.mult, op1=ALU.add), [r_pb, r_mod], [r_dst_list[g]])


def load_bcast(ph, row_ap, width=D):
    t = ph.sb([128, width]); r = R()
    ld(ph.P, t[:], row_ap.partition_broadcast(128), [], [r])
    return t, r


def post_norm(ph, ysrc, y_reads, hsrc_ap, gate, r_gate, lng, r_lng, lnb, r_lnb, dst_ap, bufs, i, pre=None):
    P = ph.P
    nb_ = len(bufs["h"])
    hb, r_hb = bufs["h"][i % nb_]
    tb, r_tb = bufs["t"][i % nb_]
    ob, r_ob = bufs["o"][i % nb_]
    st, r_st = bufs["st"][i % nb_]

    def s0():
        ld(P, hb[:], hsrc_ap, [], [r_hb])
        if pre is not None:
            pre()
        off = 0
        for ap in ysrc:
            w = ap.shape[-1]
            P.op("dve", lambda e, ap=ap, off=off, w=w: e.tensor_tensor(out=tb[:, off:off + w], in0=ap, in1=gate[:, off:off + w], op=ALU.mult), list(y_reads) + [r_gate], [r_tb])
            off += w
        P.op("act", lambda e: e.mul(out=hb[:], in_=hb[:], mul=ALPHA), [r_hb], [r_hb])
        P.op("pool", lambda e: e.tensor_tensor(out=hb[:], in0=hb[:], in1=tb[:], op=ALU.add), [r_hb, r_tb], [r_hb])

    def s1():
        for j in range(2):
            P.op("dve", lambda e, j=j: e.bn_stats(out=st[:, j * 6:(j + 1) * 6], in_=hb[:, j * 512:(j + 1) * 512]), [r_hb], [r_st])
        P.op("dve", lambda e: e.bn_aggr(out=st[:, 12:14], in_=st[:, 0:12].rearrange("p (a b) -> p a b", a=2)), [r_st], [r_st])
        P.op("dve", lambda e: e.tensor_scalar(out=st[:, 14:15], in0=st[:, 13:14], scalar1=EPS, scalar2=None, op0=ALU.add), [r_st], [r_st])
        P.op("act", lambda e: e.activation(out=st[:, 14:15], in_=st[:, 14:15], func=AF.Sqrt), [r_st], [r_st])
        P.op("dve", lambda e: e.reciprocal(out=st[:, 14:15], in_=st[:, 14:15]), [r_st], [r_st])
        P.op("dve", lambda e: e.scalar_tensor_tensor(out=st[:, 15:16], in0=st[:, 12:13], scalar=-1.0, in1=st[:, 14:15], op0=ALU.mult, op1=ALU.mult), [r_st], [r_st])

    def s2():
        P.op("act", lambda e: e.activation(out=tb[:], in_=hb[:], func=AF.Identity, scale=st[:, 14:15], bias=st[:, 15:16]), [r_hb, r_st], [r_tb])
        P.op("pool", lambda e: e.tensor_tensor(out=ob[:], in0=tb[:], in1=lng[:], op=ALU.mult), [r_tb, r_lng], [r_ob])
        P.op("dve", lambda e: e.tensor_tensor(out=ob[:], in0=ob[:], in1=lnb[:], op=ALU.add), [r_ob, r_lnb], [r_ob])
        ld(P, dst_ap, ob[:], [r_ob], [], q="pool")

    return [s0, s1, s2]


def pn_bufs(ph, n=4):
    return {
        "h": [(ph.sb([128, D]), R()) for _ in range(n)],
        "t": [(ph.sb([128, D]), R()) for _ in range(n)],
        "o": [(ph.sb([128, D]), R()) for _ in range(n)],
        "st": [(ph.sb([128, 16]), R()) for _ in range(n)],
    }


class ACtx:
    pass


def phase_A(kb, b):
    nc = kb.nc
    T = kb.t
    pers = ExitStack()
    Phase.cnt += 1
    xmT = pers.enter_context(nc.sbuf_tensor(f"A_xmT{Phase.cnt}", [128, 8, NL + NCX], BF16))
    catT = pers.enter_context(nc.sbuf_tensor(f"A_catT{Phase.cnt}", [128, 8, NL], BF16))
    identf = pers.enter_context(nc.sbuf_tensor(f"A_idf{Phase.cnt}", [128, 128], F32))
    identb = pers.enter_context(nc.sbuf_tensor(f"A_idb{Phase.cnt}", [128, 128], BF16))

    ph = Phase(kb, "A1")
    P = ph.P
    r_id = R()
    ld(P, identf[:], T["ident"], [], [r_id])
    r_idb = R()
    P.op("dve", lambda e: e.tensor_copy(out=identb[:], in_=identf[:]), [r_id], [r_idb])
    mT, r_mT = load_mT(ph, 0)
    sc1, sh, r_mod = make_mod_cols(ph, mT, r_mT, 0, 1, b)
    sc1c, shc, r_modc = make_mod_cols(ph, mT, r_mT, 0, 1, 2)
    bk = ph.banks(4)
    xin = [(ph.sb([128, 4, D]), R()) for _ in range(2)]
    r_x = [R() for _ in range(5)]
    build_xT(ph, T["x"][b], 16, xmT, 0, sc1, sh, r_mod, identf, r_id, bk, r_x[0:4], xin)
    build_xT(ph, T["ctx"][b], 2, xmT, NL, sc1c, shc, r_modc, identf, r_id, bk, r_x[4:5], xin)
    ph.finish()

    for h in range(4):
        phase_A_ret(kb, b, h, xmT, catT, identb)
    for hp in range(4):
        phase_A_na(kb, b, hp, xmT, catT, identb)
    ph = Phase(kb, "A4")
    P = ph.P
    wst = ph.sb([128, 8, D]); r_wst = R()
    wo = ph.sb([128, 8, D], BF16); r_wo = R()
    ld(P, wst[:], T["w_out"].rearrange("(k p) n -> p k n", p=128), [], [r_wst])
    for k in range(8):
        P.op("pool" if k % 2 else "dve", lambda e, k=k: e.tensor_copy(out=wo[:, k, :], in_=wst[:, k, :]), [r_wst], [r_wo])
    gate, r_gate = load_bcast(ph, T["MV"][0, b, 2 * D:3 * D])
    lng, r_lng = load_bcast(ph, T["ln_g"][0, 0])
    lnb, r_lnb = load_bcast(ph, T["ln_b"][0, 0])
    bufs = pn_bufs(ph)
    bk = ph.banks(4)
    r_cat = R()
    units = []
    for t in range(NT):
        (p0, r0), (p1, r1) = bk[(t % 2) * 2], bk[(t % 2) * 2 + 1]

        def pre(t=t, p0=p0, r0=r0, p1=p1, r1=r1):
            for half, (pb, r_pb) in enumerate(((p0, r0), (p1, r1))):
                for k in range(8):
                    P.op("pe", lambda e, k=k, pb=pb, half=half: e.matmul(pb[:], lhsT=catT[:, k, t * 128:(t + 1) * 128], rhs=wo[:, k, half * 512:(half + 1) * 512],
                                                                      start=(k == 0), stop=(k == 7)), [r_cat, r_wo], [r_pb])
        units.append(post_norm(ph, [p0[:], p1[:]], [r0, r1], T["x"][b, t * 128:(t + 1) * 128, :], gate, r_gate, lng, r_lng, lnb, r_lnb,
                               T["H1"][b, t * 128:(t + 1) * 128, :], bufs, t, pre=pre))
    emit_pipelined(units)
    ph.finish()
    pers.close()


def phase_A_ret(kb, b, h, xmT, catT, identb):
    nc = kb.nc
    T = kb.t
    ph = Phase(kb, f"AR{h}")
    P = ph.P
    r_xm = R(); r_idb = R()
    ldt = ph.sb([128, 8]); r_ld = R()
    ld(P, ldt[:], T["log_decay"].rearrange("a h -> (a h)").partition_broadcast(128), [], [r_ld])
    lg = ph.sb([128, 8]); r_lg = R()
    P.op("act", lambda e: e.activation(out=lg[:], in_=ldt[:], func=AF.Exp), [r_ld], [r_lg])
    P.op("act", lambda e: e.activation(out=lg[:], in_=lg[:], func=AF.Ln, scale=-1.0, bias=1.0), [r_lg], [r_lg])
    rtab = ph.sb([128, 4, 128]); r_rt = R()
    ld(P, rtab[:], T["rtab"], [], [r_rt])
    ktab = ph.sb([128, 4]); r_kt = R()
    ld(P, ktab[:], T["ktab"], [], [r_kt])
    lgf = lg[:, h:h + 1]
    lgb = lg[:, 4 + h:5 + h]
    dm = ph.sb([128, 128]); dm2 = ph.sb([128, 128]); r_dm = R(); r_dm2 = R()
    P.op("act", lambda e: e.activation(out=dm[:], in_=rtab[:, 0, :], func=AF.Exp, scale=lgf), [r_rt, r_lg], [r_dm])
    P.op("act", lambda e: e.activation(out=dm2[:], in_=rtab[:, 1, :], func=AF.Exp, scale=lgb), [r_rt, r_lg], [r_dm2])
    P.op("dve", lambda e: e.tensor_tensor(out=dm[:], in0=dm[:], in1=dm2[:], op=ALU.add), [r_dm, r_dm2], [r_dm])
    qd = ph.sb([128, 2, 128]); r_qd = R()
    P.op("act", lambda e: e.activation(out=qd[:, 0, :], in_=rtab[:, 2, :], func=AF.Exp, scale=lgf), [r_rt, r_lg], [r_qd])
    P.op("act", lambda e: e.activation(out=qd[:, 1, :], in_=rtab[:, 3, :], func=AF.Exp, scale=lgb), [r_rt, r_lg], [r_qd])
    kdc = ph.sb([128, 4]); r_kd = R()
    for j, s in enumerate((lgf, lgb, lgf, lgb)):
        P.op("act", lambda e, j=j, s=s: e.activation(out=kdc[:, j:j + 1], in_=ktab[:, j:j + 1], func=AF.Exp, scale=s), [r_kt, r_lg], [r_kd])
    cs = ph.sb([128, NT, 2, 64]); r_cs = R()
    ld(P, cs[:], T["rope"], [], [r_cs])
    wst = ph.sb([128, 8, 4, 128]); r_wst = R()
    for j, off in enumerate((OFF_RK, OFF_RQ, OFF_RV, OFF_RG)):
        ld(P, wst[:, :, j, :], T["w_in"][:, off + h * 128:off + (h + 1) * 128].rearrange("(k p) n -> p k n", p=128), [], [r_wst])
    wb = ph.sb([128, 8, 512], BF16); r_wb = R()
    for k in range(8):
        P.op("pool" if k % 2 else "dve", lambda e, k=k: e.tensor_copy(out=wb[:, k, :], in_=wst[:, k, :, :].rearrange("p a n -> p (a n)")), [r_wst], [r_wb])
    NTA = NT + 2
    kq = ph.sb([128, NTA, 2, 128], BF16); r_kq = [R() for _ in range(NTA)]
    vt = ph.sb([128, NTA, 128], BF16); r_vt = [R() for _ in range(NTA)]
    gs = ph.sb([128, NT, 128]); r_gs = [R() for _ in range(NT)]
    kd = ph.sb([128, NTA, 2, 128], BF16); r_kdd = [R() for _ in range(NTA)]
    kT = ph.sb([128, NTA * 128], BF16); r_kT = [R() for _ in range(NTA)]
    qT = ph.sb([128, NL], BF16); r_qT = [R() for _ in range(NT)]
    bk = ph.banks(3)
    kqf = [(ph.sb([128, 2, 128]), R()) for _ in range(3)]
    tmp = [(ph.sb([128, 4, 2, 64]), [R() for _ in range(4)]) for _ in range(3)]
    ptb = [(ph.ps([128, 1024], BF16), R()) for _ in range(2)]
    order = [16, 17] + list(range(NT))

    def tok_unit(n, t):
        pb, r_pb = bk[n % 3]
        tok = NL + (t - 16) * 128 if t >= 16 else t * 128
        kf, r_kf = kqf[n % 3]
        tm, r_tm = tmp[n % 3]
        pt, r_pt = ptb[n % 2]
        eng = "pool" if n % 3 == 2 else "dve"

        def sA():
            for k in range(8):
                P.op("pe", lambda e, k=k: e.matmul(pb[:], lhsT=xmT[:, k, tok:tok + 128], rhs=wb[:, k, :], start=(k == 0), stop=(k == 7)), [r_xm, r_wb], [r_pb])
            P.op("act", lambda e: e.activation(out=vt[:, t, :], in_=pb[:, 256:384], func=AF.Copy), [r_pb], [r_vt[t]])
            if t >= 16:
                P.op("act", lambda e: e.activation(out=kq[:, t, 0, :], in_=pb[:, 0:128], func=AF.Copy), [r_pb], [r_kq[t]])
            else:
                P.op("act", lambda e: e.activation(out=kf[:, 0, :], in_=pb[:, 0:128], func=AF.Copy), [r_pb], [r_kf])
                P.op("act", lambda e: e.activation(out=kf[:, 1, :], in_=pb[:, 128:256], func=AF.Identity, scale=float(128 ** -0.5)), [r_pb], [r_kf])
                P.op("act", lambda e: e.activation(out=gs[:, t, :], in_=pb[:, 384:512], func=AF.Silu), [r_pb], [r_gs[t]])

        def sB():
            if t < 16:
                cosb = cs[:, t, 0, :].unsqueeze(1).to_broadcast([128, 2, 64])
                sinb = cs[:, t, 1, :].unsqueeze(1).to_broadcast([128, 2, 64])
                x1 = kf[:, :, 0:64]
                x2 = kf[:, :, 64:128]
                P.op(eng, lambda e: e.tensor_tensor(out=tm[:, 0, :, :], in0=x1, in1=cosb, op=ALU.mult), [r_kf, r_cs], [r_tm[0]])
                P.op(eng, lambda e: e.tensor_tensor(out=tm[:, 1, :, :], in0=x2, in1=sinb, op=ALU.mult), [r_kf, r_cs], [r_tm[1]])
                P.op(eng, lambda e: e.tensor_tensor(out=tm[:, 2, :, :], in0=x1, in1=sinb, op=ALU.mult), [r_kf, r_cs], [r_tm[2]])
                P.op(eng, lambda e: e.tensor_tensor(out=tm[:, 3, :, :], in0=x2, in1=cosb, op=ALU.mult), [r_kf, r_cs], [r_tm[3]])
                P.op(eng, lambda e: e.tensor_tensor(out=kq[:, t, :, 0:64], in0=tm[:, 0, :, :], in1=tm[:, 1, :, :], op=ALU.subtract), [r_tm[0], r_tm[1]], [r_kq[t]])
                P.op(eng, lambda e: e.tensor_tensor(out=kq[:, t, :, 64:128], in0=tm[:, 2, :, :], in1=tm[:, 3, :, :], op=ALU.add), [r_tm[2], r_tm[3]], [r_kq[t]])
            P.op(eng, lambda e: e.tensor_scalar(out=kd[:, t, 0, :], in0=kq[:, t, 0, :], scalar1=kdc[:, 0:1], scalar2=None, op0=ALU.mult), [r_kq[t], r_kd], [r_kdd[t]])
            P.op(eng, lambda e: e.tensor_scalar(out=kd[:, t, 1, :], in0=kq[:, t, 0, :], scalar1=kdc[:, 1:2], scalar2=None, op0=ALU.mult), [r_kq[t], r_kd], [r_kdd[t]])

        def sC():
            P.op("pe", lambda e: e.transpose(out=pt[:, 0:128], in_=kq[:, t, 0, :], identity=identb[:]), [r_kq[t], r_idb], [r_pt])
            if t < 16:
                P.op("pe", lambda e: e.transpose(out=pt[:, 128:256], in_=kq[:, t, 1, :], identity=identb[:]), [r_kq[t], r_idb], [r_pt])
                P.op("act", lambda e: e.activation(out=qT[:, t * 128:(t + 1) * 128], in_=pt[:, 128:256], func=AF.Copy), [r_pt], [r_qT[t]])
            P.op("act", lambda e: e.activation(out=kT[:, t * 128:(t + 1) * 128], in_=pt[:, 0:128], func=AF.Copy), [r_pt], [r_kT[t]])

        return [sA, sB, sC]

    emit_pipelined([tok_unit(n, t) for n, t in enumerate(order)])
    ub = ph.banks(2)
    Sf = [(ph.sb([128, 128]), R()) for _ in range(2)]
    Sb = [(ph.sb([128, 128]), R()) for _ in range(2)]
    sbf = ph.sb([128, NT, 2, 128], BF16); r_sbf = [R() for _ in range(NT)]
    chains = (
        (0, [16, 17] + list(range(NT)), Sf),
        (1, [17, 16] + list(range(NT - 1, -1, -1)), Sb),
    )
    ui = 0
    for d, seq, SS in chains:
        cur = None
        for n, c in enumerate(seq):
            if n >= 2:
                P.op("act", lambda e, c=c, d=d, cur=cur: e.activation(out=sbf[:, c, d, :], in_=cur[0][:], func=AF.Copy), [cur[1]], [r_sbf[c]])
            if n == len(seq) - 1:
                break
            pb, r_pb = ub[ui % 2]
            sl = (ui // 2) % 4
            ui += 1
            P.op("pe", lambda e, pb=pb, sl=sl, c=c, d=d: e.matmul(pb[:, sl * 128:(sl + 1) * 128], lhsT=kd[:, c, d, :], rhs=vt[:, c, :], start=True, stop=True),
                 [r_kdd[c], r_vt[c]], [r_pb])
            nxt = SS[n % 2]
            if cur is None:
                P.op("dve", lambda e, pb=pb, sl=sl, nxt=nxt: e.tensor_copy(out=nxt[0][:], in_=pb[:, sl * 128:(sl + 1) * 128]), [r_pb], [nxt[1]])
            else:
                P.op("dve", lambda e, pb=pb, sl=sl, nxt=nxt, cur=cur, d=d: e.scalar_tensor_tensor(out=nxt[0][:], in0=cur[0][:], scalar=kdc[:, 2 + d:3 + d], in1=pb[:, sl * 128:(sl + 1) * 128],
                                                                                              op0=ALU.mult, op1=ALU.add), [r_pb, cur[1], r_kd], [nxt[1]])
            cur = nxt
    attm = [(ph.sb([128, 128], BF16), R()) for _ in range(3)]
    qfb = [(ph.sb([128, 2, 128], BF16), R()) for _ in range(3)]
    on = [(ph.sb([128, 128]), R()) for _ in range(3)]
    yr = ph.sb([128, NT, 128], BF16); r_yr = [R() for _ in range(NT)]
    st = [(ph.sb([128, 16]), R()) for _ in range(3)]
    r_cat = R()

    def chunk_unit(c):
        am, r_am = attm[c % 3]
        qf, r_qf = qfb[c % 3]
        pb, r_pb = bk[c % 3]
        o_sl = pb[:, 0:128]
        a_sl = pb[:, 128:256]
        r_a = r_pb
        s_, r_s = st[c % 3]
        o_, r_o = on[c % 3]
        pt, r_pt = ptb[(c // 4) % 2]

        def sa():
            P.op("pe", lambda e: e.matmul(a_sl, lhsT=kT[:, c * 128:(c + 1) * 128], rhs=qT[:, c * 128:(c + 1) * 128], start=True, stop=True), [r_kT[c], r_qT[c]], [r_a])
            P.op("dve", lambda e: e.tensor_tensor(out=am[:], in0=a_sl, in1=dm[:], op=ALU.mult), [r_a, r_dm], [r_am])
            P.op("pool", lambda e: e.tensor_tensor(out=qf[:], in0=qT[:, c * 128:(c + 1) * 128].unsqueeze(1).to_broadcast([128, 2, 128]), in1=qd[:], op=ALU.mult), [r_qT[c], r_qd], [r_qf])

        def sb_():
            P.op("pe", lambda e: e.matmul(o_sl, lhsT=am[:], rhs=vt[:, c, :], start=True, stop=False), [r_am, r_vt[c]], [r_pb])
            P.op("pe", lambda e: e.matmul(o_sl, lhsT=qf[:, 0, :], rhs=sbf[:, c, 0, :], start=False, stop=False), [r_qf, r_sbf[c]], [r_pb])
            P.op("pe", lambda e: e.matmul(o_sl, lhsT=qf[:, 1, :], rhs=sbf[:, c, 1, :], start=False, stop=True), [r_qf, r_sbf[c]], [r_pb])
            P.op("dve", lambda e: e.bn_stats(out=s_[:, 0:6], in_=o_sl), [r_pb], [r_s])
            P.op("dve", lambda e: e.bn_aggr(out=s_[:, 6:8], in_=s_[:, 0:6]), [r_s], [r_s])
            P.op("dve", lambda e: e.tensor_scalar(out=s_[:, 8:9], in0=s_[:, 7:8], scalar1=EPS, scalar2=None, op0=ALU.add), [r_s], [r_s])
            P.op("act", lambda e: e.activation(out=s_[:, 8:9], in_=s_[:, 8:9], func=AF.Sqrt), [r_s], [r_s])
            P.op("dve", lambda e: e.reciprocal(out=s_[:, 8:9], in_=s_[:, 8:9]), [r_s], [r_s])
            P.op("dve", lambda e: e.scalar_tensor_tensor(out=s_[:, 9:10], in0=s_[:, 6:7], scalar=-1.0, in1=s_[:, 8:9], op0=ALU.mult, op1=ALU.mult), [r_s], [r_s])
            P.op("act", lambda e: e.activation(out=o_[:], in_=o_sl, func=AF.Identity, scale=s_[:, 8:9], bias=s_[:, 9:10]), [r_pb, r_s], [r_o])
            P.op("pool", lambda e: e.tensor_tensor(out=yr[:, c, :], in0=o_[:], in1=gs[:, c, :], op=ALU.mult), [r_o, r_gs[c]], [r_yr[c]])

        def sc():
            P.op("pe", lambda e: e.transpose(out=pt[:, (c % 4) * 128:(c % 4 + 1) * 128], in_=yr[:, c, :], identity=identb[:]), [r_yr[c], r_idb], [r_pt])
            if c % 4 == 3:
                c0 = c - 3
                P.op("act", lambda e: e.activation(out=catT[:, h, c0 * 128:(c0 + 4) * 128], in_=pt[:, 0:512], func=AF.Copy), [r_pt], [r_cat])

        return [sa, sb_, sc]

    emit_pipelined([chunk_unit(c) for c in range(NT)])
    ph.finish()


def na_variants():
    out = {}
    for m in range(16):
        ts = min(max(m - 2, 0), 11)
        key = []
        for u in range(2):
            r = 2 * m + u
            r0 = min(max(r - 4, 0), 24)
            key.append((r0 - 2 * ts, r0 - r + 7))
        out.setdefault(tuple(key), []).append(m)
    return out


def na_unit(P, it, m, u, pu, bias_m, bk, L, Pe, PT, st, ptb, ob, qT, kT, nv, yn, allq, allk, allv, r_yn, identb, r_idb):
    ts = min(max(m - 2, 0), 11)
    bt, r_b = bias_m
    btf = bt[:].rearrange("p a j -> p (a j)")
    (pA, r_pA), (pB, r_pB) = bk[(it % 2)], bk[2 + (it % 2)]
    lq = qT[pu, m * 128:(m + 1) * 128]
    Lt, r_L = L[it % 3]
    s_, r_s = st[it % 3]
    Pt, r_P = Pe[it % 3]
    pt, r_pt = ptb[it % 2]
    PTt, r_PT = PT[it % 3]
    po, r_po = ob[it % 2]
    osl = po[:, 0:64]

    def s1():
        P.op("pe", lambda e: e.matmul(pA[:, 0:512], lhsT=lq, rhs=kT[pu, ts * 128:ts * 128 + 512], start=True, stop=True), allq + allk, [r_pA])
        P.op("pe", lambda e: e.matmul(pB[:, 0:128], lhsT=lq, rhs=kT[pu, ts * 128 + 512:ts * 128 + 640], start=True, stop=True), allq + allk, [r_pB])
        P.op("pe", lambda e: e.matmul(pB[:, 128:384], lhsT=lq, rhs=kT[pu, NL:NL + NCX], start=True, stop=True), allq + allk, [r_pB])
        P.op("dve", lambda e: e.tensor_tensor(out=Lt[:, 0:512], in0=pA[:, 0:512], in1=btf[:, 0:512], op=ALU.add), [r_pA, r_b], [r_L])
        P.op("dve", lambda e: e.tensor_tensor(out=Lt[:, 512:640], in0=pB[:, 0:128], in1=btf[:, 512:640], op=ALU.add), [r_pB, r_b], [r_L])
        P.op("dve", lambda e: e.tensor_copy(out=Lt[:, 640:896], in_=pB[:, 128:384]), [r_pB], [r_L])
        P.op("dve", lambda e: e.reduce_max(out=s_[:, 0:1], in_=Lt[:], axis=AX.X), [r_L], [r_s])
        P.op("dve", lambda e: e.tensor_scalar(out=s_[:, 1:2], in0=s_[:, 0:1], scalar1=-1.0, scalar2=None, op0=ALU.mult), [r_s], [r_s])
        P.op("act", lambda e: e.activation(out=Pt[:], in_=Lt[:], func=AF.Exp, bias=s_[:, 1:2], accum_out=s_[:, 2:3]), [r_L, r_s], [r_P, r_s])

    def s2():
        for c in range(7):
            P.op("pe", lambda e, c=c: e.transpose(out=pt[:, c * 128:(c + 1) * 128], in_=Pt[:, c * 128:(c + 1) * 128], identity=identb[:]), [r_P, r_idb], [r_pt])
        P.op("act", lambda e: e.activation(out=PTt[:].rearrange("p c n -> p (c n)"), in_=pt[:, 0:896], func=AF.Copy), [r_pt], [r_PT])
        P.op("dve", lambda e: e.reciprocal(out=s_[:, 3:4], in_=s_[:, 2:3]), [r_s], [r_s])

    def s3():
        for c in range(7):
            tile_i = ts + c if c < 5 else 16 + (c - 5)
            P.op("pe", lambda e, c=c, tile_i=tile_i: e.matmul(osl, lhsT=PTt[:, c, :], rhs=nv[:, tile_i, pu], start=(c == 0), stop=(c == 6)), [r_PT] + allv, [r_po])
        P.op("act", lambda e: e.activation(out=yn[:, m, pu], in_=osl, func=AF.Identity, scale=s_[:, 3:4]), [r_po, r_s], [r_yn[m]])

    return [s1, s2, s3]


def phase_A_na(kb, b, hp, xmT, catT, identb):
    nc = kb.nc
    T = kb.t
    ph = Phase(kb, f"AN{hp}")
    P = ph.P
    r_xm = R(); r_idb = R(); r_cat = R()
    NTA = NT + 2
    wst = ph.sb([128, 8, 3, 128]); r_wst = R()
    for j, off in enumerate((OFF_NQ, OFF_NK, OFF_NV)):
        ld(P, wst[:, :, j, :], T["w_in"][:, off + hp * 128:off + (hp + 1) * 128].rearrange("(k p) n -> p k n", p=128), [], [r_wst])
    wb = ph.sb([128, 8, 384], BF16); r_wb = R()
    for k in range(8):
        P.op("pool" if k % 2 else "dve", lambda e, k=k: e.tensor_copy(out=wb[:, k, :], in_=wst[:, k, :, :].rearrange("p a n -> p (a n)")), [r_wst], [r_wb])
    qT = ph.sb([128, NL], BF16); r_qT = [R() for _ in range(4)]
    kT = ph.sb([128, NL + NCX], BF16); r_kT = [R() for _ in range(5)]
    nv = ph.sb([128, NTA, 128], BF16); r_nv = [R() for _ in range(5)]
    bk = ph.banks(4)
    bi = 0
    for g in range(5):
        n = 512 if g < 4 else 256
        o = g * 512
        if g < 4:
            pb, r_pb = bk[bi % 4]; bi += 1
            for k in range(8):
                P.op("pe", lambda e, k=k, pb=pb, o=o, n=n: e.matmul(pb[:, 0:n], lhsT=wb[:, k, 0:128], rhs=xmT[:, k, o:o + n], start=(k == 0), stop=(k == 7)), [r_wb, r_xm], [r_pb])
            P.op("act", lambda e, pb=pb, o=o, n=n: e.activation(out=qT[:, o:o + n], in_=pb[:, 0:n], func=AF.Identity, scale=0.125), [r_pb], [r_qT[g]])
        pb, r_pb = bk[bi % 4]; bi += 1
        for k in range(8):
            P.op("pe", lambda e, k=k, pb=pb, o=o, n=n: e.matmul(pb[:, 0:n], lhsT=wb[:, k, 128:256], rhs=xmT[:, k, o:o + n], start=(k == 0), stop=(k == 7)), [r_wb, r_xm], [r_pb])
        P.op("dve", lambda e, pb=pb, o=o, n=n: e.tensor_copy(out=kT[:, o:o + n], in_=pb[:, 0:n]), [r_pb], [r_kT[g]])
        pb, r_pb = bk[bi % 4]; bi += 1
        ntl = n // 128
        for tt in range(ntl):
            for k in range(8):
                P.op("pe", lambda e, k=k, pb=pb, o=o, tt=tt: e.matmul(pb[:, tt * 128:(tt + 1) * 128], lhsT=xmT[:, k, o + tt * 128:o + (tt + 1) * 128], rhs=wb[:, k, 256:384],
                                                                   start=(k == 0), stop=(k == 7)), [r_wb, r_xm], [r_pb])
        P.op("act", lambda e, pb=pb, g=g, ntl=ntl: e.activation(out=nv[:, g * 4:g * 4 + ntl, :].rearrange("p t n -> p (t n)"), in_=pb[:, 0:ntl * 128], func=AF.Copy), [r_pb], [r_nv[g]])
    allq = r_qT; allk = r_kT; allv = r_nv
    variants = na_variants()
    ptb = [(ph.ps([128, 1024], BF16), R()) for _ in range(2)]
    ob = [(ph.ps([128, 512], F32), R()) for _ in range(2)]
    cb = (bk[0][0][:].bitcast(BF16), bk[0][1])
    yn = ph.sb([128, NT, 128], BF16); r_yn = [R() for _ in range(NT)]
    L = [(ph.sb([128, 896]), R()) for _ in range(3)]
    Pe = [(ph.sb([128, 896], BF16), R()) for _ in range(3)]
    PT = [(ph.sb([128, 7, 128], BF16), R()) for _ in range(3)]
    st = [(ph.sb([128, 4]), R()) for _ in range(3)]
    units = []
    it = 0
    for u in range(2):
        h = 2 * hp + u
        pu = slice(64 * u, 64 * u + 64)
        tb = ph.sb([128, 15, 64]); r_tb = R()
        ld(P, tb[:], T["TOEP"][h], [], [r_tb])
        bias = {}
        for vi, (key, ms) in enumerate(variants.items()):
            bt = ph.sb([128, 10, 64]); r_b = R()
            P.op("pool", lambda e, bt=bt: e.memset(bt[:], NEG), [], [r_b])
            for uu in range(2):
                i_lo, a_lo = key[uu]
                pp = slice(64 * uu, 64 * uu + 64)
                P.op("pool", lambda e, bt=bt, tb=tb, pp=pp, i_lo=i_lo, a_lo=a_lo: e.tensor_copy(out=bt[pp, i_lo:i_lo + 8, :], in_=tb[pp, a_lo:a_lo + 8, :]), [r_tb, r_b], [r_b])
            for m in ms:
                bias[m] = (bt, r_b)
        for m in range(16):
            units.append(na_unit(P, it, m, u, pu, bias[m], bk, L, Pe, PT, st, ptb, ob, qT, kT, nv, yn, allq, allk, allv, r_yn, identb, r_idb))
            it += 1
    emit_pipelined(units)
    pc, r_pc = cb
    for m in range(16):
        P.op("pe", lambda e, m=m: e.transpose(out=pc[:, (m % 8) * 128:(m % 8 + 1) * 128], in_=yn[:, m, :], identity=identb[:]), [r_yn[m], r_idb], [r_pc])
        if m % 8 == 7:
            m0 = m - 7
            P.op("act", lambda e, m0=m0: e.activation(out=catT[:, 4 + hp, m0 * 128:(m0 + 8) * 128], in_=pc[:, 0:1024], func=AF.Copy), [r_pc], [r_cat])
    ph.finish()


def phase_E(kb, l, b, src, dst):
    nc = kb.nc
    T = kb.t
    pers = ExitStack()
    Phase.cnt += 1
    acc = pers.enter_context(nc.sbuf_tensor(f"E_acc{Phase.cnt}", [128, NT, D], F32))
    x2T = pers.enter_context(nc.sbuf_tensor(f"E_x2T{Phase.cnt}", [128, 8, NL], BF16))
    comb = pers.enter_context(nc.sbuf_tensor(f"E_comb{Phase.cnt}", [128, NT, 32], F32))

    ph = Phase(kb, "Ea")
    P = ph.P
    identf = ph.sb([128, 128]); r_id = R()
    ld(P, identf[:], T["ident"], [], [r_id])
    mT, r_mT = load_mT(ph, l)
    sc1, sh, r_mod = make_mod_cols(ph, mT, r_mT, 3, 4, b)
    wr = ph.sb([128, 8, 36]); r_wr = R()
    ld(P, wr[:, :, 0:4], T["w_r1"][l].rearrange("(k p) n -> p k n", p=128), [], [r_wr])
    for g in range(4):
        ld(P, wr[:, :, 4 + g * 8:12 + g * 8], T["w_r2"][l, g].rearrange("(k p) n -> p k n", p=128), [], [r_wr])
    brow = ph.sb([128, 36]); r_br = R()
    ld(P, brow[:, 0:4], T["b_r1"][l].partition_broadcast(128), [], [r_br])
    ld(P, brow[:, 4:36], T["b_r2"][l].rearrange("g e -> (g e)").partition_broadcast(128), [], [r_br])
    bk = ph.banks(6)
    rb = ph.banks(2)
    xin = [(ph.sb([128, 4, D]), R()) for _ in range(2)]
    xf = [(ph.sb([128, 8, 512]), [R() for _ in range(8)]) for _ in range(2)]
    logit = ph.sb([128, NT, 36]); r_lg = R()
    r_x = [R() for _ in range(4)]

    def extra(g, k, pb, r_pb, nt):
        xt, r_xf = xf[g % 2]
        if k < 8:
            P.op("dve", lambda e: e.tensor_scalar(out=xt[:, k, :], in0=pb[:, 0:512], scalar1=sc1[:, k:k + 1], scalar2=sh[:, k:k + 1], op0=ALU.mult, op1=ALU.add), [r_pb, r_mod], [r_xf[k]])
            return xt, r_xf[k]
        pr, r_pr = rb[g % 2]
        for tt in range(4):
            for kk in range(8):
                P.op("pe", lambda e, tt=tt, kk=kk: e.matmul(pr[:, tt * 36:(tt + 1) * 36], lhsT=xt[:, kk, tt * 128:(tt + 1) * 128], rhs=wr[:, kk, :], start=(kk == 0), stop=(kk == 7)),
                     r_xf + [r_wr], [r_pr])
        P.op("dve", lambda e: e.tensor_tensor(out=logit[:, g * 4:(g + 1) * 4, :], in0=pr[:, 0:144].rearrange("p (t n) -> p t n", t=4),
                                              in1=brow[:].unsqueeze(1).to_broadcast([128, 4, 36]), op=ALU.add), [r_pr, r_br], [r_lg])

    build_xT(ph, src[b], 16, x2T, 0, sc1, sh, r_mod, identf, r_id, bk, r_x, xin, extra=extra, all_act=True)
    def dv(fn, reads, writes):
        P.op("dve", fn, reads, writes)
    s4 = ph.sb([128, NT, 4]); mg = ph.sb([128, NT, 4]); r_a = R()
    s1 = ph.sb([128, NT, 8]); r_s1 = R()
    lg4 = logit[:, :, 0:4]
    le = logit[:, :, 4:36].rearrange("p t (g e) -> p t g e", g=4)
    dv(lambda e: e.tensor_reduce(out=s1[:, :, 0], in_=lg4, axis=AX.X, op=ALU.max), [r_lg], [r_s1])
    dv(lambda e: e.tensor_tensor(out=mg[:], in0=lg4, in1=s1[:, :, 0:1].to_broadcast([128, NT, 4]), op=ALU.is_equal), [r_lg, r_s1], [r_a])
    dv(lambda e: e.tensor_tensor(out=s4[:], in0=lg4, in1=s1[:, :, 0:1].to_broadcast([128, NT, 4]), op=ALU.subtract), [r_lg, r_s1], [r_a])
    P.op("act", lambda e: e.activation(out=s4[:], in_=s4[:], func=AF.Exp), [r_a], [r_a])
    dv(lambda e: e.tensor_reduce(out=s1[:, :, 1], in_=s4[:], axis=AX.X, op=ALU.add), [r_a], [r_s1])
    dv(lambda e: e.reciprocal(out=s1[:, :, 2], in_=s1[:, :, 1]), [r_s1], [r_s1])
    t48 = ph.sb([128, NT, 4, 8]); r_t48 = R()
    dv(lambda e: e.tensor_tensor(out=t48[:], in0=le, in1=mg[:].unsqueeze(3).to_broadcast([128, NT, 4, 8]), op=ALU.mult), [r_lg, r_a], [r_t48])
    ls = ph.sb([128, NT, 8]); l2 = ph.sb([128, NT, 8]); k1 = ph.sb([128, NT, 8]); k2 = ph.sb([128, NT, 8]); r_ls = R()
    dv(lambda e: e.tensor_reduce(out=ls[:], in_=t48[:].rearrange("p t g e -> p t e g"), axis=AX.X, op=ALU.add), [r_t48], [r_ls])
    dv(lambda e: e.tensor_reduce(out=s1[:, :, 3], in_=ls[:], axis=AX.X, op=ALU.max), [r_ls], [r_s1])
    dv(lambda e: e.tensor_tensor(out=k1[:], in0=ls[:], in1=s1[:, :, 3:4].to_broadcast([128, NT, 8]), op=ALU.is_equal), [r_ls, r_s1], [r_ls])
    dv(lambda e: e.scalar_tensor_tensor(out=l2[:], in0=k1[:], scalar=-1e30, in1=ls[:], op0=ALU.mult, op1=ALU.add), [r_ls], [r_ls])
    dv(lambda e: e.tensor_reduce(out=s1[:, :, 4], in_=l2[:], axis=AX.X, op=ALU.max), [r_ls], [r_s1])
    dv(lambda e: e.tensor_tensor(out=k2[:], in0=l2[:], in1=s1[:, :, 4:5].to_broadcast([128, NT, 8]), op=ALU.is_equal), [r_ls, r_s1], [r_ls])
    dv(lambda e: e.tensor_tensor(out=s1[:, :, 5], in0=s1[:, :, 4], in1=s1[:, :, 3], op=ALU.subtract), [r_s1], [r_s1])
    P.op("act", lambda e: e.activation(out=s1[:, :, 5], in_=s1[:, :, 5], func=AF.Exp), [r_s1], [r_s1])
    dv(lambda e: e.tensor_scalar(out=s1[:, :, 6], in0=s1[:, :, 5], scalar1=1.0, scalar2=None, op0=ALU.add), [r_s1], [r_s1])
    dv(lambda e: e.reciprocal(out=s1[:, :, 6], in_=s1[:, :, 6]), [r_s1], [r_s1])
    dv(lambda e: e.tensor_tensor(out=s1[:, :, 6], in0=s1[:, :, 6], in1=s1[:, :, 2], op=ALU.mult), [r_s1], [r_s1])
    dv(lambda e: e.tensor_tensor(out=s1[:, :, 7], in0=s1[:, :, 6], in1=s1[:, :, 5], op=ALU.mult), [r_s1], [r_s1])
    dv(lambda e: e.tensor_tensor(out=k1[:], in0=k1[:], in1=s1[:, :, 6:7].to_broadcast([128, NT, 8]), op=ALU.mult), [r_ls, r_s1], [r_ls])
    dv(lambda e: e.tensor_tensor(out=k2[:], in0=k2[:], in1=s1[:, :, 7:8].to_broadcast([128, NT, 8]), op=ALU.mult), [r_ls, r_s1], [r_ls])
    dv(lambda e: e.tensor_tensor(out=k1[:], in0=k1[:], in1=k2[:], op=ALU.add), [r_ls], [r_ls])
    r_comb = R()
    dv(lambda e: e.tensor_tensor(out=comb[:].rearrange("p t (g e) -> p t g e", g=4), in0=mg[:].unsqueeze(3).to_broadcast([128, NT, 4, 8]),
                                 in1=k1[:].unsqueeze(2).to_broadcast([128, NT, 4, 8]), op=ALU.mult), [r_a, r_ls], [r_comb])
    if kb.dbg:
        ld(P, T["dbg_comb"][b], comb[:], [r_comb], [])
    ph.finish()

    if getattr(kb, "stop", None) == "Ea":
        pers.close()
        return
    ph = Phase(kb, "Eb")
    P = ph.P
    r_x2 = R(); r_comb = R()
    stg = [(ph.sb([128, 2048]), R()) for _ in range(4)]
    wbf = [dict(g=(ph.sb([128, 8, 512], BF16), R()), u=(ph.sb([128, 8, 512], BF16), R()), d=(ph.sb([128, 4, D], BF16), R())) for _ in range(2)]
    hT = [(ph.sb([128, 4, 512], BF16), R()) for _ in range(2)]
    sg = [(ph.sb([128, 512], BF16), R()) for _ in range(2)]
    gb = ph.banks(2); ub = ph.banks(2); yb = ph.banks(4)
    r_acc = [R() for _ in range(NT)]
    si = 0
    gi_box = [0]
    yi_box = [0]
    pending_down = [None]
    for ex in range(32):
        w = wbf[ex % 2]
        for name, srcw in (("g", T["w_gate"][l, ex]), ("u", T["w_up"][l, ex]), ("d", T["w_down"][l, ex])):
            wt, r_wt = w[name]
            for piece in range(2):
                st_, r_st = stg[si % 4]; si += 1
                if name == "d":
                    ld(P, st_[:].rearrange("p (k n) -> p k n", k=2), srcw[piece * 256:(piece + 1) * 256, :].rearrange("(k p) n -> p k n", p=128), [], [r_st])
                    P.op("pool", lambda e, wt=wt, st_=st_, piece=piece: e.tensor_copy(out=wt[:, piece * 2:(piece + 1) * 2, :].rearrange("p k n -> p (k n)"), in_=st_[:]), [r_st], [r_wt])
                else:
                    ld(P, st_[:].rearrange("p (k n) -> p k n", k=4), srcw[piece * 512:(piece + 1) * 512, :].rearrange("(k p) n -> p k n", p=128), [], [r_st])
                    P.op("pool", lambda e, wt=wt, st_=st_, piece=piece: e.tensor_copy(out=wt[:, piece * 4:(piece + 1) * 4, :].rearrange("p k n -> p (k n)"), in_=st_[:]), [r_st], [r_wt])
        (wg, r_wg), (wu, r_wu), (wd, r_wd) = w["g"], w["u"], w["d"]
        for tg in range(4):
            h_, r_h = hT[(ex * 4 + tg) % 2]

            def up(ex=ex, tg=tg, h_=h_, r_h=r_h, wg=wg, r_wg=r_wg, wu=wu, r_wu=r_wu):
                for hc in range(4):
                    gi = gi_box[0]
                    gi_box[0] += 1
                    (pg, r_pg), (pu_, r_pu) = gb[gi % 2], ub[gi % 2]
                    s_, r_s = sg[gi % 2]
                    for k in range(8):
                        P.op("pe", lambda e, k=k, pg=pg, hc=hc: e.matmul(pg[:], lhsT=wg[:, k, hc * 128:(hc + 1) * 128], rhs=x2T[:, k, tg * 512:(tg + 1) * 512], start=(k == 0), stop=(k == 7)),
                             [r_wg, r_x2], [r_pg])
                    for k in range(8):
                        P.op("pe", lambda e, k=k, pu_=pu_, hc=hc: e.matmul(pu_[:], lhsT=wu[:, k, hc * 128:(hc + 1) * 128], rhs=x2T[:, k, tg * 512:(tg + 1) * 512], start=(k == 0), stop=(k == 7)),
                             [r_wu, r_x2], [r_pu])
                    P.op("act", lambda e, s_=s_, pg=pg: e.activation(out=s_[:], in_=pg[:], func=AF.Silu), [r_pg], [r_s])
                    P.op("dve", lambda e, s_=s_, pu_=pu_, hc=hc: e.tensor_tensor(out=h_[:, hc, :], in0=pu_[:], in1=s_[:], op=ALU.mult), [r_pu, r_s], [r_h])

            def down(ex=ex, tg=tg, h_=h_, r_h=r_h, wd=wd, r_wd=r_wd):
                for tt in range(4):
                    t = tg * 4 + tt
                    for half in range(2):
                        yi = yi_box[0]
                        yi_box[0] += 1
                        py, r_py = yb[yi % 4]
                        for k in range(4):
                            P.op("pe", lambda e, k=k, py=py, tt=tt, half=half: e.matmul(py[:], lhsT=h_[:, k, tt * 128:(tt + 1) * 128], rhs=wd[:, k, half * 512:(half + 1) * 512],
                                                                                 start=(k == 0), stop=(k == 3)), [r_h, r_wd], [r_py])
                        a_sl = acc[:, t, half * 512:(half + 1) * 512]
                        cw = comb[:, t, ex:ex + 1]
                        if ex == 0:
                            P.op("dve", lambda e, a_sl=a_sl, py=py, cw=cw: e.tensor_scalar(out=a_sl, in0=py[:], scalar1=cw, scalar2=None, op0=ALU.mult), [r_py, r_comb], [r_acc[t]])
                        else:
                            P.op("dve", lambda e, a_sl=a_sl, py=py, cw=cw: e.scalar_tensor_tensor(out=a_sl, in0=py[:], scalar=cw, in1=a_sl, op0=ALU.mult, op1=ALU.add), [r_py, r_comb, r_acc[t]], [r_acc[t]])

            up()
            if pending_down[0] is not None:
                pending_down[0]()
            pending_down[0] = down
    pending_down[0]()
    ph.finish()

    if getattr(kb, "stop", None) == "Eb":
        pers.close()
        return
    ph = Phase(kb, "Ec")
    P = ph.P
    gate, r_gate = load_bcast(ph, T["MV"][l, b, 5 * D:6 * D])
    lng, r_lng = load_bcast(ph, T["ln_g"][l, 1])
    lnb, r_lnb = load_bcast(ph, T["ln_b"][l, 1])
    bufs = pn_bufs(ph)
    r_acc2 = R()
    emit_pipelined([post_norm(ph, [acc[:, t, :]], [r_acc2], src[b, t * 128:(t + 1) * 128, :], gate, r_gate, lng, r_lng, lnb, r_lnb,
                              dst[b, t * 128:(t + 1) * 128, :], bufs, t) for t in range(NT)])
    ph.finish()
    pers.close()


def phase_P(kb, b, src, dst):
    nc = kb.nc
    T = kb.t
    l = 1
    NP_ = NL + 16
    pers = ExitStack()
    Phase.cnt += 1
    zT = pers.enter_context(nc.sbuf_tensor(f"P_zT{Phase.cnt}", [128, 8, NL], BF16))
    ph = Phase(kb, "Pa")
    P = ph.P
    identf = ph.sb([128, 128]); r_id = R()
    ld(P, identf[:], T["ident"], [], [r_id])
    mT, r_mT = load_mT(ph, l)
    sc1, sh, r_mod = make_mod_cols(ph, mT, r_mT, 0, 1, b)
    hm = ph.sb([128, 8, NP_]); r_hm = [R() for _ in range(4)]
    r_pad = R()
    P.op("pool", lambda e: e.memset(hm[:, :, 0:8], 0.0), [], [r_pad])
    P.op("pool", lambda e: e.memset(hm[:, :, NL + 8:NL + 16], 0.0), [], [r_pad])
    bk = ph.banks(6)
    xin = [(ph.sb([128, 4, D]), R()) for _ in range(2)]
    build_xT(ph, src[b], 16, hm, 8, sc1, sh, r_mod, identf, r_id, bk, r_hm, xin)
    invc = ph.sb([128, 4, 16]); r_ic = R()
    ld(P, invc[:], T["invc"], [], [r_ic])
    A = [(ph.sb([128, NP_]), R()) for _ in range(2)]
    Bf = [(ph.sb([128, NP_]), R()) for _ in range(2)]
    zb = [(ph.sb([128, 16]), R()) for _ in range(2)]
    r_z = R()
    allhm = r_hm + [r_pad]
    for c in range(8):
        g = c // 2
        w = (2, 4, 8, 16)[g]
        x = hm[:, c, :]
        eng = "dve" if c % 2 == 0 else "pool"
        (a, r_a), (bb, r_b) = A[c % 2], Bf[c % 2]
        P.op(eng, lambda e, a=a, x=x: e.tensor_tensor(out=a[:, 1:NL + 15], in0=x[:, 0:NL + 14], in1=x[:, 1:NL + 15], op=ALU.add), allhm, [r_a])
        cur, r_cur = a, r_a
        if g >= 1:
            P.op(eng, lambda e, a=a, bb=bb: e.tensor_tensor(out=bb[:, 2:NL + 14], in0=a[:, 1:NL + 13], in1=a[:, 3:NL + 15], op=ALU.add), [r_a], [r_b])
            cur, r_cur = bb, r_b
        if g >= 2:
            P.op(eng, lambda e, a=a, bb=bb: e.tensor_tensor(out=a[:, 4:NL + 12], in0=bb[:, 2:NL + 10], in1=bb[:, 6:NL + 14], op=ALU.add), [r_b], [r_a])
            cur, r_cur = a, r_a
        if g >= 3:
            P.op(eng, lambda e, a=a, bb=bb: e.tensor_tensor(out=bb[:, 8:NL + 8], in0=a[:, 4:NL + 4], in1=a[:, 12:NL + 12], op=ALU.add), [r_a], [r_b])
            cur, r_cur = bb, r_b
        P.op("dve", lambda e, cur=cur, x=x, c=c, w=w: e.scalar_tensor_tensor(out=zT[:, c, :], in0=cur[:, 8:NL + 8], scalar=1.0 / w, in1=x[:, 8:NL + 8], op0=ALU.mult, op1=ALU.subtract),
             [r_cur] + allhm, [r_z])
        z_, r_zb = zb[c % 2]
        for side, (o_src, o_dst) in enumerate(((8, 0), (NL, NL - 8))):
            P.op(eng, lambda e, cur=cur, z_=z_, g=g, side=side, o_src=o_src: e.tensor_tensor(out=z_[:, side * 8:(side + 1) * 8], in0=cur[:, o_src:o_src + 8], in1=invc[:, g, side * 8:(side + 1) * 8], op=ALU.mult),
                 [r_cur, r_ic], [r_zb])
            P.op(eng, lambda e, z_=z_, x=x, c=c, side=side, o_src=o_src, o_dst=o_dst: e.tensor_tensor(out=zT[:, c, o_dst:o_dst + 8], in0=z_[:, side * 8:(side + 1) * 8], in1=x[:, o_src:o_src + 8], op=ALU.subtract),
                 [r_zb] + allhm, [r_z])
    ph.finish()
    ph = Phase(kb, "Pb")
    P = ph.P
    pws = ph.sb([128, 4, 2, 256]); r_pws = R()
    for g in range(4):
        ld(P, pws[:, g, :, :], T["pool_w"][g].rearrange("(k p) e -> p k e", p=128), [], [r_pws])
    pw = ph.sb([128, 4, 2, 256], BF16); r_pw = R()
    P.op("pool", lambda e: e.tensor_copy(out=pw[:], in_=pws[:]), [r_pws], [r_pw])
    gate, r_gate = load_bcast(ph, T["MV"][l, b, 2 * D:3 * D])
    psc, r_psc = load_bcast(ph, T["pool_scale"])
    P.op("dve", lambda e: e.tensor_tensor(out=gate[:], in0=gate[:], in1=psc[:], op=ALU.mult), [r_gate, r_psc], [r_gate])
    lng, r_lng = load_bcast(ph, T["ln_g"][l, 0])
    lnb, r_lnb = load_bcast(ph, T["ln_b"][l, 0])
    bufs = pn_bufs(ph)
    bk = ph.banks(4)
    r_z = R()
    units = []
    for t in range(NT):
        (p0, r0), (p1, r1) = bk[(t % 2) * 2], bk[(t % 2) * 2 + 1]

        def pre(t=t, p0=p0, r0=r0, p1=p1, r1=r1):
            for g in range(4):
                pb, r_pb = (p0, r0) if g < 2 else (p1, r1)
                for kk in range(2):
                    P.op("pe", lambda e, pb=pb, g=g, kk=kk: e.matmul(pb[:, (g % 2) * 256:(g % 2 + 1) * 256], lhsT=zT[:, 2 * g + kk, t * 128:(t + 1) * 128], rhs=pw[:, g, kk, :],
                                                                  start=(kk == 0), stop=(kk == 1)), [r_z, r_pw], [r_pb])
        units.append(post_norm(ph, [p0[:], p1[:]], [r0, r1], src[b, t * 128:(t + 1) * 128, :], gate, r_gate, lng, r_lng, lnb, r_lnb,
                               dst[b, t * 128:(t + 1) * 128, :], bufs, t, pre=pre))
    emit_pipelined(units)
    ph.finish()
    pers.close()


IN_SHAPES = {
    "x": [2, NL, D], "ctx": [2, NCX, D], "c": [2, D], "c_ctx": [D],
    "w_mod": [2, D, 6 * D], "b_mod": [2, 6 * D], "ln_g": [2, 2, D], "ln_b": [2, 2, D],
    "w_in": [D, 3584], "w_out": [D, D], "log_decay": [2, 4], "rpb": [8, 15, 31],
    "pool_w": [4, 256, 256], "pool_scale": [D],
    "w_r1": [2, D, 4], "b_r1": [2, 4], "w_r2": [2, 4, D, 8], "b_r2": [2, 4, 8],
    "w_gate": [2, 32, D, 512], "w_up": [2, 32, D, 512], "w_down": [2, 32, 512, D],
    "ident": [128, 128], "rope": [128, NT, 2, 64], "rtab": [128, 4, 128], "ktab": [128, 4],
    "colmask": [128, 64], "invc": [128, 4, 16],
}
SCRATCH = {
    "MV": [2, 3, 6 * D], "MT": [2, 128, 48, 3], "RP": [8, 15, 128], "TOEP": [8, 128, 15, 64],
    "H1": [2, NL, D], "H2": [2, NL, D], "H3": [2, NL, D],
}


class KB:
    pass


def build(phases=("M", "A", "E0", "P", "E1"), ext_in=(), ext_out=(), dbg=False, nb=2, stop=None):
    nc = bass.Bass("TRN2", target_bir_lowering=False)
    kb = KB()
    kb.stop = stop
    kb.nc = nc
    kb.dbg = dbg
    kb.t = {}
    for k, shp in IN_SHAPES.items():
        kb.t[k] = nc.dram_tensor(k, list(shp), F32, kind="ExternalInput").ap()
    for k, shp in SCRATCH.items():
        kind = "ExternalInput" if k in ext_in else ("ExternalOutput" if k in ext_out else "Internal")
        kb.t[k] = nc.dram_tensor(k, list(shp), F32, kind=kind).ap()
    kb.t["out"] = nc.dram_tensor("out", [2, NL, D], F32, kind="ExternalOutput").ap()
    if dbg:
        kb.t["dbg_comb"] = nc.dram_tensor("dbg_comb", [2, 128, NT, 32], F32, kind="ExternalOutput").ap()
    es = ExitStack()
    kb.ss = SemState(nc, es)
    T = kb.t
    with es:
        if "M" in phases:
            phase_M(kb)
        for b in range(nb):
            if "A" in phases:
                phase_A(kb, b)
            if "E0" in phases:
                phase_E(kb, 0, b, T["H1"], T["H2"])
            if "P" in phases:
                phase_P(kb, b, T["H2"], T["H3"])
            if "E1" in phases:
                phase_E(kb, 1, b, T["H3"], T["out"])
    return nc


def host_consts():
    f32 = np.float32
    c = {}
    c["ident"] = np.eye(128, dtype=f32)
    pos = (np.arange(NT)[None, :] * 128 + np.arange(128)[:, None]).astype(np.int64)
    rows = (pos // 64).astype(f32)
    cols = (pos % 64).astype(f32)
    n_freq = 32
    inv_freq = (np.float32(10000.0) ** (-np.arange(n_freq, dtype=f32) / np.float32(n_freq))).astype(f32)
    ang = np.concatenate([rows[..., None] * inv_freq, cols[..., None] * inv_freq], axis=-1).astype(f32)
    c["rope"] = np.stack([np.cos(ang), np.sin(ang)], axis=2).astype(f32)
    j = np.arange(128, dtype=f32)[:, None]
    i = np.arange(128, dtype=f32)[None, :]
    BIGT = np.float32(1e6)
    tf = np.where(i >= j, i - j, BIGT)
    tb = np.where(j >= i, j - i, BIGT)
    qif = np.broadcast_to(i + 1.0, (128, 128))
    qib = np.broadcast_to(128.0 - i, (128, 128))
    c["rtab"] = np.stack([tf, tb, qif, qib], axis=1).astype(f32)
    p = np.arange(128, dtype=f32)
    c["ktab"] = np.stack([127.0 - p, p, np.full(128, 128.0, f32), np.full(128, 128.0, f32)], axis=1).astype(f32)
    qc = np.arange(64)[:, None]
    kc = np.arange(64)[None, :]
    ws = np.clip(qc - 8, 0, 48)
    ok = (kc >= ws) & (kc < ws + 16)
    cm = np.where(ok, 0.0, NEG).astype(f32)
    c["colmask"] = np.concatenate([cm, cm], axis=0)
    invc = np.zeros((4, 16), f32)
    for g, w in enumerate((2, 4, 8, 16)):
        for s, t0 in enumerate((0, NL - 8)):
            for k in range(8):
                t = t0 + k
                lo = min(max(t - w // 2, 0), NL)
                hi = min(max(t + (w - w // 2), 0), NL)
                invc[g, s * 8 + k] = 1.0 / (hi - lo)
    c["invc"] = np.broadcast_to(invc[None], (128, 4, 16)).astype(f32).copy()
    return c


def make_in_maps(inputs, n_cores=8, nb=2):
    consts = host_consts()
    f = lambda a: np.ascontiguousarray(np.asarray(a, dtype=np.float32))
    shared = {
        "c_ctx": f(inputs["c_ctx"]), "w_mod": f(inputs["w_mod"]), "b_mod": f(inputs["b_mod"]),
        "ln_g": f(inputs["ln_g"]), "ln_b": f(inputs["ln_b"]),
        "w_in": f(inputs["ab_w_in"][0]), "w_out": f(inputs["ab_w_out"][0]),
        "log_decay": f(inputs["ab_log_decay"][0]), "rpb": f(inputs["ab_rpb"][0]),
        "pool_w": f(inputs["pool_w"][0]), "pool_scale": f(inputs["pool_scale"][0]),
        "w_r1": f(inputs["moe_w_r1"]), "b_r1": f(inputs["moe_b_r1"]), "w_r2": f(inputs["moe_w_r2"]), "b_r2": f(inputs["moe_b_r2"]),
        "w_gate": f(inputs["moe_w_gate"]), "w_up": f(inputs["moe_w_up"]), "w_down": f(inputs["moe_w_down"]),
    }
    shared.update(consts)
    maps = []
    for i in range(n_cores):
        m = dict(shared)
        m["x"] = f(inputs["x"][i * nb:(i + 1) * nb])
        m["ctx"] = f(inputs["ctx"][i * nb:(i + 1) * nb])
        m["c"] = f(inputs["c"][i * nb:(i + 1) * nb])
        maps.append(m)
    return maps


def kernel(**inputs):
    nc = build()
    maps = make_in_maps(inputs, n_cores=8, nb=2)
    res = run_bass_kernel_spmd(nc, maps, core_ids=list(range(8)))
    out = np.concatenate([np.asarray(r["out"]) for r in res.results], axis=0)
    return out.astype(np.float32)
```
